# Optimizing a Trainium2 kernel written in Bass

```python
import jax
import jax.numpy as jnp
from jax import lax
import numpy as np

D_MODEL = 1024
BATCH = 16
SEQ = 2048
DEPTH = 1

N_MOD = 9
D_FF = 2816
EPS = 1e-6
ATT_GROUPS = ((128, 1), (512, 4), (2048, 16))
ATT_HEADS_PER_GROUP = 4
ATT_HEADS = ATT_HEADS_PER_GROUP * len(ATT_GROUPS)
ATT_HEAD_DIM = 64
ATT_WIDTH = ATT_HEADS * ATT_HEAD_DIM
ATT_OUT_WIDTH = ATT_HEADS_PER_GROUP * ATT_HEAD_DIM
ALIBI_MAX = 8.0
GLA_HEADS = 4
GLA_KEY_DIM = D_MODEL // 2
GLA_VAL_DIM = D_MODEL
GLA_DK = GLA_KEY_DIM // GLA_HEADS
GLA_DV = GLA_VAL_DIM // GLA_HEADS
GLA_GATE_RANK = 16
GLA_TAU = 16.0
GLA_CHUNK = 64
IN_SPLITS = (ATT_WIDTH, ATT_WIDTH, ATT_WIDTH, GLA_KEY_DIM, GLA_KEY_DIM, GLA_VAL_DIM, GLA_VAL_DIM, GLA_GATE_RANK, D_MODEL, D_MODEL)
IN_WIDTH = sum(IN_SPLITS)

kernel_name = 'hybrid_dilated_attn_gla_macaron_adaln'


def rms_norm(x, g):
    x32 = x.astype(jnp.float32)
    y = x32 * lax.rsqrt(jnp.mean(x32 * x32, axis=-1, keepdims=True) + EPS)
    return (y * g.astype(jnp.float32)).astype(x.dtype)


def modulate(h, shift, scale):
    return h * (1 + scale[:, None, :]) + shift[:, None, :]


def swiglu(u, w1, w3, w2):
    return (jax.nn.silu(u @ w1) * (u @ w3)) @ w2


def alibi_slopes(n):
    return 2.0 ** (-ALIBI_MAX * jnp.arange(1, n + 1, dtype=jnp.float32) / n)


def dilated_window_attention(q, k, v, slopes, window, dilation):
    B, S, H, Dh = q.shape
    blk = window // dilation
    L = S // dilation
    nb = -(-L // blk)
    pad = nb * blk - L

    def to_blocks(t):
        t = t.reshape(B, L, dilation, H, Dh)
        t = jnp.pad(t, ((0, 0), (0, pad), (0, 0), (0, 0), (0, 0)))
        return t.reshape(B, nb, blk, dilation, H, Dh)

    def band(t):
        prev = jnp.pad(t[:, :-1], ((0, 0), (1, 0), (0, 0), (0, 0), (0, 0), (0, 0)))
        return jnp.concatenate([prev, t], axis=2)

    qb = to_blocks(q)
    kk = band(to_blocks(k))
    vv = band(to_blocks(v))
    s = jnp.einsum('bnqrhd,bnkrhd->bnrhqk', qb, kk).astype(jnp.float32)
    qi = jnp.arange(blk)[:, None]
    kj = jnp.arange(2 * blk)[None, :]
    steps = qi + blk - kj
    valid = (steps >= 0) & (steps <= blk)
    valid = valid[None] & ~((jnp.arange(nb) == 0)[:, None, None] & (kj < blk)[None])
    bias = -(slopes * dilation)[:, None, None] * steps.astype(jnp.float32)
    s = jnp.where(valid[None, :, None, None], s + bias, -jnp.inf)
    m = jnp.max(s, axis=-1, keepdims=True)
    e = jnp.exp(s - m)
    den = jnp.sum(e, axis=-1, keepdims=True)
    p = (e / den).astype(v.dtype)
    lse = (m + jnp.log(den))[..., 0]
    o = jnp.einsum('bnrhqk,bnkrhd->bnqrhd', p, vv)
    o = o.reshape(B, nb * blk, dilation, H, Dh)[:, :L].reshape(B, S, H, Dh)
    lse = lse.transpose(0, 1, 4, 2, 3).reshape(B, nb * blk, dilation, H)[:, :L].reshape(B, S, H)
    return o, lse


def gla_chunked(q, k, v, log_a):
    B, S, H, Dk = q.shape
    Dv = v.shape[-1]
    C = GLA_CHUNK
    N = S // C

    def chunks(t):
        return t.astype(jnp.float32).reshape(B, N, C, H, t.shape[-1]).transpose(1, 0, 3, 2, 4)

    qc, kc, vc = chunks(q), chunks(k), chunks(v)
    bc = jnp.cumsum(chunks(log_a), axis=3)
    causal = jnp.tril(jnp.ones((C, C), dtype=bool))

    def step(state, inp):
        q_, k_, v_, b_ = inp
        b_last = b_[:, :, -1:, :]
        q_dec = q_ * jnp.exp(b_)
        attn = jnp.einsum('bhid,bhjd->bhij', q_dec, k_ * jnp.exp(-b_))
        attn = jnp.where(causal, attn, 0.0)
        o = jnp.einsum('bhij,bhjv->bhiv', attn, v_) + jnp.einsum('bhid,bhdv->bhiv', q_dec, state)
        state = (jnp.exp(b_last)[:, :, 0, :, None] * state
                 + jnp.einsum('bhjd,bhjv->bhdv', k_ * jnp.exp(b_last - b_), v_))
        return state, o

    state0 = jnp.zeros((B, H, Dk, Dv), jnp.float32)
    _, o = lax.scan(step, state0, (qc, kc, vc, bc))
    return o.transpose(1, 0, 3, 2, 4).reshape(B, S, H, Dv)


def token_mixing(u, w_in, q_norm_g, k_norm_g, gla_gate_up, gla_gate_bias, gla_out_norm_g,
                 w_branch_att, w_branch_gla, w_out):
    B, S, _ = u.shape
    split_points = [int(i) for i in np.cumsum(IN_SPLITS)[:-1]]
    (aq, ak, av, gq, gk, gv, gr, g_down, gate_att, gate_gla) = jnp.split(u @ w_in, split_points, axis=-1)

    aq = rms_norm(aq.reshape(B, S, ATT_HEADS, ATT_HEAD_DIM), q_norm_g) * (ATT_HEAD_DIM ** -0.5)
    ak = rms_norm(ak.reshape(B, S, ATT_HEADS, ATT_HEAD_DIM), k_norm_g)
    av = av.reshape(B, S, ATT_HEADS, ATT_HEAD_DIM)
    slopes = alibi_slopes(ATT_HEADS)
    outs, lses = [], []
    for gi, (window, dilation) in enumerate(ATT_GROUPS):
        hs = slice(gi * ATT_HEADS_PER_GROUP, (gi + 1) * ATT_HEADS_PER_GROUP)
        o_g, lse_g = dilated_window_attention(aq[:, :, hs], ak[:, :, hs], av[:, :, hs], slopes[hs], window, dilation)
        outs.append(o_g)
        lses.append(lse_g)
    mix_w = jax.nn.softmax(jnp.stack(lses), axis=0).astype(u.dtype)
    o_att = jnp.sum(mix_w[..., None] * jnp.stack(outs), axis=0).reshape(B, S, ATT_OUT_WIDTH)

    gq = gq.reshape(B, S, GLA_HEADS, GLA_DK) * (GLA_DK ** -0.5)
    gk = gk.reshape(B, S, GLA_HEADS, GLA_DK)
    gv = gv.reshape(B, S, GLA_HEADS, GLA_DV)
    log_a = jax.nn.log_sigmoid((g_down @ gla_gate_up + gla_gate_bias).astype(jnp.float32)) / GLA_TAU
    o_gla = gla_chunked(gq, gk, gv, log_a.reshape(B, S, GLA_HEADS, GLA_DK))
    o_gla = rms_norm(o_gla, gla_out_norm_g).astype(u.dtype) * jax.nn.silu(gr).reshape(B, S, GLA_HEADS, GLA_DV)
    o_gla = o_gla.reshape(B, S, GLA_VAL_DIM)

    merged = (jax.nn.sigmoid(gate_att) * (o_att @ w_branch_att)
              + jax.nn.sigmoid(gate_gla) * (o_gla @ w_branch_gla))
    return merged @ w_out


def setup_inputs(seed: int = 0) -> dict:
    key = jax.random.key(seed)
    ks = jax.random.split(key, 22)
    f32 = jnp.float32

    def nrm(k, shape, fan_in, scale=1.0):
        return jax.random.normal(k, shape, f32) * (scale * fan_in ** -0.5)

    def gain(k, shape):
        return 1.0 + 0.05 * jax.random.normal(k, shape, f32)

    return {
        'x': jax.random.normal(ks[0], (BATCH, SEQ, D_MODEL), f32),
        'c': jax.random.normal(ks[1], (BATCH, D_MODEL), f32),
        'w_mod': nrm(ks[2], (DEPTH, D_MODEL, N_MOD * D_MODEL), D_MODEL, 0.2),
        'b_mod': 0.01 * jax.random.normal(ks[3], (DEPTH, N_MOD * D_MODEL), f32),
        'g_ffn1': gain(ks[4], (DEPTH, D_MODEL)),
        'ffn1_w1': nrm(ks[5], (DEPTH, D_MODEL, D_FF), D_MODEL),
        'ffn1_w3': nrm(ks[6], (DEPTH, D_MODEL, D_FF), D_MODEL),
        'ffn1_w2': nrm(ks[7], (DEPTH, D_FF, D_MODEL), D_FF),
        'g_mix': gain(ks[8], (DEPTH, D_MODEL)),
        'w_in': nrm(ks[9], (DEPTH, D_MODEL, IN_WIDTH), D_MODEL),
        'q_norm_g': gain(ks[10], (DEPTH, ATT_HEAD_DIM)),
        'k_norm_g': gain(ks[11], (DEPTH, ATT_HEAD_DIM)),
        'gla_gate_up': nrm(ks[12], (DEPTH, GLA_GATE_RANK, GLA_KEY_DIM), GLA_GATE_RANK),
        'gla_gate_bias': 0.1 * jax.random.normal(ks[13], (DEPTH, GLA_KEY_DIM), f32),
        'gla_out_norm_g': gain(ks[14], (DEPTH, GLA_DV)),
        'w_branch_att': nrm(ks[15], (DEPTH, ATT_OUT_WIDTH, D_MODEL), ATT_OUT_WIDTH),
        'w_branch_gla': nrm(ks[16], (DEPTH, GLA_VAL_DIM, D_MODEL), GLA_VAL_DIM),
        'w_out': nrm(ks[17], (DEPTH, D_MODEL, D_MODEL), D_MODEL),
        'g_ffn2': gain(ks[18], (DEPTH, D_MODEL)),
        'ffn2_w1': nrm(ks[19], (DEPTH, D_MODEL, D_FF), D_MODEL),
        'ffn2_w3': nrm(ks[20], (DEPTH, D_MODEL, D_FF), D_MODEL),
        'ffn2_w2': nrm(ks[21], (DEPTH, D_FF, D_MODEL), D_FF),
    }


def reference(x, c, w_mod, b_mod, g_ffn1, ffn1_w1, ffn1_w3, ffn1_w2, g_mix, w_in, q_norm_g, k_norm_g,
              gla_gate_up, gla_gate_bias, gla_out_norm_g, w_branch_att, w_branch_gla, w_out,
              g_ffn2, ffn2_w1, ffn2_w3, ffn2_w2):
    h = x
    c_act = jax.nn.silu(c)
    for l in range(DEPTH):
        mod = c_act @ w_mod[l] + b_mod[l]
        (sh1, sc1, gt1, sh2, sc2, gt2, sh3, sc3, gt3) = jnp.split(mod, N_MOD, axis=-1)
        u = modulate(rms_norm(h, g_ffn1[l]), sh1, sc1)
        h = h + 0.5 * (1 + gt1)[:, None, :] * swiglu(u, ffn1_w1[l], ffn1_w3[l], ffn1_w2[l])
        u = modulate(rms_norm(h, g_mix[l]), sh2, sc2)
        h = h + (1 + gt2)[:, None, :] * token_mixing(u, w_in[l], q_norm_g[l], k_norm_g[l], gla_gate_up[l],
                                                     gla_gate_bias[l], gla_out_norm_g[l], w_branch_att[l],
                                                     w_branch_gla[l], w_out[l])
        u = modulate(rms_norm(h, g_ffn2[l]), sh3, sc3)
        h = h + 0.5 * (1 + gt3)[:, None, :] * swiglu(u, ffn2_w1[l], ffn2_w3[l], ffn2_w2[l])
    return h
```

```python
import math
from contextlib import ExitStack
import numpy as np
import concourse.bass as bass
import concourse.mybir as mybir
from concourse.bass_utils import run_bass_kernel_spmd

F32 = mybir.dt.float32
BF16 = mybir.dt.bfloat16
AF = mybir.ActivationFunctionType
ALU = mybir.AluOpType

P = 128
S = 2048
D = 1024
DC = 8
NTS = 4
TS = 512
FF = 2816
FC = 22
NB = 2
EPS = 1e-6
ATT_GROUPS = ((128, 1), (512, 4), (2048, 16))
OFF_AQ, OFF_AK, OFF_AV = 0, 768, 1536
OFF_GQ, OFF_GK, OFF_GV, OFF_GR, OFF_GD, OFF_GA, OFF_GG = 2304, 2816, 3328, 4352, 5376, 5392, 6416
IN_W = 7440
SLOPES = [2.0 ** (-8.0 * (i + 1) / 12.0) for i in range(12)]
C_ID, C_STEP, C_CAUS, C_SCAN, C_BD = 0, 128, 384, 512, 1024
C_TOT = 1152


class Prog:
    ENG = ("pe", "act", "dve", "pool", "sp")

    def __init__(self):
        self.ins = {e: [] for e in self.ENG}
        self.lastw = {}
        self.readers = {}
        self.waited = {e: {} for e in self.ENG}
        self.dcount = {}
        self.group_slots = set()

    def _deps(self, eng, reads, writes):
        toks = []
        for k in reads:
            t = self.lastw.get(k)
            if t is not None:
                toks.append(t)
        for k in writes:
            t = self.lastw.get(k)
            if t is not None:
                toks.append(t)
            toks.extend(self.readers.get(k, ()))
        need = {}
        for t in toks:
            src = (t[0], t[1])
            if t[0] == "e" and t[1] == eng and eng in ("pe", "sp"):
                continue
            if need.get(src, -1) < t[2]:
                need[src] = t[2]
        waits = []
        wd = self.waited[eng]
        for src, idx in need.items():
            if wd.get(src, -1) >= idx:
                continue
            wd[src] = idx
            if src[0] == "e":
                self.ins[src[1]][idx]["sig"] = True
            waits.append((src, idx))
        return waits

    def _commit(self, tok, reads, writes):
        for k in reads:
            self.readers.setdefault(k, []).append(tok)
        for k in writes:
            self.lastw[k] = tok
            self.readers[k] = []

    def op(self, eng, fn, reads=(), writes=()):
        waits = self._deps(eng, reads, writes)
        idx = len(self.ins[eng])
        self.ins[eng].append(dict(fn=fn, waits=waits, sig=False, dma=None))
        self._commit(("e", eng, idx), reads, writes)

    def dma(self, fn, slot, reads=(), writes=(), group=False, queue="sp"):
        waits = self._deps(queue, reads, writes)
        self.dcount[slot] = self.dcount.get(slot, 0) + 1
        if group:
            self.group_slots.add(slot)
            waits = [w for w in waits if not (w[0][0] == "d" and w[0][1] == slot)]
        self.ins[queue].append(dict(fn=fn, waits=waits, sig=False, dma=slot))
        self._commit(("d", slot, self.dcount[slot]), reads, writes)

    def transfer(self, old_keys, new_keys):
        toks = []
        for k in old_keys:
            t = self.lastw.get(k)
            if t is not None:
                toks.append(t)
            toks.extend(self.readers.get(k, ()))
        best = {}
        for t in toks:
            s = (t[0], t[1])
            if best.get(s, -1) < t[2]:
                best[s] = t[2]
        toks = [(s[0], s[1], i) for s, i in best.items()]
        for k in new_keys:
            self.lastw[k] = None
            self.readers[k] = list(toks)

    def emit(self, nc, block, sems, dsems, final_waits):
        signo = {}
        for e in self.ENG:
            c = 0
            arr = []
            for it in self.ins[e]:
                if it["sig"]:
                    c += 1
                arr.append(c)
            signo[e] = arr

        def run(e, eng):
            for it in self.ins[e]:
                for src, idx in it["waits"]:
                    if src[0] == "e":
                        eng.wait_ge(sems[src[1]], signo[src[1]][idx])
                    else:
                        cnt = self.dcount[src[1]] if src[1] in self.group_slots else idx
                        eng.wait_ge(dsems[src[1]], 16 * cnt)
                r = getattr(eng, it["fn"][0])(**it["fn"][1])
                if it["dma"] is not None:
                    r.then_inc(dsems[it["dma"]], 16)
                elif it["sig"]:
                    r.then_inc(sems[e], 1)
            if e == "sp":
                for slot in final_waits:
                    if self.dcount.get(slot, 0):
                        eng.wait_ge(dsems[slot], 16 * self.dcount[slot])

        @block.tensor
        def _(eng):
            run("pe", eng)

        @block.scalar
        def _(eng):
            run("act", eng)

        @block.vector
        def _(eng):
            run("dve", eng)

        @block.gpsimd
        def _(eng):
            run("pool", eng)

        @block.sync
        def _(eng):
            run("sp", eng)


def build_nc(upto=99, dbg=False, nbr=NB):
    nc = bass.Bass("TRN2", target_bir_lowering=False)

    def din(name, shape):
        return nc.dram_tensor(name, list(shape), F32, kind="ExternalInput").ap()

    x = din("x", [NB, S, D])
    c = din("c", [NB, D])
    w_mod = din("w_mod", [D, 9 * D])
    b_mod = din("b_mod", [9 * D])
    g_l = [din("g_ffn1", [D]), din("g_mix", [D]), din("g_ffn2", [D])]
    ffn_w = [(din("ffn1_w1", [D, FF]), din("ffn1_w3", [D, FF]), din("ffn1_w2", [FF, D])),
             (din("ffn2_w1", [D, FF]), din("ffn2_w3", [D, FF]), din("ffn2_w2", [FF, D]))]
    w_in = din("w_in", [D, IN_W])
    q_norm_g = din("q_norm_g", [64])
    k_norm_g = din("k_norm_g", [64])
    gla_gate_up = din("gla_gate_up", [16, 512])
    gla_gate_bias = din("gla_gate_bias", [512])
    gla_out_norm_g = din("gla_out_norm_g", [256])
    w_branch_att = din("w_branch_att", [256, D])
    w_branch_gla = din("w_branch_gla", [D, D])
    w_out = din("w_out", [D, D])
    consts = din("consts", [P, C_TOT])
    y = nc.dram_tensor("y", [NB, S, D], F32, kind="ExternalOutput").ap()
    dbg_out = None
    if dbg:
        dbg_out = nc.dram_tensor("dbg", [4, P, DC * S], F32, kind="ExternalOutput").ap()

    pg = Prog()
    es = ExitStack()

    def sb(name, shape, dt):
        return es.enter_context(nc.sbuf_tensor(name, list(shape), dt))

    with es:
        h = sb("h", [P, DC, S], F32)
        u = sb("u", [P, DC, S], BF16)
        arena = sb("arena", [P, 28 * 1024], BF16)
        NST = 2
        stg = [sb(f"stg{i}", [P, 2048], F32) for i in range(NST)]
        NWB = 3
        wbs = [sb(f"wb{i}", [P, 2048], BF16) for i in range(NWB)]
        cst = sb("cst", [P, C_TOT], F32)
        NT32 = 4
        t32 = [sb(f"t32_{i}", [P, TS], F32) for i in range(NT32)]
        NT16 = 4
        t16 = [sb(f"t16_{i}", [P, TS], BF16) for i in range(NT16)]
        ident_bf = sb("ident_bf", [P, P], BF16)
        ones_bf = sb("ones_bf", [P, P], BF16)
        bd_bf = sb("bd_bf", [P, P], BF16)
        kst = sb("kst", [P, 8], F32)
        cT = sb("cT", [P, DC, NB], F32)
        cact = sb("cact", [P, DC, NB], BF16)
        bmodT = sb("bmodT", [P, 72], F32)
        modT = sb("modT", [P, 72, NB], F32)
        gT = sb("gT", [P, 3, DC], F32)
        Amod = sb("Amod", [P, 3, DC, NB], F32)
        Gmod = sb("Gmod", [P, 3, DC, NB], F32)
        qg = sb("qg", [P, 1], F32)
        kg = sb("kg", [P, 1], F32)
        negb = sb("negb", [P, 4], F32)
        gno = sb("gno", [P, 2], F32)
        gup = sb("gup", [16, 512], BF16)
        gdT = sb("gdT", [16, S], BF16)
        Sst = sb("Sst", [P, 256], F32)
        Sbf = sb("Sbf", [P, 256], BF16)
        ebl = sb("ebl", [P, 16], F32)
        kdT = sb("kdT", [P, 2, P], BF16)
        kdtm = sb("kdtm", [P, 2, P], BF16)
        amk = sb("amk", [P, 2, P], BF16)
        psum = es.enter_context(nc.psum_tensor("ps", [P, 8 * TS], F32))
        psum16 = psum.bitcast(BF16) if hasattr(psum, "bitcast") else None

        sems = {e: es.enter_context(nc.semaphore(f"s_{e}")) for e in ("pe", "act", "dve", "pool")}
        dslots = ["pro", "stg0", "stg1", "dbg"]
        dsems = {s_: es.enter_context(nc.semaphore(f"d_{s_}")) for s_ in dslots}
        block = es.enter_context(nc.Block())

        cnt = dict(bank=0, t32=0, tL=0, t16=0, stg=0, wb=0, alt=0)

        def bank():
            i = cnt["bank"] % 8
            cnt["bank"] += 1
            return psum[:, i * TS:(i + 1) * TS], ("ps", i), i

        def tmp32():
            i = cnt["t32"] % 2
            cnt["t32"] += 1
            return t32[i], ("t32", i)

        def tmpL():
            i = 2 + cnt["tL"] % 2
            cnt["tL"] += 1
            return t32[i], ("t32", i)

        def tmp16():
            i = cnt["t16"] % NT16
            cnt["t16"] += 1
            return t16[i], ("t16", i)

        def stage():
            i = cnt["stg"] % NST
            cnt["stg"] += 1
            return stg[i], ("stg", i), f"stg{i}"

        def alt():
            cnt["alt"] += 1
            return "act" if cnt["alt"] % 2 else "dve"

        def copy_op(eng, out, in_, reads, writes):
            if eng == "act":
                pg.op("act", ("copy", dict(out=out, in_=in_)), reads, writes)
            else:
                pg.op(eng, ("tensor_copy", dict(out=out, in_=in_)), reads, writes)

        def load_w(src, a, b_):
            st, skey, sslot = stage()
            n = a * b_
            stv = st[:, 0:n].rearrange("p (a b) -> p a b", a=a)
            pg.dma(("dma_start", dict(out=stv, in_=src)), sslot, writes=[skey])
            i = cnt["wb"] % NWB
            cnt["wb"] += 1
            wv = wbs[i][:, 0:n].rearrange("p (a b) -> p a b", a=a)
            pg.op("pool", ("tensor_copy", dict(out=wv, in_=stv)), [skey], [("wb", i)])
            return wv, ("wb", i)

        def kc_view(w, c0, ncol):
            return w.rearrange("(kc p) f -> p kc f", p=P)[:, :, c0:c0 + ncol]

        def mm(out, lhsT, rhs, start, stop, reads, writes):
            pg.op("pe", ("matmul", dict(out=out, lhsT=lhsT, rhs=rhs, start=start, stop=stop)),
                  reads, writes)

        ukeys = [("u", t) for t in range(NTS)]

        def pro(out, in_, key, slow=False):
            if slow:
                pg.dma(("dma_start", dict(out=out, in_=in_, allow_slow_non_contiguous=True)), "pro",
                       writes=[key], group=True)
            else:
                pg.dma(("dma_start", dict(out=out, in_=in_)), "pro", writes=[key], group=True)

        pro(cst[:], consts, "cst")
        for b in range(NB):
            pro(cT[:, :, b], c[b].rearrange("(kc p) -> p kc", p=P), ("cT", b), slow=True)
        pro(bmodT[:], b_mod.rearrange("(n p) -> p n", p=P), "bmodT", slow=True)
        for l in range(3):
            pro(gT[:, l, :], g_l[l].rearrange("(n p) -> p n", p=P), ("gT", l), slow=True)
        for hh in range(2):
            pro(qg[hh * 64:(hh + 1) * 64, :], q_norm_g.rearrange("(p o) -> p o", o=1), ("qg", hh), slow=True)
            pro(kg[hh * 64:(hh + 1) * 64, :], k_norm_g.rearrange("(p o) -> p o", o=1), ("kg", hh), slow=True)
        pro(negb[:], gla_gate_bias.rearrange("(n p) -> p n", p=P), "negb", slow=True)
        pro(gno[:], gla_out_norm_g.rearrange("(n p) -> p n", p=P), "gno", slow=True)
        pro(t32[0][0:16, :], gla_gate_up, ("t32", 0))

        pg.op("dve", ("memset", dict(ap=ones_bf[:], constant=1.0)), [], ["ones"])
        pg.op("dve", ("memset", dict(ap=kst[:, 0:1], constant=EPS)), [], ["kst0"])
        pg.op("dve", ("memset", dict(ap=kst[:, 1:2], constant=1.0)), [], ["kst1"])
        pg.op("dve", ("memset", dict(ap=kst[:, 2:3], constant=math.log(128.0 ** -0.5))), [], ["kst2"])
        pg.op("dve", ("tensor_copy", dict(out=ident_bf[:], in_=cst[:, C_ID:C_ID + P])), ["cst"], ["identbf"])
        pg.op("dve", ("tensor_copy", dict(out=bd_bf[:], in_=cst[:, C_BD:C_BD + P])), ["cst"], ["bdbf"])
        pg.op("dve", ("tensor_copy", dict(out=gup[:], in_=t32[0][0:16, :])), [("t32", 0)], ["gup"])
        pg.op("dve", ("tensor_scalar", dict(out=negb[:], in0=negb[:], scalar1=-1.0, scalar2=None, op0=ALU.mult)),
              ["negb"], ["negb"])
        pg.op("dve", ("tensor_scalar", dict(out=qg[:], in0=qg[:], scalar1=0.125, scalar2=None, op0=ALU.mult)),
              [("qg", 0), ("qg", 1)], [("qg", 0), ("qg", 1)])
        pg.op("act", ("activation", dict(out=cact[:], in_=cT[:], func=AF.Silu)), [("cT", 0), ("cT", 1)], ["cact"])
        ident = cst[:, C_ID:C_ID + P]
        stepsM = cst[:, C_STEP:C_STEP + 256]
        causM = cst[:, C_CAUS:C_CAUS + P]
        scanM = cst[:, C_SCAN:C_SCAN + TS]

        pm, pmk, _ = bank()
        for t in range(36):
            wv, wk = load_w(kc_view(w_mod, t * 256, 256), DC, 256)
            for j in range(2):
                n = t * 2 + j
                for kc in range(DC):
                    mm(pm[:, n * 2:n * 2 + 2], wv[:, kc, j * P:(j + 1) * P], cact[:, kc, :],
                       kc == 0, kc == DC - 1, [wk, "cact"], [pmk])
        pmv = pm[:, 0:144].rearrange("p (n b) -> p n b", b=NB)
        for b in range(NB):
            pg.op("dve", ("tensor_tensor", dict(out=modT[:, :, b], in0=pmv[:, :, b], in1=bmodT[:],
                                                         op=ALU.add)), [pmk, "bmodT"], [("modT", b)])
        for l in range(3):
            coef = 1.0 if l == 1 else 0.5
            for b in range(NB):
                pg.op("dve", ("scalar_tensor_tensor", dict(
                    out=Amod[:, l, :, b], in0=modT[:, l * 24 + 8:l * 24 + 16, b], scalar=1.0, in1=gT[:, l, :],
                    op0=ALU.add, op1=ALU.mult)), [("modT", b), ("gT", l)], [("Amod", l, b)])
                pg.op("dve", ("tensor_scalar", dict(
                    out=Gmod[:, l, :, b], in0=modT[:, l * 24 + 16:l * 24 + 24, b], scalar1=1.0, scalar2=coef,
                    op0=ALU.add, op1=ALU.mult)), [("modT", b)], [("Gmod", l, b)])

        def load_x(b):
            for tt in range(16):
                st, skey, sslot = stage()
                xs = st[:, 0:D]
                pg.dma(("dma_start", dict(out=xs, in_=x[b, tt * P:(tt + 1) * P, :])), sslot,
                       writes=[skey])
                for half in range(2):
                    pb, pk, _ = bank()
                    for j in range(4):
                        dc = half * 4 + j
                        pg.op("pe", ("transpose", dict(
                            out=pb[:, j * P:(j + 1) * P], in_=xs[:, dc * P:(dc + 1) * P], identity=ident)),
                            [skey, "cst"], [pk])
                    copy_op(alt(), h[:, half * 4:half * 4 + 4, tt * P:(tt + 1) * P],
                            pb.rearrange("p (a t) -> p a t", a=4), [pk], [("h", tt // 4)])

        def store_y(b):
            for tt in range(16):
                st, skey, sslot = stage()
                ys = st[:, 0:D]
                for half in range(2):
                    pb, pk, _ = bank()
                    for j in range(4):
                        dc = half * 4 + j
                        pg.op("pe", ("transpose", dict(
                            out=pb[:, j * P:(j + 1) * P], in_=h[:, dc, tt * P:(tt + 1) * P], identity=ident)),
                            [("h", tt // 4), "cst"], [pk])
                    copy_op(alt(), ys[:, half * 512:(half + 1) * 512], pb, [pk], [skey])
                pg.dma(("dma_start", dict(out=y[b, tt * P:(tt + 1) * P, :], in_=ys)), sslot,
                       reads=[skey])

        def rstd_from(pss, pssk, scale):
            r32, rk = tmpL()
            pg.op("act", ("activation", dict(out=r32[:], in_=pss, func=AF.Ln, bias=kst[:, 0:1], scale=scale)),
                  [pssk, "kst0"], [rk])
            pg.op("act", ("activation", dict(out=r32[:], in_=r32[:], func=AF.Exp, scale=-0.5)), [rk], [rk])
            return r32, rk

        def norm_mod(l, b):
            for ts in range(NTS):
                sl = slice(ts * TS, (ts + 1) * TS)
                pss, pssk, _ = bank()
                for dc in range(DC):
                    sq, sqk = tmp16()
                    pg.op("dve", ("tensor_tensor", dict(out=sq[:], in0=h[:, dc, sl], in1=h[:, dc, sl],
                                                                          op=ALU.mult)), [("h", ts)], [sqk])
                    mm(pss, ones_bf[:], sq[:], dc == 0, dc == DC - 1, [sqk, "ones"], [pssk])
                r32, rk = rstd_from(pss, pssk, 1.0 / D)
                for dc in range(DC):
                    tt_, tk = tmp32()
                    pg.op("dve", ("tensor_tensor", dict(out=tt_[:], in0=h[:, dc, sl], in1=r32[:],
                                                                            op=ALU.mult)), [("h", ts), rk], [tk])
                    pg.op("act", ("activation", dict(
                        out=u[:, dc, sl], in_=tt_[:], func=AF.Identity, bias=modT[:, l * 24 + dc, b:b + 1],
                        scale=Amod[:, l, dc, b:b + 1])), [tk, ("modT", b), ("Amod", l, b)], [("u", ts)])

        def gkey(f, ts):
            return ("g", f, ts)

        def ffn(fi, l, b):
            w1, w3, w2 = ffn_w[fi]
            gbuf = arena[:, 0:12 * S].rearrange("p (f t) -> p f t", f=12)
            for (f0, nf) in ((0, 12), (12, 10)):
                for tl in range(nf // 2):
                    c0 = (f0 + tl * 2) * P
                    w1v, w1k = load_w(kc_view(w1, c0, 256), DC, 256)
                    w3v, w3k = load_w(kc_view(w3, c0, 256), DC, 256)
                    for j in range(2):
                        fl = tl * 2 + j
                        for ts in range(NTS):
                            sl = slice(ts * TS, (ts + 1) * TS)
                            pa, pak, _ = bank()
                            pb, pbk, _ = bank()
                            for kc in range(DC):
                                mm(pa, w1v[:, kc, j * P:(j + 1) * P], u[:, kc, sl], kc == 0, kc == DC - 1,
                                   [w1k, ("u", ts)], [pak])
                            for kc in range(DC):
                                mm(pb, w3v[:, kc, j * P:(j + 1) * P], u[:, kc, sl], kc == 0, kc == DC - 1,
                                   [w3k, ("u", ts)], [pbk])
                            s1, s1k = tmp16()
                            pg.op("act", ("activation", dict(out=s1[:], in_=pa, func=AF.Silu)),
                                  [pak], [s1k])
                            pg.op("dve", ("tensor_tensor", dict(
                                out=gbuf[:, fl, sl], in0=pb, in1=s1[:], op=ALU.mult)), [pbk, s1k], [gkey(fl, ts)])
                w2v_all = w2.rearrange("(fc p) d -> p fc d", p=P)
                for dc in range(DC):
                    w2v, w2k = load_w(w2v_all[:, f0:f0 + nf, dc * P:(dc + 1) * P], nf, P)
                    for ts in range(NTS):
                        sl = slice(ts * TS, (ts + 1) * TS)
                        po, pok, _ = bank()
                        for fl in range(nf):
                            mm(po, w2v[:, fl, :], gbuf[:, fl, sl], fl == 0, fl == nf - 1, [w2k, gkey(fl, ts)], [pok])
                        pg.op("dve", ("scalar_tensor_tensor", dict(
                            out=h[:, dc, sl], in0=po, scalar=Gmod[:, l, dc, b:b + 1], in1=h[:, dc, sl],
                            op0=ALU.mult, op1=ALU.add)), [pok, ("Gmod", l, b), ("h", ts)], [("h", ts)])

        G_KEYS = [gkey(f, t) for f in range(12) for t in range(NTS)]

        KB = 512
        acc = arena[:, 0:32 * KB].bitcast(F32).rearrange("p (c o t) -> p c o t", c=2, o=2)
        ogla = arena[:, 0:32 * KB].rearrange("p (c t) -> p c t", c=8)
        qbuf = arena[:, 32 * KB:40 * KB].rearrange("p (c t) -> p c t", c=2)
        kbuf = arena[:, 40 * KB:48 * KB].rearrange("p (c t) -> p c t", c=2)
        vtm = arena[:, 48 * KB:56 * KB].rearrange("p (b f) -> p b f", b=16)
        oatt = arena[:, 32 * KB:40 * KB].rearrange("p (c t) -> p c t", c=2)
        gvtm = arena[:, 40 * KB:48 * KB].rearrange("p (b f) -> p b f", b=16)
        qdec = arena[:, 48 * KB:52 * KB]
        kinv = arena[:, 52 * KB:56 * KB]
        merged = arena[:, 40 * KB:56 * KB].rearrange("p (c t) -> p c t", c=8)

        ACC_KEYS = [("acc", ch, t) for ch in range(2) for t in range(NTS)]
        Q_KEYS = [("q", ch, t) for ch in range(2) for t in range(NTS)]
        K_KEYS = [("k", ch, t) for ch in range(2) for t in range(NTS)]
        V_KEYS = [("v", tb) for tb in range(16)]
        OATT_KEYS = [("oatt", t) for t in range(NTS)]
        OGLA_KEYS = [("ogla", hh, t) for hh in range(4) for t in range(NTS)]
        GV_KEYS = [("gv", tb) for tb in range(16)]
        QD_KEYS = [("qd", t) for t in range(NTS)]
        KI_KEYS = [("ki", t) for t in range(NTS)]
        KD_KEYS = []
        MG_KEYS = [("mg", t) for t in range(2)]

        def tok_slice(dil, r, n):
            st0 = r + dil * P * n
            return slice(st0, st0 + dil * (P - 1) + 1, dil)

        def blk_ts(dil, r, n):
            if dil == 1:
                return [n // 4]
            return list(range(NTS)) if dil == 16 else [n]

        def attention(b):
            for gi, (win, dil) in enumerate(ATT_GROUPS):
                nb = S // dil // P
                for which, off, buf, gain, nm in ((0, OFF_AQ, qbuf, qg, "q"), (1, OFF_AK, kbuf, kg, "k")):
                    wv, wk = load_w(kc_view(w_in, off + gi * 256, 256), DC, 256)
                    for ch in range(2):
                        for ts in range(NTS):
                            sl = slice(ts * TS, (ts + 1) * TS)
                            pq, pqk, _ = bank()
                            for kc in range(DC):
                                mm(pq, wv[:, kc, ch * P:(ch + 1) * P], u[:, kc, sl], kc == 0, kc == DC - 1,
                                   [wk, ("u", ts)], [pqk])
                            sq, sqk = tmp16()
                            pg.op("act", ("activation", dict(out=sq[:], in_=pq, func=AF.Square)),
                                  [pqk], [sqk])
                            pss, pssk, _ = bank()
                            mm(pss, bd_bf[:], sq[:], True, True, [sqk, "bdbf"], [pssk])
                            r32, rk = rstd_from(pss, pssk, 1.0 / 64)
                            pg.op("dve", ("scalar_tensor_tensor", dict(out=buf[:, ch, sl], in0=pq, scalar=gain[:, 0:1], in1=r32[:],
                                                         op0=ALU.mult, op1=ALU.mult)),
                                  [pqk, rk, (nm + "g", 0), (nm + "g", 1)], [(nm, ch, ts)])
                wv, wk = load_w(kc_view(w_in, OFF_AV + gi * 256, 256), DC, 256)
                blocks = [(r, n) for r in range(dil) for n in range(nb)]
                for tb, (r, n) in enumerate(blocks):
                    tsl = tok_slice(dil, r, n)
                    pv, pvk, _ = bank()
                    for kc in range(DC):
                        mm(pv[:, 0:256], u[:, kc, tsl], wv[:, kc, :], kc == 0, kc == DC - 1,
                           [wk] + [("u", t) for t in blk_ts(dil, r, n)], [pvk])
                    copy_op(alt(), vtm[:, tb, :], pv[:, 0:256], [pvk], [("v", tb)])
                work = [(ch, tb) for ch in range(2) for tb in range(len(blocks))]
                pend = {}

                def emit_qk(ch, tb):
                    r, n = blocks[tb]
                    tsl = tok_slice(dil, r, n)
                    tss = blk_ts(dil, r, n)
                    Es = []
                    for hh in range(2):
                        head = gi * 4 + ch * 2 + hh
                        cs = -SLOPES[head] * dil
                        ps_, psk, _ = bank()
                        prt = slice(hh * 64, (hh + 1) * 64)
                        rd = [("q", ch, t) for t in tss] + [("k", ch, t) for t in tss]
                        mm(ps_[:, 128:256], kbuf[prt, ch, tsl], qbuf[prt, ch, tsl], True, True, rd, [psk])
                        lo = 128
                        if n > 0:
                            psl = tok_slice(dil, r, n - 1)
                            rd2 = rd + [("k", ch, t) for t in blk_ts(dil, r, n - 1)]
                            mm(ps_[:, 0:128], kbuf[prt, ch, psl], qbuf[prt, ch, tsl], True, True, rd2, [psk])
                            lo = 0
                        t_, tk = tmp32()
                        pg.op("dve", ("scalar_tensor_tensor", dict(
                            out=t_[:, lo:256], in0=stepsM[:, lo:256], scalar=cs, in1=ps_[:, lo:256],
                            op0=ALU.mult, op1=ALU.add)), [psk, "cst"], [tk])
                        E, Ek = tmp16()
                        pg.op("act", ("activation", dict(out=E[:, lo:256], in_=t_[:, lo:256],
                                                                               func=AF.Exp)), [tk], [Ek])
                        Es.append((E, Ek, lo))
                    pend[(ch, tb)] = Es

                def emit_pv(ch, tb):
                    r, n = blocks[tb]
                    tsl = tok_slice(dil, r, n)
                    tss = blk_ts(dil, r, n)
                    Es = pend.pop((ch, tb))
                    po, pok, _ = bank()
                    for hh in range(2):
                        E, Ek, lo = Es[hh]
                        prt = slice(hh * 64, (hh + 1) * 64)
                        vc = slice(ch * P + hh * 64, ch * P + hh * 64 + 64)
                        mm(po[prt, 0:128], vtm[:, tb, vc], E[:, 128:256], True, n == 0, [Ek, ("v", tb)], [pok])
                        if n > 0:
                            mm(po[prt, 0:128], vtm[:, tb - 1, vc], E[:, 0:128], False, True, [Ek, ("v", tb - 1)], [pok])
                        mm(po[prt, 128:256], ones_bf[:, 0:64], E[:, 128:256], True, n == 0, [Ek, "ones"], [pok])
                        if n > 0:
                            mm(po[prt, 128:256], ones_bf[:, 0:64], E[:, 0:128], False, True, [Ek, "ones"], [pok])
                    pov = po[:, 0:256].rearrange("p (o t) -> p o t", o=2)
                    akeys = [("acc", ch, t) for t in tss]
                    if gi == 0:
                        pg.op("dve", ("tensor_copy", dict(out=acc[:, ch, :, tsl], in_=pov)), [pok], akeys)
                    else:
                        pg.op("dve", ("tensor_tensor", dict(out=acc[:, ch, :, tsl], in0=acc[:, ch, :, tsl], in1=pov,
                                                               op=ALU.add)), [pok] + akeys, akeys)

                emit_qk(*work[0])
                for i in range(len(work)):
                    if i + 1 < len(work):
                        emit_qk(*work[i + 1])
                    emit_pv(*work[i])
            pg.transfer(Q_KEYS, OATT_KEYS)
            for ch in range(2):
                for ts in range(NTS):
                    sl = slice(ts * TS, (ts + 1) * TS)
                    r_, rk = tmp32()
                    pg.op("dve", ("reciprocal", dict(out=r_[:], in_=acc[:, ch, 1, sl])),
                          [("acc", ch, ts)], [rk])
                    pg.op("dve", ("tensor_tensor", dict(out=oatt[:, ch, sl], in0=acc[:, ch, 0, sl],
                                                                              in1=r_[:], op=ALU.mult)),
                          [("acc", ch, ts), rk], [("oatt", ts)])

        def gla(b):
            wv, wk = load_w(kc_view(w_in, OFF_GD, 16), DC, 16)
            for ts in range(NTS):
                sl = slice(ts * TS, (ts + 1) * TS)
                pd, pdk, _ = bank()
                for kc in range(DC):
                    mm(pd[0:16, :], wv[:, kc, :], u[:, kc, sl], kc == 0, kc == DC - 1, [wk, ("u", ts)], [pdk])
                copy_op("act", gdT[:, sl], pd[0:16, :], [pdk], [("gd", ts)])
            for hh in range(4):
                wq, wqk = load_w(kc_view(w_in, OFF_GQ + hh * P, P), DC, P)
                wkk, wkkk = load_w(kc_view(w_in, OFF_GK + hh * P, P), DC, P)
                for ts in range(NTS):
                    sl = slice(ts * TS, (ts + 1) * TS)
                    px, pxk, _ = bank()
                    mm(px, gup[:, hh * P:(hh + 1) * P], gdT[:, sl], True, True, ["gup", ("gd", ts)], [pxk])
                    e_, ek = tmp32()
                    pg.op("act", ("activation", dict(out=e_[:], in_=px, func=AF.Exp,
                                                                             bias=negb[:, hh:hh + 1], scale=-1.0)),
                          [pxk, "negb"], [ek])
                    sp_, spk = tmp32()
                    pg.op("act", ("activation", dict(out=sp_[:], in_=e_[:], func=AF.Ln,
                                                                        bias=kst[:, 1:2], scale=1.0)), [ek, "kst1"], [spk])
                    B_, Bk = tmpL()
                    pg.op("dve", ("tensor_tensor_scan", dict(
                        out=B_[:], data0=scanM, data1=sp_[:], initial=0.0, op0=ALU.mult, op1=ALU.add)),
                        [spk, "cst"], [Bk])
                    pg.op("act", ("activation", dict(
                        out=ebl[:, ts * 4:ts * 4 + 4], in_=B_[:, 127:512:128], func=AF.Exp, scale=-1.0 / 16)),
                        [Bk], [("ebl", ts)])
                    eb, ebk = tmp32()
                    pg.op("act", ("activation", dict(out=eb[:], in_=B_[:], func=AF.Exp,
                                                                      bias=kst[:, 2:3], scale=-1.0 / 16)), [Bk, "kst2"], [ebk])
                    pq, pqk, _ = bank()
                    for kc in range(DC):
                        mm(pq, wq[:, kc, :], u[:, kc, sl], kc == 0, kc == DC - 1, [wqk, ("u", ts)], [pqk])
                    pg.op("dve", ("tensor_tensor", dict(out=qdec[:, sl], in0=pq, in1=eb[:],
                                                                                op=ALU.mult)), [pqk, ebk], [("qd", ts)])
                    en, enk = tmp32()
                    pg.op("act", ("activation", dict(out=en[:], in_=B_[:], func=AF.Exp, scale=1.0 / 16)),
                          [Bk], [enk])
                    pk_, pkk, _ = bank()
                    for kc in range(DC):
                        mm(pk_, wkk[:, kc, :], u[:, kc, sl], kc == 0, kc == DC - 1, [wkkk, ("u", ts)], [pkk])
                    pg.op("dve", ("tensor_tensor", dict(out=kinv[:, sl], in0=pk_, in1=en[:],
                                                                                  op=ALU.mult)), [pkk, enk], [("ki", ts)])
                wv, wk = load_w(kc_view(w_in, OFF_GV + hh * 256, 256), DC, 256)
                for tb in range(16):
                    pv, pvk, _ = bank()
                    for kc in range(DC):
                        mm(pv[:, 0:256], u[:, kc, tb * P:(tb + 1) * P], wv[:, kc, :], kc == 0, kc == DC - 1,
                           [wk, ("u", tb // 4)], [pvk])
                    copy_op(alt(), gvtm[:, tb, :], pv[:, 0:256], [pvk], [("gv", tb)])
                def emit_kd(cc):
                    csl = slice(cc * P, (cc + 1) * P)
                    j = cc % 2
                    pg.op("dve", ("tensor_scalar", dict(
                        out=kdT[:, j, :], in0=kinv[:, csl], scalar1=ebl[:, cc:cc + 1], scalar2=None, op0=ALU.mult)),
                        [("ki", cc // 4), ("ebl", cc // 4)], [("kdT", j)])
                    pt, ptk, bi = bank()
                    pt16 = psum16[:, bi * 2 * TS:bi * 2 * TS + P]
                    pg.op("pe", ("transpose", dict(out=pt16, in_=kdT[:, j, :], identity=ident_bf[:])),
                          [("kdT", j), "identbf"], [ptk])
                    copy_op("act", kdtm[:, j, :], pt16, [ptk], [("kd", j)])
                emit_kd(0)
                for cc in range(16):
                    csl = slice(cc * P, (cc + 1) * P)
                    ts = cc // 4
                    pa, pak, _ = bank()
                    mm(pa[:, 0:P], kinv[:, csl], qdec[:, csl], True, True, [("ki", ts), ("qd", ts)], [pak])
                    j = cc % 2
                    if cc + 1 < 15:
                        emit_kd(cc + 1)
                    pg.op("dve", ("tensor_tensor", dict(out=amk[:, j, :], in0=pa[:, 0:P], in1=causM,
                                                                       op=ALU.mult)), [pak, "cst"], [("amk", j)])
                    po, pok, _ = bank()
                    for dv in range(2):
                        mm(po[:, dv * P:(dv + 1) * P], gvtm[:, cc, dv * P:(dv + 1) * P], amk[:, j, :], True, cc == 0,
                           [("gv", cc), ("amk", j)], [pok])
                        if cc > 0:
                            mm(po[:, dv * P:(dv + 1) * P], Sbf[:, dv * P:(dv + 1) * P], qdec[:, csl], False, True,
                               ["Sbf", ("qd", ts)], [pok])
                    copy_op("act", ogla[:, hh * 2:hh * 2 + 2, csl], po[:, 0:256].rearrange("p (a t) -> p a t", a=2),
                            [pok], [("ogla", hh, ts)])
                    if cc < 15:
                        pu, puk, _ = bank()
                        mm(pu[:, 0:256], kdtm[:, j, :], gvtm[:, cc, :], True, True, [("kd", j), ("gv", cc)], [puk])
                        if cc == 0:
                            pg.op("dve", ("tensor_copy", dict(out=Sst[:], in_=pu[:, 0:256])), [puk], ["Sst"])
                        else:
                            pg.op("dve", ("scalar_tensor_tensor", dict(
                                out=Sst[:], in0=Sst[:], scalar=ebl[:, cc:cc + 1], in1=pu[:, 0:256],
                                op0=ALU.mult, op1=ALU.add)), [puk, "Sst", ("ebl", ts)], ["Sst"])
                        copy_op("act", Sbf[:], Sst[:], ["Sst"], ["Sbf"])
                wv, wk = load_w(kc_view(w_in, OFF_GR + hh * 256, 256), DC, 256)
                for ts in range(NTS):
                    sl = slice(ts * TS, (ts + 1) * TS)
                    pss, pssk, _ = bank()
                    for dv in range(2):
                        sq, sqk = tmp16()
                        pg.op("dve", ("tensor_tensor", dict(
                            out=sq[:], in0=ogla[:, hh * 2 + dv, sl], in1=ogla[:, hh * 2 + dv, sl], op=ALU.mult)),
                            [("ogla", hh, ts)], [sqk])
                        mm(pss, ones_bf[:], sq[:], dv == 0, dv == 1, [sqk, "ones"], [pssk])
                    r32, rk = rstd_from(pss, pssk, 1.0 / 256)
                    for dv in range(2):
                        pr, prk, _ = bank()
                        for kc in range(DC):
                            mm(pr, wv[:, kc, dv * P:(dv + 1) * P], u[:, kc, sl], kc == 0, kc == DC - 1,
                               [wk, ("u", ts)], [prk])
                        sg, sgk = tmp32()
                        pg.op("act", ("activation", dict(out=sg[:], in_=pr, func=AF.Silu)), [prk], [sgk])
                        t1, t1k = tmp32()
                        pg.op("dve", ("scalar_tensor_tensor", dict(
                            out=t1[:], in0=ogla[:, hh * 2 + dv, sl], scalar=gno[:, dv:dv + 1], in1=r32[:],
                            op0=ALU.mult, op1=ALU.mult)), [("ogla", hh, ts), rk, "gno"], [t1k])
                        pg.op("dve", ("tensor_tensor", dict(
                            out=ogla[:, hh * 2 + dv, sl], in0=t1[:], in1=sg[:], op=ALU.mult)),
                            [t1k, sgk, ("ogla", hh, ts)], [("ogla", hh, ts)])

        def merge_out(b):
            wba_all = w_branch_att.rearrange("(kc p) d -> p kc d", p=P)
            for th in range(2):
                for dp in range(4):
                    c0 = dp * 256
                    wga, wgak = load_w(kc_view(w_in, OFF_GA + c0, 256), DC, 256)
                    wba, wbak = load_w(wba_all[:, :, c0:c0 + 256], 2, 256)
                    for j in range(2):
                        dc = dp * 2 + j
                        for t2 in range(2):
                            ts = th * 2 + t2
                            sl = slice(ts * TS, (ts + 1) * TS)
                            p1, p1k, _ = bank()
                            for kc in range(DC):
                                mm(p1, wga[:, kc, j * P:(j + 1) * P], u[:, kc, sl], kc == 0, kc == DC - 1,
                                   [wgak, ("u", ts)], [p1k])
                            sa, sak = tmp32()
                            pg.op("act", ("activation", dict(out=sa[:], in_=p1, func=AF.Sigmoid)),
                                  [p1k], [sak])
                            p2, p2k, _ = bank()
                            for c2 in range(2):
                                mm(p2, wba[:, c2, j * P:(j + 1) * P], oatt[:, c2, sl], c2 == 0, c2 == 1,
                                   [wbak, ("oatt", ts)], [p2k])
                            pg.op("dve", ("tensor_tensor", dict(
                                out=merged[:, dc, t2 * TS:(t2 + 1) * TS], in0=p2, in1=sa[:], op=ALU.mult)),
                                [p2k, sak], [("mg", t2)])
                    wgg, wggk = load_w(kc_view(w_in, OFF_GG + c0, 256), DC, 256)
                    wbg, wbgk = load_w(kc_view(w_branch_gla, c0, 256), DC, 256)
                    for j in range(2):
                        dc = dp * 2 + j
                        for t2 in range(2):
                            ts = th * 2 + t2
                            sl = slice(ts * TS, (ts + 1) * TS)
                            p1, p1k, _ = bank()
                            for kc in range(DC):
                                mm(p1, wgg[:, kc, j * P:(j + 1) * P], u[:, kc, sl], kc == 0, kc == DC - 1,
                                   [wggk, ("u", ts)], [p1k])
                            sa, sak = tmp32()
                            pg.op("act", ("activation", dict(out=sa[:], in_=p1, func=AF.Sigmoid)),
                                  [p1k], [sak])
                            p2, p2k, _ = bank()
                            for kc in range(DC):
                                mm(p2, wbg[:, kc, j * P:(j + 1) * P], ogla[:, kc, sl], kc == 0, kc == DC - 1,
                                   [wbgk, ("ogla", kc // 2, ts)], [p2k])
                            m2, m2k = tmp32()
                            pg.op("dve", ("tensor_tensor", dict(out=m2[:], in0=p2, in1=sa[:],
                                                                                         op=ALU.mult)), [p2k, sak], [m2k])
                            pg.op("pool", ("tensor_tensor", dict(
                                out=merged[:, dc, t2 * TS:(t2 + 1) * TS], in0=merged[:, dc, t2 * TS:(t2 + 1) * TS],
                                in1=m2[:], op=ALU.add)), [m2k, ("mg", t2)], [("mg", t2)])
                for dp in range(4):
                    wo, wok = load_w(kc_view(w_out, dp * 256, 256), DC, 256)
                    for j in range(2):
                        dc = dp * 2 + j
                        for t2 in range(2):
                            ts = th * 2 + t2
                            sl = slice(ts * TS, (ts + 1) * TS)
                            po, pok, _ = bank()
                            for kc in range(DC):
                                mm(po, wo[:, kc, j * P:(j + 1) * P], merged[:, kc, t2 * TS:(t2 + 1) * TS], kc == 0,
                                   kc == DC - 1, [wok, ("mg", t2)], [pok])
                            pg.op("dve", ("scalar_tensor_tensor", dict(
                                out=h[:, dc, sl], in0=po, scalar=Gmod[:, 1, dc, b:b + 1], in1=h[:, dc, sl],
                                op0=ALU.mult, op1=ALU.add)), [pok, ("Gmod", 1, b), ("h", ts)], [("h", ts)])

        def dump(slot_i, b):
            if dbg and b == 0:
                pg.dma(("dma_start", dict(out=dbg_out[slot_i].rearrange("p (c t) -> p c t", c=DC), in_=h[:])), "dbg",
                       reads=[("h", t) for t in range(NTS)])

        MIX_A = ACC_KEYS + Q_KEYS + K_KEYS + V_KEYS
        for b in range(nbr):
            load_x(b)
            norm_mod(0, b)
            if upto >= 1:
                ffn(0, 0, b)
            dump(0, b)
            if upto >= 2:
                norm_mod(1, b)
                pg.transfer(G_KEYS, MIX_A + KD_KEYS)
                attention(b)
                pg.transfer(ACC_KEYS, OGLA_KEYS)
                pg.transfer(K_KEYS, GV_KEYS)
                pg.transfer(V_KEYS, QD_KEYS + KI_KEYS)
                gla(b)
                pg.transfer(GV_KEYS + QD_KEYS + KI_KEYS, MG_KEYS)
                merge_out(b)
                dump(1, b)
            if upto >= 3:
                norm_mod(2, b)
                pg.transfer(OGLA_KEYS + OATT_KEYS + MG_KEYS + KD_KEYS + MIX_A + GV_KEYS + QD_KEYS + KI_KEYS, G_KEYS)
                ffn(1, 2, b)
            elif upto >= 2:
                pg.transfer(OGLA_KEYS + OATT_KEYS + MG_KEYS + KD_KEYS + MIX_A + GV_KEYS + QD_KEYS + KI_KEYS, G_KEYS)
            store_y(b)

        pg.emit(nc, block, sems, dsems, final_waits=["stg0", "stg1", "dbg"])
    return nc


def make_consts():
    cs = np.zeros((P, C_TOT), np.float32)
    cs[:, C_ID:C_ID + P] = np.eye(P, dtype=np.float32)
    kk = np.arange(P)[:, None]
    qq = np.arange(P)[None, :]
    BIG = 1.0e4
    prev = np.where(qq <= kk, (qq + P - kk).astype(np.float32), BIG)
    cur = np.where(qq >= kk, (qq - kk).astype(np.float32), BIG)
    cs[:, C_STEP:C_STEP + P] = prev
    cs[:, C_STEP + P:C_STEP + 2 * P] = cur
    cs[:, C_CAUS:C_CAUS + P] = (kk <= qq).astype(np.float32)
    sc = np.ones((P, TS), np.float32)
    sc[:, 0::P] = 0.0
    cs[:, C_SCAN:C_SCAN + TS] = sc
    bd = np.zeros((P, P), np.float32)
    bd[:64, :64] = 1.0
    bd[64:, 64:] = 1.0
    cs[:, C_BD:C_BD + P] = bd
    return cs


_NC_CACHE = {}


def _run(inputs, upto=99, dbg=False, ncores=8):
    key = (upto, dbg)
    if key not in _NC_CACHE:
        _NC_CACHE[key] = build_nc(upto, dbg)
    nc = _NC_CACHE[key]
    f = lambda a: np.ascontiguousarray(np.asarray(a, dtype=np.float32))
    sq = lambda a: f(a)[0]
    shared = {
        "w_mod": sq(inputs["w_mod"]), "b_mod": sq(inputs["b_mod"]),
        "g_ffn1": sq(inputs["g_ffn1"]), "g_mix": sq(inputs["g_mix"]), "g_ffn2": sq(inputs["g_ffn2"]),
        "ffn1_w1": sq(inputs["ffn1_w1"]), "ffn1_w3": sq(inputs["ffn1_w3"]), "ffn1_w2": sq(inputs["ffn1_w2"]),
        "ffn2_w1": sq(inputs["ffn2_w1"]), "ffn2_w3": sq(inputs["ffn2_w3"]), "ffn2_w2": sq(inputs["ffn2_w2"]),
        "w_in": sq(inputs["w_in"]), "q_norm_g": sq(inputs["q_norm_g"]), "k_norm_g": sq(inputs["k_norm_g"]),
        "gla_gate_up": sq(inputs["gla_gate_up"]), "gla_gate_bias": sq(inputs["gla_gate_bias"]),
        "gla_out_norm_g": sq(inputs["gla_out_norm_g"]), "w_branch_att": sq(inputs["w_branch_att"]),
        "w_branch_gla": sq(inputs["w_branch_gla"]), "w_out": sq(inputs["w_out"]),
        "consts": make_consts(),
    }
    xf = f(inputs["x"])
    cf = f(inputs["c"])
    in_maps = []
    for i in range(ncores):
        m = dict(shared)
        m["x"] = np.ascontiguousarray(xf[i * NB:(i + 1) * NB])
        m["c"] = np.ascontiguousarray(cf[i * NB:(i + 1) * NB])
        in_maps.append(m)
    res = run_bass_kernel_spmd(nc, in_maps, core_ids=list(range(ncores)))
    return res


def kernel(**inputs):
    res = _run(inputs)
    return np.concatenate([np.asarray(r["y"], dtype=np.float32) for r in res.results], axis=0)
```

```python
import math
from contextlib import ExitStack
import numpy as np
import concourse.bass as bass
import concourse.mybir as mybir
from concourse.bass_utils import run_bass_kernel_spmd

F32 = mybir.dt.float32
BF16 = mybir.dt.bfloat16
AF = mybir.ActivationFunctionType
ALU = mybir.AluOpType

P = 128
S = 2048
D = 1024
DC = 8
NTS = 4
TS = 512
FF = 2816
FC = 22
NB = 2
EPS = 1e-6
ATT_GROUPS = ((128, 1), (512, 4), (2048, 16))
OFF_AQ, OFF_AK, OFF_AV = 0, 768, 1536
OFF_GQ, OFF_GK, OFF_GV, OFF_GR, OFF_GD, OFF_GA, OFF_GG = 2304, 2816, 3328, 4352, 5376, 5392, 6416
IN_W = 7440
SLOPES = [2.0 ** (-8.0 * (i + 1) / 12.0) for i in range(12)]
C_ID, C_STEP, C_CAUS, C_SCAN, C_BD = 0, 128, 384, 512, 640
C_TOT = 768


class Prog:
    ENG = ("pe", "act", "dve", "pool", "sp")

    def __init__(self):
        self.ins = {e: [] for e in self.ENG}
        self.lastw = {}
        self.readers = {}
        self.waited = {e: {} for e in self.ENG}
        self.dcount = {}
        self.group_slots = set()

    def _deps(self, eng, reads, writes):
        toks = []
        for k in reads:
            t = self.lastw.get(k)
            if t is not None:
                toks.append(t)
        for k in writes:
            t = self.lastw.get(k)
            if t is not None:
                toks.append(t)
            toks.extend(self.readers.get(k, ()))
        need = {}
        for t in toks:
            src = (t[0], t[1])
            if t[0] == "e" and t[1] == eng and eng in ("pe", "sp"):
                continue
            if need.get(src, -1) < t[2]:
                need[src] = t[2]
        waits = []
        wd = self.waited[eng]
        for src, idx in need.items():
            if wd.get(src, -1) >= idx:
                continue
            wd[src] = idx
            if src[0] == "e":
                self.ins[src[1]][idx]["sig"] = True
            waits.append((src, idx))
        return waits

    def _commit(self, tok, reads, writes):
        for k in reads:
            self.readers.setdefault(k, []).append(tok)
        for k in writes:
            self.lastw[k] = tok
            self.readers[k] = []

    def op(self, eng, fn, reads=(), writes=()):
        waits = self._deps(eng, reads, writes)
        idx = len(self.ins[eng])
        self.ins[eng].append(dict(fn=fn, waits=waits, sig=False, dma=None))
        self._commit(("e", eng, idx), reads, writes)

    def dma(self, fn, slot, reads=(), writes=(), group=False, queue="sp"):
        waits = self._deps(queue, reads, writes)
        self.dcount[slot] = self.dcount.get(slot, 0) + 1
        if group:
            self.group_slots.add(slot)
            waits = [w for w in waits if not (w[0][0] == "d" and w[0][1] == slot)]
        self.ins[queue].append(dict(fn=fn, waits=waits, sig=False, dma=slot))
        self._commit(("d", slot, self.dcount[slot]), reads, writes)

    def transfer(self, old_keys, new_keys):
        toks = []
        for k in old_keys:
            t = self.lastw.get(k)
            if t is not None:
                toks.append(t)
            toks.extend(self.readers.get(k, ()))
        best = {}
        for t in toks:
            s = (t[0], t[1])
            if best.get(s, -1) < t[2]:
                best[s] = t[2]
        toks = [(s[0], s[1], i) for s, i in best.items()]
        for k in new_keys:
            self.lastw[k] = None
            self.readers[k] = list(toks)

    def emit(self, nc, block, sems, dsems, final_waits):
        signo = {}
        for e in self.ENG:
            c = 0
            arr = []
            for it in self.ins[e]:
                if it["sig"]:
                    c += 1
                arr.append(c)
            signo[e] = arr

        def run(e, eng):
            for it in self.ins[e]:
                for src, idx in it["waits"]:
                    if src[0] == "e":
                        eng.wait_ge(sems[src[1]], signo[src[1]][idx])
                    else:
                        cnt = self.dcount[src[1]] if src[1] in self.group_slots else idx
                        eng.wait_ge(dsems[src[1]], 16 * cnt)
                r = getattr(eng, it["fn"][0])(**it["fn"][1])
                if it["dma"] is not None:
                    r.then_inc(dsems[it["dma"]], 16)
                elif it["sig"]:
                    r.then_inc(sems[e], 1)
            if e == "sp":
                for slot in final_waits:
                    if self.dcount.get(slot, 0):
                        eng.wait_ge(dsems[slot], 16 * self.dcount[slot])

        @block.tensor
        def _(eng):
            run("pe", eng)

        @block.scalar
        def _(eng):
            run("act", eng)

        @block.vector
        def _(eng):
            run("dve", eng)

        @block.gpsimd
        def _(eng):
            run("pool", eng)

        @block.sync
        def _(eng):
            run("sp", eng)


def build_nc(upto=99, dbg=False, nbr=NB):
    nc = bass.Bass("TRN2", target_bir_lowering=False)

    def din(name, shape):
        return nc.dram_tensor(name, list(shape), F32, kind="ExternalInput").ap()

    x = din("x", [NB, S, D])
    c = din("c", [NB, D])
    w_mod = din("w_mod", [D, 9 * D])
    b_mod = din("b_mod", [9 * D])
    g_l = [din("g_ffn1", [D]), din("g_mix", [D]), din("g_ffn2", [D])]
    ffn_w = [(din("ffn1_w1", [D, FF]), din("ffn1_w3", [D, FF]), din("ffn1_w2", [FF, D])),
             (din("ffn2_w1", [D, FF]), din("ffn2_w3", [D, FF]), din("ffn2_w2", [FF, D]))]
    w_in = din("w_in", [D, IN_W])
    q_norm_g = din("q_norm_g", [64])
    k_norm_g = din("k_norm_g", [64])
    gla_gate_up = din("gla_gate_up", [16, 512])
    gla_gate_bias = din("gla_gate_bias", [512])
    gla_out_norm_g = din("gla_out_norm_g", [256])
    w_branch_att = din("w_branch_att", [256, D])
    w_branch_gla = din("w_branch_gla", [D, D])
    w_out = din("w_out", [D, D])
    consts = din("consts", [P, C_TOT])
    y = nc.dram_tensor("y", [NB, S, D], F32, kind="ExternalOutput").ap()
    dbg_out = None
    if dbg:
        dbg_out = nc.dram_tensor("dbg", [4, P, DC * S], F32, kind="ExternalOutput").ap()

    pg = Prog()
    es = ExitStack()

    def sb(name, shape, dt):
        return es.enter_context(nc.sbuf_tensor(name, list(shape), dt))

    with es:
        h = sb("h", [P, DC, S], F32)
        u = sb("u", [P, DC, S], BF16)
        arena = sb("arena", [P, 28 * 1024], BF16)
        NST = 2
        stg = [sb(f"stg{i}", [P, 2048], F32) for i in range(NST)]
        NWB = 4
        wbs = [sb(f"wb{i}", [P, 2048], BF16) for i in range(NWB)]
        cst = sb("cst", [P, C_TOT], F32)
        NT32 = 4
        t32 = [sb(f"t32_{i}", [P, TS], F32) for i in range(NT32)]
        NT16 = 4
        t16 = [sb(f"t16_{i}", [P, TS], BF16) for i in range(NT16)]
        ident_bf = sb("ident_bf", [P, P], BF16)
        ones_bf = sb("ones_bf", [P, P], BF16)
        bd_bf = sb("bd_bf", [P, P], BF16)
        kst = sb("kst", [P, 8], F32)
        cT = sb("cT", [P, DC, NB], F32)
        cact = sb("cact", [P, DC, NB], BF16)
        bmodT = sb("bmodT", [P, 72], F32)
        modT = sb("modT", [P, 72, NB], F32)
        gT = sb("gT", [P, 3, DC], F32)
        Amod = sb("Amod", [P, 3, DC, NB], F32)
        Gmod = sb("Gmod", [P, 3, DC, NB], F32)
        qg = sb("qg", [P, 1], F32)
        kg = sb("kg", [P, 1], F32)
        negb = sb("negb", [P, 4], F32)
        gno = sb("gno", [P, 2], F32)
        gup = sb("gup", [80, 512], BF16)
        gdT = sb("gdT", [80, 2 * TS], BF16)
        Sst = sb("Sst", [P, 256], F32)
        Sbf = sb("Sbf", [P, 256], BF16)
        ebl = sb("ebl", [P, 16], F32)
        kdT = sb("kdT", [P, 2, P], BF16)
        kdtm = sb("kdtm", [P, 2, P], BF16)
        amk = sb("amk", [P, 2, P], BF16)
        psum = es.enter_context(nc.psum_tensor("ps", [P, 8 * TS], F32))
        psum16 = psum.bitcast(BF16) if hasattr(psum, "bitcast") else None

        sems = {e: es.enter_context(nc.semaphore(f"s_{e}")) for e in ("pe", "act", "dve", "pool")}
        dslots = ["pro", "stg0", "stg1", "dbg"]
        dsems = {s_: es.enter_context(nc.semaphore(f"d_{s_}")) for s_ in dslots}
        block = es.enter_context(nc.Block())

        cnt = dict(bank=0, t32=0, tL=0, t16=0, stg=0, wb=0, alt=0)

        def bank():
            i = cnt["bank"] % 8
            cnt["bank"] += 1
            return psum[:, i * TS:(i + 1) * TS], ("ps", i), i

        def tmp32():
            i = cnt["t32"] % 2
            cnt["t32"] += 1
            return t32[i], ("t32", i)

        def tmpL():
            i = 2 + cnt["tL"] % 2
            cnt["tL"] += 1
            return t32[i], ("t32", i)

        def tmp16():
            i = cnt["t16"] % NT16
            cnt["t16"] += 1
            return t16[i], ("t16", i)

        def stage():
            i = cnt["stg"] % NST
            cnt["stg"] += 1
            return stg[i], ("stg", i), f"stg{i}"

        def alt():
            cnt["alt"] += 1
            return "act" if cnt["alt"] % 2 else "dve"

        def copy_op(eng, out, in_, reads, writes):
            if eng == "act":
                pg.op("act", ("copy", dict(out=out, in_=in_)), reads, writes)
            else:
                pg.op(eng, ("tensor_copy", dict(out=out, in_=in_)), reads, writes)

        def load_w(src, a, b_):
            st, skey, sslot = stage()
            n = a * b_
            stv = st[:, 0:n].rearrange("p (a b) -> p a b", a=a)
            pg.dma(("dma_start", dict(out=stv, in_=src)), sslot, writes=[skey])
            i = cnt["wb"] % NWB
            cnt["wb"] += 1
            wv = wbs[i][:, 0:n].rearrange("p (a b) -> p a b", a=a)
            pg.op("pool", ("tensor_copy", dict(out=wv, in_=stv)), [skey], [("wb", i)])
            return wv, ("wb", i)

        def kc_view(w, c0, ncol):
            return w.rearrange("(kc p) f -> p kc f", p=P)[:, :, c0:c0 + ncol]

        def mm(out, lhsT, rhs, start, stop, reads, writes):
            pg.op("pe", ("matmul", dict(out=out, lhsT=lhsT, rhs=rhs, start=start, stop=stop)),
                  reads, writes)

        ukeys = [("u", t) for t in range(NTS)]

        def pro(out, in_, key, slow=False):
            if slow:
                pg.dma(("dma_start", dict(out=out, in_=in_, allow_slow_non_contiguous=True)), "pro",
                       writes=[key], group=True)
            else:
                pg.dma(("dma_start", dict(out=out, in_=in_)), "pro", writes=[key], group=True)

        pro(cst[:], consts, "cst")
        for b in range(NB):
            pro(cT[:, :, b], c[b].rearrange("(kc p) -> p kc", p=P), ("cT", b), slow=True)
        pro(bmodT[:], b_mod.rearrange("(n p) -> p n", p=P), "bmodT", slow=True)
        for l in range(3):
            pro(gT[:, l, :], g_l[l].rearrange("(n p) -> p n", p=P), ("gT", l), slow=True)
        for hh in range(2):
            pro(qg[hh * 64:(hh + 1) * 64, :], q_norm_g.rearrange("(p o) -> p o", o=1), ("qg", hh), slow=True)
            pro(kg[hh * 64:(hh + 1) * 64, :], k_norm_g.rearrange("(p o) -> p o", o=1), ("kg", hh), slow=True)
        pro(negb[:], gla_gate_bias.rearrange("(n p) -> p n", p=P), "negb", slow=True)
        pro(gno[:], gla_out_norm_g.rearrange("(n p) -> p n", p=P), "gno", slow=True)

        pg.op("dve", ("memset", dict(ap=ones_bf[:], constant=1.0)), [], ["ones"])
        pg.op("dve", ("memset", dict(ap=kst[:, 0:1], constant=EPS)), [], ["kst0"])
        pg.op("dve", ("memset", dict(ap=kst[:, 1:2], constant=1.0)), [], ["kst1"])
        pg.op("dve", ("memset", dict(ap=kst[:, 2:3], constant=math.log(128.0 ** -0.5))), [], ["kst2"])
        pg.op("dve", ("tensor_copy", dict(out=ident_bf[:], in_=cst[:, C_ID:C_ID + P])), ["cst"], ["identbf"])
        pg.op("dve", ("tensor_copy", dict(out=bd_bf[:], in_=cst[:, C_BD:C_BD + P])), ["cst"], ["bdbf"])
        for pb_ in (0, 32, 64):
            pro(t32[0][pb_:pb_ + 16, :], gla_gate_up, ("gupst", pb_))
        for pb_ in (0, 32, 64):
            pg.op("dve", ("tensor_copy", dict(out=gup[pb_:pb_ + 16, :], in_=t32[0][pb_:pb_ + 16, :])),
                  [("gupst", pb_)], ["gup", ("t32", 0)])
        pg.op("dve", ("tensor_scalar", dict(out=negb[:], in0=negb[:], scalar1=-1.0, scalar2=None, op0=ALU.mult)),
              ["negb"], ["negb"])
        pg.op("dve", ("tensor_scalar", dict(out=qg[:], in0=qg[:], scalar1=0.125, scalar2=None, op0=ALU.mult)),
              [("qg", 0), ("qg", 1)], [("qg", 0), ("qg", 1)])
        pg.op("act", ("activation", dict(out=cact[:], in_=cT[:], func=AF.Silu)), [("cT", 0), ("cT", 1)], ["cact"])
        ident = cst[:, C_ID:C_ID + P]
        stepsM = cst[:, C_STEP:C_STEP + 256]
        causM = cst[:, C_CAUS:C_CAUS + P]
        scanM = cst[:, C_SCAN:C_SCAN + P]

        bg = []

        def run_bg(n=1):
            for _ in range(n):
                if bg:
                    bg.pop(0)()

        def mod_tile(t):
            l = t // 12
            wv, wk = load_w(kc_view(w_mod, t * 256, 256), DC, 256)
            pm, pmk, _ = bank()
            for j in range(2):
                for kc in range(DC):
                    mm(pm[:, j * 2:j * 2 + 2], wv[:, kc, j * P:(j + 1) * P], cact[:, kc, :],
                       kc == 0, kc == DC - 1, [wk, "cact"], [pmk])
            for b in range(NB):
                pg.op("dve", ("tensor_tensor", dict(out=modT[:, 2 * t:2 * t + 2, b], in0=pm[:, b:4:2],
                                                    in1=bmodT[:, 2 * t:2 * t + 2], op=ALU.add)),
                      [pmk, "bmodT"], [("modT", l, b)])

        def mod_fin(l):
            coef = 1.0 if l == 1 else 0.5
            for b in range(NB):
                pg.op("dve", ("scalar_tensor_tensor", dict(
                    out=Amod[:, l, :, b], in0=modT[:, l * 24 + 8:l * 24 + 16, b], scalar=1.0, in1=gT[:, l, :],
                    op0=ALU.add, op1=ALU.mult)), [("modT", l, b), ("gT", l)], [("Amod", l, b)])
                pg.op("dve", ("tensor_scalar", dict(
                    out=Gmod[:, l, :, b], in0=modT[:, l * 24 + 16:l * 24 + 24, b], scalar1=1.0, scalar2=coef,
                    op0=ALU.add, op1=ALU.mult)), [("modT", l, b)], [("Gmod", l, b)])

        for t in range(12):
            mod_tile(t)
        mod_fin(0)
        for l in (1, 2):
            for t in range(12 * l, 12 * l + 12):
                bg.append(lambda t=t: mod_tile(t))
            bg.append(lambda l=l: mod_fin(l))

        def load_x(b):
            for tt in range(16):
                st, skey, sslot = stage()
                xs = st[:, 0:D]
                pg.dma(("dma_start", dict(out=xs, in_=x[b, tt * P:(tt + 1) * P, :])), sslot,
                       writes=[skey])
                for half in range(2):
                    pb, pk, _ = bank()
                    for j in range(4):
                        dc = half * 4 + j
                        pg.op("pe", ("transpose", dict(
                            out=pb[:, j * P:(j + 1) * P], in_=xs[:, dc * P:(dc + 1) * P], identity=ident)),
                            [skey, "cst"], [pk])
                    copy_op(alt(), h[:, half * 4:half * 4 + 4, tt * P:(tt + 1) * P],
                            pb.rearrange("p (a t) -> p a t", a=4), [pk], [("h", tt // 4)])

        def store_y(b):
            for tt in range(16):
                st, skey, sslot = stage()
                ys = st[:, 0:D]
                for half in range(2):
                    pb, pk, _ = bank()
                    for j in range(4):
                        dc = half * 4 + j
                        pg.op("pe", ("transpose", dict(
                            out=pb[:, j * P:(j + 1) * P], in_=h[:, dc, tt * P:(tt + 1) * P], identity=ident)),
                            [("h", tt // 4), "cst"], [pk])
                    copy_op(alt(), ys[:, half * 512:(half + 1) * 512], pb, [pk], [skey])
                pg.dma(("dma_start", dict(out=y[b, tt * P:(tt + 1) * P, :], in_=ys)), sslot,
                       reads=[skey])

        def rstd_from(pss, pssk, scale):
            r32, rk = tmpL()
            pg.op("act", ("activation", dict(out=r32[:], in_=pss, func=AF.Ln, bias=kst[:, 0:1], scale=scale)),
                  [pssk, "kst0"], [rk])
            pg.op("act", ("activation", dict(out=r32[:], in_=r32[:], func=AF.Exp, scale=-0.5)), [rk], [rk])
            return r32, rk

        def norm_mod(l, b):
            for ts in range(NTS):
                sl = slice(ts * TS, (ts + 1) * TS)
                pss, pssk, _ = bank()
                for dc in range(DC):
                    sq, sqk = tmp16()
                    pg.op("dve", ("tensor_tensor", dict(out=sq[:], in0=h[:, dc, sl], in1=h[:, dc, sl],
                                                                          op=ALU.mult)), [("h", ts)], [sqk])
                    mm(pss, ones_bf[:], sq[:], dc == 0, dc == DC - 1, [sqk, "ones"], [pssk])
                r32, rk = rstd_from(pss, pssk, 1.0 / D)
                for dc in range(DC):
                    tt_, tk = tmp32()
                    pg.op("dve", ("tensor_tensor", dict(out=tt_[:], in0=h[:, dc, sl], in1=r32[:],
                                                                            op=ALU.mult)), [("h", ts), rk], [tk])
                    pg.op("act", ("activation", dict(
                        out=u[:, dc, sl], in_=tt_[:], func=AF.Identity, bias=modT[:, l * 24 + dc, b:b + 1],
                        scale=Amod[:, l, dc, b:b + 1])), [tk, ("modT", l, b), ("Amod", l, b)], [("u", ts)])

        def gkey(f, ts):
            return ("g", f, ts)

        def ffn(fi, l, b):
            w1, w3, w2 = ffn_w[fi]
            gbuf = arena[:, 0:12 * S].rearrange("p (f t) -> p f t", f=12)
            for (f0, nf) in ((0, 12), (12, 10)):
                for tl in range(nf // 2):
                    c0 = (f0 + tl * 2) * P
                    w1v, w1k = load_w(kc_view(w1, c0, 256), DC, 256)
                    w3v, w3k = load_w(kc_view(w3, c0, 256), DC, 256)
                    for j in range(2):
                        fl = tl * 2 + j
                        for ts in range(NTS):
                            sl = slice(ts * TS, (ts + 1) * TS)
                            pa, pak, _ = bank()
                            pb, pbk, _ = bank()
                            for kc in range(DC):
                                mm(pa, w1v[:, kc, j * P:(j + 1) * P], u[:, kc, sl], kc == 0, kc == DC - 1,
                                   [w1k, ("u", ts)], [pak])
                            for kc in range(DC):
                                mm(pb, w3v[:, kc, j * P:(j + 1) * P], u[:, kc, sl], kc == 0, kc == DC - 1,
                                   [w3k, ("u", ts)], [pbk])
                            s1, s1k = tmp16()
                            pg.op("act", ("activation", dict(out=s1[:], in_=pa, func=AF.Silu)),
                                  [pak], [s1k])
                            pg.op("dve", ("tensor_tensor", dict(
                                out=gbuf[:, fl, sl], in0=pb, in1=s1[:], op=ALU.mult)), [pbk, s1k], [gkey(fl, ts)])
                    run_bg()
                w2v_all = w2.rearrange("(fc p) d -> p fc d", p=P)
                for dc in range(DC):
                    w2v, w2k = load_w(w2v_all[:, f0:f0 + nf, dc * P:(dc + 1) * P], nf, P)
                    for ts in range(NTS):
                        sl = slice(ts * TS, (ts + 1) * TS)
                        po, pok, _ = bank()
                        for fl in range(nf):
                            mm(po, w2v[:, fl, :], gbuf[:, fl, sl], fl == 0, fl == nf - 1, [w2k, gkey(fl, ts)], [pok])
                        pg.op("dve", ("scalar_tensor_tensor", dict(
                            out=h[:, dc, sl], in0=po, scalar=Gmod[:, l, dc, b:b + 1], in1=h[:, dc, sl],
                            op0=ALU.mult, op1=ALU.add)), [pok, ("Gmod", l, b), ("h", ts)], [("h", ts)])
                    run_bg()

        G_KEYS = [gkey(f, t) for f in range(12) for t in range(NTS)]

        KB = 512
        acc = arena[:, 0:32 * KB].bitcast(F32).rearrange("p (c o t) -> p c o t", c=2, o=2)
        ogla = arena[:, 0:32 * KB].rearrange("p (c t) -> p c t", c=8)
        qbuf = arena[:, 32 * KB:40 * KB].rearrange("p (c t) -> p c t", c=2)
        kbuf = arena[:, 40 * KB:48 * KB].rearrange("p (c t) -> p c t", c=2)
        vtm = arena[:, 48 * KB:56 * KB].rearrange("p (b f) -> p b f", b=16)
        oatt = arena[:, 32 * KB:40 * KB].rearrange("p (c t) -> p c t", c=2)
        gvtm = arena[:, 40 * KB:48 * KB].rearrange("p (b f) -> p b f", b=16)
        qdec = arena[:, 48 * KB:52 * KB]
        kinv = arena[:, 52 * KB:56 * KB]
        merged = arena[:, 40 * KB:56 * KB].rearrange("p (c t) -> p c t", c=8)

        ACC_KEYS = [("acc", ch, t) for ch in range(2) for t in range(NTS)]
        Q_KEYS = [("q", ch, t) for ch in range(2) for t in range(NTS)]
        K_KEYS = [("k", ch, t) for ch in range(2) for t in range(NTS)]
        V_KEYS = [("v", tb) for tb in range(16)]
        OATT_KEYS = [("oatt", t) for t in range(NTS)]
        OGLA_KEYS = [("ogla", hh, t) for hh in range(4) for t in range(NTS)]
        GV_KEYS = [("gv", tb) for tb in range(16)]
        QD_KEYS = [("qd", t) for t in range(NTS)]
        KI_KEYS = [("ki", t) for t in range(NTS)]
        KD_KEYS = []
        MG_KEYS = [("mg", t) for t in range(2)]

        def tok_slice(dil, r, n):
            st0 = r + dil * P * n
            return slice(st0, st0 + dil * (P - 1) + 1, dil)

        def blk_ts(dil, r, n):
            if dil == 1:
                return [n // 4]
            return list(range(NTS)) if dil == 16 else [n]

        def attention(b):
            for gi, (win, dil) in enumerate(ATT_GROUPS):
                nb = S // dil // P
                wq_, wqk_ = load_w(kc_view(w_in, OFF_AQ + gi * 256, 256), DC, 256)
                wk_, wkk_ = load_w(kc_view(w_in, OFF_AK + gi * 256, 256), DC, 256)
                items = [(wq_, wqk_, qbuf, qg, "q", ch, ts) for ch in range(2) for ts in range(NTS)] + \
                        [(wk_, wkk_, kbuf, kg, "k", ch, ts) for ch in range(2) for ts in range(NTS)]

                def qk_s1(it):
                    wv, wk, buf, gain, nm, ch, ts = it
                    sl = slice(ts * TS, (ts + 1) * TS)
                    pq, pqk, _ = bank()
                    for kc in range(DC):
                        mm(pq, wv[:, kc, ch * P:(ch + 1) * P], u[:, kc, sl], kc == 0, kc == DC - 1,
                           [wk, ("u", ts)], [pqk])
                    sq, sqk = tmp16()
                    pg.op("act", ("activation", dict(out=sq[:], in_=pq, func=AF.Square)), [pqk], [sqk])
                    return (pq, pqk, sq, sqk)

                def qk_s2(it, ctx):
                    wv, wk, buf, gain, nm, ch, ts = it
                    pq, pqk, sq, sqk = ctx
                    sl = slice(ts * TS, (ts + 1) * TS)
                    pss, pssk, _ = bank()
                    mm(pss, bd_bf[:], sq[:], True, True, [sqk, "bdbf"], [pssk])
                    r32, rk = rstd_from(pss, pssk, 1.0 / 64)
                    pg.op("dve", ("scalar_tensor_tensor", dict(out=buf[:, ch, sl], in0=pq, scalar=gain[:, 0:1], in1=r32[:],
                                                               op0=ALU.mult, op1=ALU.mult)),
                          [pqk, rk, (nm + "g", 0), (nm + "g", 1)], [(nm, ch, ts)])

                ctxs = {0: qk_s1(items[0])}
                for i in range(len(items)):
                    if i + 1 < len(items):
                        ctxs[i + 1] = qk_s1(items[i + 1])
                    qk_s2(items[i], ctxs.pop(i))
                wv, wk = load_w(kc_view(w_in, OFF_AV + gi * 256, 256), DC, 256)
                blocks = [(r, n) for r in range(dil) for n in range(nb)]
                for tb, (r, n) in enumerate(blocks):
                    tsl = tok_slice(dil, r, n)
                    pv, pvk, _ = bank()
                    for kc in range(DC):
                        mm(pv[:, 0:256], u[:, kc, tsl], wv[:, kc, :], kc == 0, kc == DC - 1,
                           [wk] + [("u", t) for t in blk_ts(dil, r, n)], [pvk])
                    copy_op(alt(), vtm[:, tb, :], pv[:, 0:256], [pvk], [("v", tb)])
                work = [(ch, tb) for ch in range(2) for tb in range(len(blocks))]
                pend = {}

                def emit_qk(ch, tb):
                    r, n = blocks[tb]
                    tsl = tok_slice(dil, r, n)
                    tss = blk_ts(dil, r, n)
                    Es = []
                    for hh in range(2):
                        head = gi * 4 + ch * 2 + hh
                        cs = -SLOPES[head] * dil
                        ps_, psk, _ = bank()
                        prt = slice(hh * 64, (hh + 1) * 64)
                        rd = [("q", ch, t) for t in tss] + [("k", ch, t) for t in tss]
                        mm(ps_[:, 128:256], kbuf[prt, ch, tsl], qbuf[prt, ch, tsl], True, True, rd, [psk])
                        lo = 128
                        if n > 0:
                            psl = tok_slice(dil, r, n - 1)
                            rd2 = rd + [("k", ch, t) for t in blk_ts(dil, r, n - 1)]
                            mm(ps_[:, 0:128], kbuf[prt, ch, psl], qbuf[prt, ch, tsl], True, True, rd2, [psk])
                            lo = 0
                        t_, tk = tmp32()
                        pg.op("dve", ("scalar_tensor_tensor", dict(
                            out=t_[:, lo:256], in0=stepsM[:, lo:256], scalar=cs, in1=ps_[:, lo:256],
                            op0=ALU.mult, op1=ALU.add)), [psk, "cst"], [tk])
                        E, Ek = tmp16()
                        pg.op("act", ("activation", dict(out=E[:, lo:256], in_=t_[:, lo:256],
                                                                               func=AF.Exp)), [tk], [Ek])
                        Es.append((E, Ek, lo))
                    pend[(ch, tb)] = Es

                def emit_pv(ch, tb):
                    r, n = blocks[tb]
                    tsl = tok_slice(dil, r, n)
                    tss = blk_ts(dil, r, n)
                    Es = pend.pop((ch, tb))
                    po, pok, _ = bank()
                    for hh in range(2):
                        E, Ek, lo = Es[hh]
                        prt = slice(hh * 64, (hh + 1) * 64)
                        vc = slice(ch * P + hh * 64, ch * P + hh * 64 + 64)
                        mm(po[prt, 0:128], vtm[:, tb, vc], E[:, 128:256], True, n == 0, [Ek, ("v", tb)], [pok])
                        if n > 0:
                            mm(po[prt, 0:128], vtm[:, tb - 1, vc], E[:, 0:128], False, True, [Ek, ("v", tb - 1)], [pok])
                        mm(po[prt, 128:256], ones_bf[:, 0:64], E[:, 128:256], True, n == 0, [Ek, "ones"], [pok])
                        if n > 0:
                            mm(po[prt, 128:256], ones_bf[:, 0:64], E[:, 0:128], False, True, [Ek, "ones"], [pok])
                    pov = po[:, 0:256].rearrange("p (o t) -> p o t", o=2)
                    akeys = [("acc", ch, t) for t in tss]
                    if gi == 0:
                        pg.op("dve", ("tensor_copy", dict(out=acc[:, ch, :, tsl], in_=pov)), [pok], akeys)
                    else:
                        pg.op("dve", ("tensor_tensor", dict(out=acc[:, ch, :, tsl], in0=acc[:, ch, :, tsl], in1=pov,
                                                               op=ALU.add)), [pok] + akeys, akeys)

                emit_qk(*work[0])
                for i in range(len(work)):
                    if i + 1 < len(work):
                        emit_qk(*work[i + 1])
                    emit_pv(*work[i])
            pg.transfer(Q_KEYS, OATT_KEYS)
            for ch in range(2):
                for ts in range(NTS):
                    sl = slice(ts * TS, (ts + 1) * TS)
                    r_, rk = tmp32()
                    pg.op("dve", ("reciprocal", dict(out=r_[:], in_=acc[:, ch, 1, sl])),
                          [("acc", ch, ts)], [rk])
                    pg.op("dve", ("tensor_tensor", dict(out=oatt[:, ch, sl], in0=acc[:, ch, 0, sl],
                                                                              in1=r_[:], op=ALU.mult)),
                          [("acc", ch, ts), rk], [("oatt", ts)])

        def gd_pb(ts):
            return 0 if ts == 3 else 32 * ts

        def gd_ap(ts):
            c0 = TS if ts == 3 else 0
            return gdT[gd_pb(ts):gd_pb(ts) + 16, c0:c0 + TS]

        def gla(b):
            wv, wk = load_w(kc_view(w_in, OFF_GD, 16), DC, 16)
            for ts in range(NTS):
                sl = slice(ts * TS, (ts + 1) * TS)
                pd, pdk, _ = bank()
                for kc in range(DC):
                    mm(pd[gd_pb(ts):gd_pb(ts) + 16, :], wv[:, kc, :], u[:, kc, sl], kc == 0, kc == DC - 1, [wk, ("u", ts)], [pdk])
                copy_op("act", gd_ap(ts), pd[gd_pb(ts):gd_pb(ts) + 16, :], [pdk], [("gd", ts)])
            for hh in range(4):
                wq, wqk = load_w(kc_view(w_in, OFF_GQ + hh * P, P), DC, P)
                wkk, wkkk = load_w(kc_view(w_in, OFF_GK + hh * P, P), DC, P)
                for ts in range(NTS):
                    sl = slice(ts * TS, (ts + 1) * TS)
                    px, pxk, _ = bank()
                    mm(px, gup[gd_pb(ts):gd_pb(ts) + 16, hh * P:(hh + 1) * P], gd_ap(ts), True, True, ["gup", ("gd", ts)], [pxk])
                    e_, ek = tmp32()
                    pg.op("act", ("activation", dict(out=e_[:], in_=px, func=AF.Exp,
                                                                             bias=negb[:, hh:hh + 1], scale=-1.0)),
                          [pxk, "negb"], [ek])
                    sp_, spk = tmp32()
                    pg.op("act", ("activation", dict(out=sp_[:], in_=e_[:], func=AF.Ln,
                                                                        bias=kst[:, 1:2], scale=1.0)), [ek, "kst1"], [spk])
                    B_, Bk = tmpL()
                    for c4 in range(4):
                        pg.op("dve", ("tensor_tensor_scan", dict(
                            out=B_[:, c4 * P:(c4 + 1) * P], data0=scanM, data1=sp_[:, c4 * P:(c4 + 1) * P], initial=0.0,
                            op0=ALU.mult, op1=ALU.add)), [spk, "cst"], [Bk])
                    pg.op("act", ("activation", dict(
                        out=ebl[:, ts * 4:ts * 4 + 4], in_=B_[:, 127:512:128], func=AF.Exp, scale=-1.0 / 16)),
                        [Bk], [("ebl", ts)])
                    eb, ebk = tmp32()
                    pg.op("act", ("activation", dict(out=eb[:], in_=B_[:], func=AF.Exp,
                                                                      bias=kst[:, 2:3], scale=-1.0 / 16)), [Bk, "kst2"], [ebk])
                    pq, pqk, _ = bank()
                    for kc in range(DC):
                        mm(pq, wq[:, kc, :], u[:, kc, sl], kc == 0, kc == DC - 1, [wqk, ("u", ts)], [pqk])
                    pg.op("dve", ("tensor_tensor", dict(out=qdec[:, sl], in0=pq, in1=eb[:],
                                                                                op=ALU.mult)), [pqk, ebk], [("qd", ts)])
                    en, enk = tmp32()
                    pg.op("act", ("activation", dict(out=en[:], in_=B_[:], func=AF.Exp, scale=1.0 / 16)),
                          [Bk], [enk])
                    pk_, pkk, _ = bank()
                    for kc in range(DC):
                        mm(pk_, wkk[:, kc, :], u[:, kc, sl], kc == 0, kc == DC - 1, [wkkk, ("u", ts)], [pkk])
                    pg.op("dve", ("tensor_tensor", dict(out=kinv[:, sl], in0=pk_, in1=en[:],
                                                                                  op=ALU.mult)), [pkk, enk], [("ki", ts)])
                wv, wk = load_w(kc_view(w_in, OFF_GV + hh * 256, 256), DC, 256)
                for tb in range(16):
                    pv, pvk, _ = bank()
                    for kc in range(DC):
                        mm(pv[:, 0:256], u[:, kc, tb * P:(tb + 1) * P], wv[:, kc, :], kc == 0, kc == DC - 1,
                           [wk, ("u", tb // 4)], [pvk])
                    copy_op(alt(), gvtm[:, tb, :], pv[:, 0:256], [pvk], [("gv", tb)])
                def emit_kd(cc):
                    csl = slice(cc * P, (cc + 1) * P)
                    j = cc % 2
                    pg.op("dve", ("tensor_scalar", dict(
                        out=kdT[:, j, :], in0=kinv[:, csl], scalar1=ebl[:, cc:cc + 1], scalar2=None, op0=ALU.mult)),
                        [("ki", cc // 4), ("ebl", cc // 4)], [("kdT", j)])
                    pt, ptk, bi = bank()
                    pt16 = psum16[:, bi * 2 * TS:bi * 2 * TS + P]
                    pg.op("pe", ("transpose", dict(out=pt16, in_=kdT[:, j, :], identity=ident_bf[:])),
                          [("kdT", j), "identbf"], [ptk])
                    copy_op("act", kdtm[:, j, :], pt16, [ptk], [("kd", j)])
                emit_kd(0)
                for cc in range(16):
                    csl = slice(cc * P, (cc + 1) * P)
                    ts = cc // 4
                    pa, pak, _ = bank()
                    mm(pa[:, 0:P], kinv[:, csl], qdec[:, csl], True, True, [("ki", ts), ("qd", ts)], [pak])
                    j = cc % 2
                    if cc + 1 < 15:
                        emit_kd(cc + 1)
                    pg.op("dve", ("tensor_tensor", dict(out=amk[:, j, :], in0=pa[:, 0:P], in1=causM,
                                                                       op=ALU.mult)), [pak, "cst"], [("amk", j)])
                    po, pok, _ = bank()
                    for dv in range(2):
                        mm(po[:, dv * P:(dv + 1) * P], gvtm[:, cc, dv * P:(dv + 1) * P], amk[:, j, :], True, cc == 0,
                           [("gv", cc), ("amk", j)], [pok])
                        if cc > 0:
                            mm(po[:, dv * P:(dv + 1) * P], Sbf[:, dv * P:(dv + 1) * P], qdec[:, csl], False, True,
                               ["Sbf", ("qd", ts)], [pok])
                    copy_op("act", ogla[:, hh * 2:hh * 2 + 2, csl], po[:, 0:256].rearrange("p (a t) -> p a t", a=2),
                            [pok], [("ogla", hh, ts)])
                    if cc < 15:
                        pu, puk, _ = bank()
                        mm(pu[:, 0:256], kdtm[:, j, :], gvtm[:, cc, :], True, True, [("kd", j), ("gv", cc)], [puk])
                        if cc == 0:
                            pg.op("dve", ("tensor_copy", dict(out=Sst[:], in_=pu[:, 0:256])), [puk], ["Sst"])
                        else:
                            pg.op("dve", ("scalar_tensor_tensor", dict(
                                out=Sst[:], in0=Sst[:], scalar=ebl[:, cc:cc + 1], in1=pu[:, 0:256],
                                op0=ALU.mult, op1=ALU.add)), [puk, "Sst", ("ebl", ts)], ["Sst"])
                        copy_op("act", Sbf[:], Sst[:], ["Sst"], ["Sbf"])
                wv, wk = load_w(kc_view(w_in, OFF_GR + hh * 256, 256), DC, 256)
                for ts in range(NTS):
                    sl = slice(ts * TS, (ts + 1) * TS)
                    pss, pssk, _ = bank()
                    for dv in range(2):
                        sq, sqk = tmp16()
                        pg.op("dve", ("tensor_tensor", dict(
                            out=sq[:], in0=ogla[:, hh * 2 + dv, sl], in1=ogla[:, hh * 2 + dv, sl], op=ALU.mult)),
                            [("ogla", hh, ts)], [sqk])
                        mm(pss, ones_bf[:], sq[:], dv == 0, dv == 1, [sqk, "ones"], [pssk])
                    r32, rk = rstd_from(pss, pssk, 1.0 / 256)
                    for dv in range(2):
                        pr, prk, _ = bank()
                        for kc in range(DC):
                            mm(pr, wv[:, kc, dv * P:(dv + 1) * P], u[:, kc, sl], kc == 0, kc == DC - 1,
                               [wk, ("u", ts)], [prk])
                        sg, sgk = tmp32()
                        pg.op("act", ("activation", dict(out=sg[:], in_=pr, func=AF.Silu)), [prk], [sgk])
                        t1, t1k = tmp32()
                        pg.op("dve", ("scalar_tensor_tensor", dict(
                            out=t1[:], in0=ogla[:, hh * 2 + dv, sl], scalar=gno[:, dv:dv + 1], in1=r32[:],
                            op0=ALU.mult, op1=ALU.mult)), [("ogla", hh, ts), rk, "gno"], [t1k])
                        pg.op("dve", ("tensor_tensor", dict(
                            out=ogla[:, hh * 2 + dv, sl], in0=t1[:], in1=sg[:], op=ALU.mult)),
                            [t1k, sgk, ("ogla", hh, ts)], [("ogla", hh, ts)])

        def merge_out(b):
            wba_all = w_branch_att.rearrange("(kc p) d -> p kc d", p=P)
            for th in range(2):
                for dp in range(4):
                    c0 = dp * 256
                    wga, wgak = load_w(kc_view(w_in, OFF_GA + c0, 256), DC, 256)
                    wba, wbak = load_w(wba_all[:, :, c0:c0 + 256], 2, 256)
                    for j in range(2):
                        dc = dp * 2 + j
                        for t2 in range(2):
                            ts = th * 2 + t2
                            sl = slice(ts * TS, (ts + 1) * TS)
                            p1, p1k, _ = bank()
                            for kc in range(DC):
                                mm(p1, wga[:, kc, j * P:(j + 1) * P], u[:, kc, sl], kc == 0, kc == DC - 1,
                                   [wgak, ("u", ts)], [p1k])
                            sa, sak = tmp32()
                            pg.op("act", ("activation", dict(out=sa[:], in_=p1, func=AF.Sigmoid)),
                                  [p1k], [sak])
                            p2, p2k, _ = bank()
                            for c2 in range(2):
                                mm(p2, wba[:, c2, j * P:(j + 1) * P], oatt[:, c2, sl], c2 == 0, c2 == 1,
                                   [wbak, ("oatt", ts)], [p2k])
                            pg.op("dve", ("tensor_tensor", dict(
                                out=merged[:, dc, t2 * TS:(t2 + 1) * TS], in0=p2, in1=sa[:], op=ALU.mult)),
                                [p2k, sak], [("mg", t2)])
                    wgg, wggk = load_w(kc_view(w_in, OFF_GG + c0, 256), DC, 256)
                    wbg, wbgk = load_w(kc_view(w_branch_gla, c0, 256), DC, 256)
                    for j in range(2):
                        dc = dp * 2 + j
                        for t2 in range(2):
                            ts = th * 2 + t2
                            sl = slice(ts * TS, (ts + 1) * TS)
                            p1, p1k, _ = bank()
                            for kc in range(DC):
                                mm(p1, wgg[:, kc, j * P:(j + 1) * P], u[:, kc, sl], kc == 0, kc == DC - 1,
                                   [wggk, ("u", ts)], [p1k])
                            sa, sak = tmp32()
                            pg.op("act", ("activation", dict(out=sa[:], in_=p1, func=AF.Sigmoid)),
                                  [p1k], [sak])
                            p2, p2k, _ = bank()
                            for kc in range(DC):
                                mm(p2, wbg[:, kc, j * P:(j + 1) * P], ogla[:, kc, sl], kc == 0, kc == DC - 1,
                                   [wbgk, ("ogla", kc // 2, ts)], [p2k])
                            m2, m2k = tmp32()
                            pg.op("dve", ("tensor_tensor", dict(out=m2[:], in0=p2, in1=sa[:],
                                                                                         op=ALU.mult)), [p2k, sak], [m2k])
                            pg.op("pool", ("tensor_tensor", dict(
                                out=merged[:, dc, t2 * TS:(t2 + 1) * TS], in0=merged[:, dc, t2 * TS:(t2 + 1) * TS],
                                in1=m2[:], op=ALU.add)), [m2k, ("mg", t2)], [("mg", t2)])
                for dp in range(4):
                    wo, wok = load_w(kc_view(w_out, dp * 256, 256), DC, 256)
                    for j in range(2):
                        dc = dp * 2 + j
                        for t2 in range(2):
                            ts = th * 2 + t2
                            sl = slice(ts * TS, (ts + 1) * TS)
                            po, pok, _ = bank()
                            for kc in range(DC):
                                mm(po, wo[:, kc, j * P:(j + 1) * P], merged[:, kc, t2 * TS:(t2 + 1) * TS], kc == 0,
                                   kc == DC - 1, [wok, ("mg", t2)], [pok])
                            pg.op("dve", ("scalar_tensor_tensor", dict(
                                out=h[:, dc, sl], in0=po, scalar=Gmod[:, 1, dc, b:b + 1], in1=h[:, dc, sl],
                                op0=ALU.mult, op1=ALU.add)), [pok, ("Gmod", 1, b), ("h", ts)], [("h", ts)])

        def dump(slot_i, b):
            if dbg and b == 0:
                pg.dma(("dma_start", dict(out=dbg_out[slot_i].rearrange("p (c t) -> p c t", c=DC), in_=h[:])), "dbg",
                       reads=[("h", t) for t in range(NTS)])

        MIX_A = ACC_KEYS + Q_KEYS + K_KEYS + V_KEYS
        for b in range(nbr):
            load_x(b)
            norm_mod(0, b)
            if upto >= 1:
                ffn(0, 0, b)
            dump(0, b)
            run_bg(len(bg))
            if upto >= 2:
                norm_mod(1, b)
                pg.transfer(G_KEYS, MIX_A + KD_KEYS)
                attention(b)
                pg.transfer(ACC_KEYS, OGLA_KEYS)
                pg.transfer(K_KEYS, GV_KEYS)
                pg.transfer(V_KEYS, QD_KEYS + KI_KEYS)
                gla(b)
                pg.transfer(GV_KEYS + QD_KEYS + KI_KEYS, MG_KEYS)
                merge_out(b)
                dump(1, b)
            if upto >= 3:
                norm_mod(2, b)
                pg.transfer(OGLA_KEYS + OATT_KEYS + MG_KEYS + KD_KEYS + MIX_A + GV_KEYS + QD_KEYS + KI_KEYS, G_KEYS)
                ffn(1, 2, b)
            elif upto >= 2:
                pg.transfer(OGLA_KEYS + OATT_KEYS + MG_KEYS + KD_KEYS + MIX_A + GV_KEYS + QD_KEYS + KI_KEYS, G_KEYS)
            store_y(b)

        pg.emit(nc, block, sems, dsems, final_waits=["stg0", "stg1", "dbg"])
    return nc


def make_consts():
    cs = np.zeros((P, C_TOT), np.float32)
    cs[:, C_ID:C_ID + P] = np.eye(P, dtype=np.float32)
    kk = np.arange(P)[:, None]
    qq = np.arange(P)[None, :]
    BIG = 1.0e4
    prev = np.where(qq <= kk, (qq + P - kk).astype(np.float32), BIG)
    cur = np.where(qq >= kk, (qq - kk).astype(np.float32), BIG)
    cs[:, C_STEP:C_STEP + P] = prev
    cs[:, C_STEP + P:C_STEP + 2 * P] = cur
    cs[:, C_CAUS:C_CAUS + P] = (kk <= qq).astype(np.float32)
    cs[:, C_SCAN:C_SCAN + P] = 1.0
    bd = np.zeros((P, P), np.float32)
    bd[:64, :64] = 1.0
    bd[64:, 64:] = 1.0
    cs[:, C_BD:C_BD + P] = bd
    return cs


_NC_CACHE = {}


def _run(inputs, upto=99, dbg=False, ncores=8):
    key = (upto, dbg)
    if key not in _NC_CACHE:
        _NC_CACHE[key] = build_nc(upto, dbg)
    nc = _NC_CACHE[key]
    f = lambda a: np.ascontiguousarray(np.asarray(a, dtype=np.float32))
    sq = lambda a: f(a)[0]
    shared = {
        "w_mod": sq(inputs["w_mod"]), "b_mod": sq(inputs["b_mod"]),
        "g_ffn1": sq(inputs["g_ffn1"]), "g_mix": sq(inputs["g_mix"]), "g_ffn2": sq(inputs["g_ffn2"]),
        "ffn1_w1": sq(inputs["ffn1_w1"]), "ffn1_w3": sq(inputs["ffn1_w3"]), "ffn1_w2": sq(inputs["ffn1_w2"]),
        "ffn2_w1": sq(inputs["ffn2_w1"]), "ffn2_w3": sq(inputs["ffn2_w3"]), "ffn2_w2": sq(inputs["ffn2_w2"]),
        "w_in": sq(inputs["w_in"]), "q_norm_g": sq(inputs["q_norm_g"]), "k_norm_g": sq(inputs["k_norm_g"]),
        "gla_gate_up": sq(inputs["gla_gate_up"]), "gla_gate_bias": sq(inputs["gla_gate_bias"]),
        "gla_out_norm_g": sq(inputs["gla_out_norm_g"]), "w_branch_att": sq(inputs["w_branch_att"]),
        "w_branch_gla": sq(inputs["w_branch_gla"]), "w_out": sq(inputs["w_out"]),
        "consts": make_consts(),
    }
    xf = f(inputs["x"])
    cf = f(inputs["c"])
    in_maps = []
    for i in range(ncores):
        m = dict(shared)
        m["x"] = np.ascontiguousarray(xf[i * NB:(i + 1) * NB])
        m["c"] = np.ascontiguousarray(cf[i * NB:(i + 1) * NB])
        in_maps.append(m)
    res = run_bass_kernel_spmd(nc, in_maps, core_ids=list(range(ncores)))
    return res


def kernel(**inputs):
    res = _run(inputs)
    return np.concatenate([np.asarray(r["y"], dtype=np.float32) for r in res.results], axis=0)
```

```python
import math
from contextlib import ExitStack
import numpy as np
import concourse.bass as bass
import concourse.mybir as mybir
from concourse.bass_utils import run_bass_kernel_spmd

F32 = mybir.dt.float32
BF16 = mybir.dt.bfloat16
AF = mybir.ActivationFunctionType
ALU = mybir.AluOpType

P = 128
S = 2048
D = 1024
DC = 8
NTS = 4
TS = 512
FF = 2816
FC = 22
NB = 2
EPS = 1e-6
ATT_GROUPS = ((128, 1), (512, 4), (2048, 16))
OFF_AQ, OFF_AK, OFF_AV = 0, 768, 1536
OFF_GQ, OFF_GK, OFF_GV, OFF_GR, OFF_GD, OFF_GA, OFF_GG = 2304, 2816, 3328, 4352, 5376, 5392, 6416
IN_W = 7440
SLOPES = [2.0 ** (-8.0 * (i + 1) / 12.0) for i in range(12)]
C_ID, C_STEP, C_CAUS, C_SCAN, C_BD = 0, 128, 384, 512, 640
C_TOT = 768


class Prog:
    ENG = ("pe", "act", "dve", "pool", "sp")

    def __init__(self):
        self.ins = {e: [] for e in self.ENG}
        self.lastw = {}
        self.readers = {}
        self.waited = {e: {} for e in self.ENG}
        self.dcount = {}
        self.group_slots = set()

    def _deps(self, eng, reads, writes):
        toks = []
        for k in reads:
            t = self.lastw.get(k)
            if t is not None:
                toks.append(t)
        for k in writes:
            t = self.lastw.get(k)
            if t is not None:
                toks.append(t)
            toks.extend(self.readers.get(k, ()))
        need = {}
        for t in toks:
            src = (t[0], t[1])
            if t[0] == "e" and t[1] == eng and eng in ("pe", "sp"):
                continue
            if need.get(src, -1) < t[2]:
                need[src] = t[2]
        waits = []
        wd = self.waited[eng]
        for src, idx in need.items():
            if wd.get(src, -1) >= idx:
                continue
            wd[src] = idx
            if src[0] == "e":
                self.ins[src[1]][idx]["sig"] = True
            waits.append((src, idx))
        return waits

    def _commit(self, tok, reads, writes):
        for k in reads:
            self.readers.setdefault(k, []).append(tok)
        for k in writes:
            self.lastw[k] = tok
            self.readers[k] = []

    def op(self, eng, fn, reads=(), writes=()):
        waits = self._deps(eng, reads, writes)
        idx = len(self.ins[eng])
        self.ins[eng].append(dict(fn=fn, waits=waits, sig=False, dma=None))
        self._commit(("e", eng, idx), reads, writes)

    def dma(self, fn, slot, reads=(), writes=(), group=False, queue="sp"):
        waits = self._deps(queue, reads, writes)
        self.dcount[slot] = self.dcount.get(slot, 0) + 1
        if group:
            self.group_slots.add(slot)
            waits = [w for w in waits if not (w[0][0] == "d" and w[0][1] == slot)]
        self.ins[queue].append(dict(fn=fn, waits=waits, sig=False, dma=slot))
        self._commit(("d", slot, self.dcount[slot]), reads, writes)

    def transfer(self, old_keys, new_keys):
        toks = []
        for k in old_keys:
            t = self.lastw.get(k)
            if t is not None:
                toks.append(t)
            toks.extend(self.readers.get(k, ()))
        best = {}
        for t in toks:
            s = (t[0], t[1])
            if best.get(s, -1) < t[2]:
                best[s] = t[2]
        toks = [(s[0], s[1], i) for s, i in best.items()]
        for k in new_keys:
            self.lastw[k] = None
            self.readers[k] = list(toks)

    def emit(self, nc, block, sems, dsems, final_waits):
        signo = {}
        for e in self.ENG:
            c = 0
            arr = []
            for it in self.ins[e]:
                if it["sig"]:
                    c += 1
                arr.append(c)
            signo[e] = arr

        def run(e, eng):
            for it in self.ins[e]:
                for src, idx in it["waits"]:
                    if src[0] == "e":
                        eng.wait_ge(sems[src[1]], signo[src[1]][idx])
                    else:
                        cnt = self.dcount[src[1]] if src[1] in self.group_slots else idx
                        eng.wait_ge(dsems[src[1]], 16 * cnt)
                r = getattr(eng, it["fn"][0])(**it["fn"][1])
                if it["dma"] is not None:
                    r.then_inc(dsems[it["dma"]], 16)
                elif it["sig"]:
                    r.then_inc(sems[e], 1)
            if e == "sp":
                for slot in final_waits:
                    if self.dcount.get(slot, 0):
                        eng.wait_ge(dsems[slot], 16 * self.dcount[slot])

        @block.tensor
        def _(eng):
            run("pe", eng)

        @block.scalar
        def _(eng):
            run("act", eng)

        @block.vector
        def _(eng):
            run("dve", eng)

        @block.gpsimd
        def _(eng):
            run("pool", eng)

        @block.sync
        def _(eng):
            run("sp", eng)


def build_nc(upto=99, dbg=False, nbr=NB):
    nc = bass.Bass("TRN2", target_bir_lowering=False)

    def din(name, shape):
        return nc.dram_tensor(name, list(shape), F32, kind="ExternalInput").ap()

    x = din("x", [NB, S, D])
    c = din("c", [NB, D])
    w_mod = din("w_mod", [D, 9 * D])
    b_mod = din("b_mod", [9 * D])
    g_l = [din("g_ffn1", [D]), din("g_mix", [D]), din("g_ffn2", [D])]
    ffn_w = [(din("ffn1_w1", [D, FF]), din("ffn1_w3", [D, FF]), din("ffn1_w2", [FF, D])),
             (din("ffn2_w1", [D, FF]), din("ffn2_w3", [D, FF]), din("ffn2_w2", [FF, D]))]
    w_in = din("w_in", [D, IN_W])
    q_norm_g = din("q_norm_g", [64])
    k_norm_g = din("k_norm_g", [64])
    gla_gate_up = din("gla_gate_up", [16, 512])
    gla_gate_bias = din("gla_gate_bias", [512])
    gla_out_norm_g = din("gla_out_norm_g", [256])
    w_branch_att = din("w_branch_att", [256, D])
    w_branch_gla = din("w_branch_gla", [D, D])
    w_out = din("w_out", [D, D])
    consts = din("consts", [P, C_TOT])
    y = nc.dram_tensor("y", [NB, S, D], F32, kind="ExternalOutput").ap()
    dbg_out = None
    if dbg:
        dbg_out = nc.dram_tensor("dbg", [4, P, DC * S], F32, kind="ExternalOutput").ap()

    pg = Prog()
    es = ExitStack()

    def sb(name, shape, dt):
        return es.enter_context(nc.sbuf_tensor(name, list(shape), dt))

    with es:
        h = sb("h", [P, DC, S], F32)
        u = sb("u", [P, DC, S], BF16)
        arena = sb("arena", [P, 28 * 1024], BF16)
        NST = 2
        stg = [sb(f"stg{i}", [P, 2048], F32) for i in range(NST)]
        NWB = 4
        wbs = [sb(f"wb{i}", [P, 2048], BF16) for i in range(NWB)]
        cst = sb("cst", [P, C_TOT], F32)
        NT32 = 4
        t32 = [sb(f"t32_{i}", [P, TS], F32) for i in range(NT32)]
        NT16 = 4
        t16 = [sb(f"t16_{i}", [P, TS], BF16) for i in range(NT16)]
        ident_bf = sb("ident_bf", [P, P], BF16)
        ones_bf = sb("ones_bf", [P, P], BF16)
        bd_bf = sb("bd_bf", [P, P], BF16)
        kst = sb("kst", [P, 8], F32)
        cT = sb("cT", [P, DC, NB], F32)
        cact = sb("cact", [P, DC, NB], BF16)
        bmodT = sb("bmodT", [P, 72], F32)
        modT = sb("modT", [P, 72, NB], F32)
        gT = sb("gT", [P, 3, DC], F32)
        Amod = sb("Amod", [P, 3, DC, NB], F32)
        Gmod = sb("Gmod", [P, 3, DC, NB], F32)
        qg = sb("qg", [P, 1], F32)
        kg = sb("kg", [P, 1], F32)
        negb = sb("negb", [P, 4], F32)
        gno = sb("gno", [P, 2], F32)
        gup = sb("gup", [80, 512], BF16)
        gdT = sb("gdT", [80, 2 * TS], BF16)
        Sst = sb("Sst", [P, 256], F32)
        Sbf = sb("Sbf", [P, 256], BF16)
        ebl = sb("ebl", [P, 16], F32)
        kdT = sb("kdT", [P, 2, P], BF16)
        kdtm = sb("kdtm", [P, 2, P], BF16)
        amk = sb("amk", [P, 2, P], BF16)
        psum = es.enter_context(nc.psum_tensor("ps", [P, 8 * TS], F32))
        psum16 = psum.bitcast(BF16) if hasattr(psum, "bitcast") else None

        sems = {e: es.enter_context(nc.semaphore(f"s_{e}")) for e in ("pe", "act", "dve", "pool")}
        dslots = ["pro", "stg0", "stg1", "dbg"]
        dsems = {s_: es.enter_context(nc.semaphore(f"d_{s_}")) for s_ in dslots}
        block = es.enter_context(nc.Block())

        cnt = dict(bank=0, t32=0, tL=0, t16=0, stg=0, wb=0, alt=0)

        def bank():
            i = cnt["bank"] % 8
            cnt["bank"] += 1
            return psum[:, i * TS:(i + 1) * TS], ("ps", i), i

        def tmp32():
            i = cnt["t32"] % 2
            cnt["t32"] += 1
            return t32[i], ("t32", i)

        def tmpL():
            i = 2 + cnt["tL"] % 2
            cnt["tL"] += 1
            return t32[i], ("t32", i)

        def tmp16():
            i = cnt["t16"] % NT16
            cnt["t16"] += 1
            return t16[i], ("t16", i)

        def stage():
            i = cnt["stg"] % NST
            cnt["stg"] += 1
            return stg[i], ("stg", i), f"stg{i}"

        def alt():
            cnt["alt"] += 1
            return "act" if cnt["alt"] % 2 else "dve"

        def copy_op(eng, out, in_, reads, writes):
            if eng == "act":
                pg.op("act", ("copy", dict(out=out, in_=in_)), reads, writes)
            else:
                pg.op(eng, ("tensor_copy", dict(out=out, in_=in_)), reads, writes)

        def load_w(src, a, b_):
            st, skey, sslot = stage()
            n = a * b_
            stv = st[:, 0:n].rearrange("p (a b) -> p a b", a=a)
            pg.dma(("dma_start", dict(out=stv, in_=src)), sslot, writes=[skey])
            i = cnt["wb"] % NWB
            cnt["wb"] += 1
            wv = wbs[i][:, 0:n].rearrange("p (a b) -> p a b", a=a)
            pg.op("pool", ("tensor_copy", dict(out=wv, in_=stv)), [skey], [("wb", i)])
            return wv, ("wb", i)

        def kc_view(w, c0, ncol):
            return w.rearrange("(kc p) f -> p kc f", p=P)[:, :, c0:c0 + ncol]

        def mm(out, lhsT, rhs, start, stop, reads, writes):
            pg.op("pe", ("matmul", dict(out=out, lhsT=lhsT, rhs=rhs, start=start, stop=stop)),
                  reads, writes)

        ukeys = [("u", t) for t in range(NTS)]

        def pro(out, in_, key, slow=False):
            if slow:
                pg.dma(("dma_start", dict(out=out, in_=in_, allow_slow_non_contiguous=True)), "pro",
                       writes=[key], group=True)
            else:
                pg.dma(("dma_start", dict(out=out, in_=in_)), "pro", writes=[key], group=True)

        pro(cst[:], consts, "cst")
        for b in range(NB):
            pro(cT[:, :, b], c[b].rearrange("(kc p) -> p kc", p=P), ("cT", b), slow=True)
        pro(bmodT[:], b_mod.rearrange("(n p) -> p n", p=P), "bmodT", slow=True)
        for l in range(3):
            pro(gT[:, l, :], g_l[l].rearrange("(n p) -> p n", p=P), ("gT", l), slow=True)
        for hh in range(2):
            pro(qg[hh * 64:(hh + 1) * 64, :], q_norm_g.rearrange("(p o) -> p o", o=1), ("qg", hh), slow=True)
            pro(kg[hh * 64:(hh + 1) * 64, :], k_norm_g.rearrange("(p o) -> p o", o=1), ("kg", hh), slow=True)
        pro(negb[:], gla_gate_bias.rearrange("(n p) -> p n", p=P), "negb", slow=True)
        pro(gno[:], gla_out_norm_g.rearrange("(n p) -> p n", p=P), "gno", slow=True)

        pg.op("dve", ("memset", dict(ap=ones_bf[:], constant=1.0)), [], ["ones"])
        pg.op("dve", ("memset", dict(ap=kst[:, 0:1], constant=EPS)), [], ["kst0"])
        pg.op("dve", ("memset", dict(ap=kst[:, 1:2], constant=1.0)), [], ["kst1"])
        pg.op("dve", ("memset", dict(ap=kst[:, 2:3], constant=math.log(128.0 ** -0.5))), [], ["kst2"])
        pg.op("dve", ("tensor_copy", dict(out=ident_bf[:], in_=cst[:, C_ID:C_ID + P])), ["cst"], ["identbf"])
        pg.op("dve", ("tensor_copy", dict(out=bd_bf[:], in_=cst[:, C_BD:C_BD + P])), ["cst"], ["bdbf"])
        for pb_ in (0, 32, 64):
            pro(t32[0][pb_:pb_ + 16, :], gla_gate_up, ("gupst", pb_))
        for pb_ in (0, 32, 64):
            pg.op("dve", ("tensor_copy", dict(out=gup[pb_:pb_ + 16, :], in_=t32[0][pb_:pb_ + 16, :])),
                  [("gupst", pb_)], ["gup", ("t32", 0)])
        pg.op("dve", ("tensor_scalar", dict(out=negb[:], in0=negb[:], scalar1=-1.0, scalar2=None, op0=ALU.mult)),
              ["negb"], ["negb"])
        pg.op("dve", ("tensor_scalar", dict(out=qg[:], in0=qg[:], scalar1=0.125, scalar2=None, op0=ALU.mult)),
              [("qg", 0), ("qg", 1)], [("qg", 0), ("qg", 1)])
        pg.op("act", ("activation", dict(out=cact[:], in_=cT[:], func=AF.Silu)), [("cT", 0), ("cT", 1)], ["cact"])
        ident = cst[:, C_ID:C_ID + P]
        stepsM = cst[:, C_STEP:C_STEP + 256]
        causM = cst[:, C_CAUS:C_CAUS + P]
        scanM = cst[:, C_SCAN:C_SCAN + P]

        bg = []

        def run_bg(n=1):
            for _ in range(n):
                if bg:
                    bg.pop(0)()

        def mod_tile(t):
            l = t // 12
            wv, wk = load_w(kc_view(w_mod, t * 256, 256), DC, 256)
            pm, pmk, _ = bank()
            for j in range(2):
                for kc in range(DC):
                    mm(pm[:, j * 2:j * 2 + 2], wv[:, kc, j * P:(j + 1) * P], cact[:, kc, :],
                       kc == 0, kc == DC - 1, [wk, "cact"], [pmk])
            for b in range(NB):
                pg.op("dve", ("tensor_tensor", dict(out=modT[:, 2 * t:2 * t + 2, b], in0=pm[:, b:4:2],
                                                    in1=bmodT[:, 2 * t:2 * t + 2], op=ALU.add)),
                      [pmk, "bmodT"], [("modT", l, b)])

        def mod_fin(l):
            coef = 1.0 if l == 1 else 0.5
            for b in range(NB):
                pg.op("dve", ("scalar_tensor_tensor", dict(
                    out=Amod[:, l, :, b], in0=modT[:, l * 24 + 8:l * 24 + 16, b], scalar=1.0, in1=gT[:, l, :],
                    op0=ALU.add, op1=ALU.mult)), [("modT", l, b), ("gT", l)], [("Amod", l, b)])
                pg.op("dve", ("tensor_scalar", dict(
                    out=Gmod[:, l, :, b], in0=modT[:, l * 24 + 16:l * 24 + 24, b], scalar1=1.0, scalar2=coef,
                    op0=ALU.add, op1=ALU.mult)), [("modT", l, b)], [("Gmod", l, b)])

        for t in range(12):
            mod_tile(t)
        mod_fin(0)
        for l in (1, 2):
            for t in range(12 * l, 12 * l + 12):
                bg.append(lambda t=t: mod_tile(t))
            bg.append(lambda l=l: mod_fin(l))

        def load_x(b):
            for tt in range(16):
                st, skey, sslot = stage()
                xs = st[:, 0:D]
                pg.dma(("dma_start", dict(out=xs, in_=x[b, tt * P:(tt + 1) * P, :])), sslot,
                       writes=[skey])
                for half in range(2):
                    pb, pk, _ = bank()
                    for j in range(4):
                        dc = half * 4 + j
                        pg.op("pe", ("transpose", dict(
                            out=pb[:, j * P:(j + 1) * P], in_=xs[:, dc * P:(dc + 1) * P], identity=ident)),
                            [skey, "cst"], [pk])
                    copy_op(alt(), h[:, half * 4:half * 4 + 4, tt * P:(tt + 1) * P],
                            pb.rearrange("p (a t) -> p a t", a=4), [pk], [("h", tt // 4)])

        def store_y(b):
            for tt in range(16):
                st, skey, sslot = stage()
                ys = st[:, 0:D]
                for half in range(2):
                    pb, pk, _ = bank()
                    for j in range(4):
                        dc = half * 4 + j
                        pg.op("pe", ("transpose", dict(
                            out=pb[:, j * P:(j + 1) * P], in_=h[:, dc, tt * P:(tt + 1) * P], identity=ident)),
                            [("h", tt // 4), "cst"], [pk])
                    copy_op(alt(), ys[:, half * 512:(half + 1) * 512], pb, [pk], [skey])
                pg.dma(("dma_start", dict(out=y[b, tt * P:(tt + 1) * P, :], in_=ys)), sslot,
                       reads=[skey])

        def rstd_from(pss, pssk, scale):
            r32, rk = tmpL()
            pg.op("act", ("activation", dict(out=r32[:], in_=pss, func=AF.Ln, bias=kst[:, 0:1], scale=scale)),
                  [pssk, "kst0"], [rk])
            pg.op("act", ("activation", dict(out=r32[:], in_=r32[:], func=AF.Exp, scale=-0.5)), [rk], [rk])
            return r32, rk

        SQ_ENG = ("pool", "dve", "pool", "act", "pool", "dve", "pool", "act")

        def norm_mod(l, b):
            def s1(ts):
                sl = slice(ts * TS, (ts + 1) * TS)
                pss, pssk, _ = bank()
                for dc in range(DC):
                    sq, sqk = tmp16()
                    e_ = SQ_ENG[dc]
                    if e_ == "act":
                        pg.op("act", ("activation", dict(out=sq[:], in_=h[:, dc, sl], func=AF.Square)), [("h", ts)], [sqk])
                    else:
                        pg.op(e_, ("tensor_tensor", dict(out=sq[:], in0=h[:, dc, sl], in1=h[:, dc, sl], op=ALU.mult)),
                              [("h", ts)], [sqk])
                    mm(pss, ones_bf[:], sq[:], dc == 0, dc == DC - 1, [sqk, "ones"], [pssk])
                return rstd_from(pss, pssk, 1.0 / D)

            def s2(ts, r32, rk):
                sl = slice(ts * TS, (ts + 1) * TS)
                for dc in range(DC):
                    tt_, tk = tmp32()
                    pg.op("dve", ("tensor_tensor", dict(out=tt_[:], in0=h[:, dc, sl], in1=r32[:], op=ALU.mult)),
                          [("h", ts), rk], [tk])
                    pg.op("act", ("activation", dict(
                        out=u[:, dc, sl], in_=tt_[:], func=AF.Identity, bias=modT[:, l * 24 + dc, b:b + 1],
                        scale=Amod[:, l, dc, b:b + 1])), [tk, ("modT", l, b), ("Amod", l, b)], [("u", ts)])

            rs = {0: s1(0)}
            for ts in range(NTS):
                if ts + 1 < NTS:
                    rs[ts + 1] = s1(ts + 1)
                s2(ts, *rs.pop(ts))

        def gkey(f, ts):
            return ("g", f, ts)

        def ffn(fi, l, b):
            w1, w3, w2 = ffn_w[fi]
            gbuf = arena[:, 0:12 * S].rearrange("p (f t) -> p f t", f=12)
            for (f0, nf) in ((0, 12), (12, 10)):
                for tl in range(nf // 2):
                    c0 = (f0 + tl * 2) * P
                    w1v, w1k = load_w(kc_view(w1, c0, 256), DC, 256)
                    w3v, w3k = load_w(kc_view(w3, c0, 256), DC, 256)
                    for j in range(2):
                        fl = tl * 2 + j
                        for ts in range(NTS):
                            sl = slice(ts * TS, (ts + 1) * TS)
                            pa, pak, _ = bank()
                            pb, pbk, _ = bank()
                            for kc in range(DC):
                                mm(pa, w1v[:, kc, j * P:(j + 1) * P], u[:, kc, sl], kc == 0, kc == DC - 1,
                                   [w1k, ("u", ts)], [pak])
                            for kc in range(DC):
                                mm(pb, w3v[:, kc, j * P:(j + 1) * P], u[:, kc, sl], kc == 0, kc == DC - 1,
                                   [w3k, ("u", ts)], [pbk])
                            s1, s1k = tmp16()
                            pg.op("act", ("activation", dict(out=s1[:], in_=pa, func=AF.Silu)),
                                  [pak], [s1k])
                            pg.op("dve", ("tensor_tensor", dict(
                                out=gbuf[:, fl, sl], in0=pb, in1=s1[:], op=ALU.mult)), [pbk, s1k], [gkey(fl, ts)])
                    run_bg()
                w2v_all = w2.rearrange("(fc p) d -> p fc d", p=P)
                for dc in range(DC):
                    w2v, w2k = load_w(w2v_all[:, f0:f0 + nf, dc * P:(dc + 1) * P], nf, P)
                    for ts in range(NTS):
                        sl = slice(ts * TS, (ts + 1) * TS)
                        po, pok, _ = bank()
                        for fl in range(nf):
                            mm(po, w2v[:, fl, :], gbuf[:, fl, sl], fl == 0, fl == nf - 1, [w2k, gkey(fl, ts)], [pok])
                        pg.op("dve", ("scalar_tensor_tensor", dict(
                            out=h[:, dc, sl], in0=po, scalar=Gmod[:, l, dc, b:b + 1], in1=h[:, dc, sl],
                            op0=ALU.mult, op1=ALU.add)), [pok, ("Gmod", l, b), ("h", ts)], [("h", ts)])
                    run_bg()

        G_KEYS = [gkey(f, t) for f in range(12) for t in range(NTS)]

        KB = 512
        acc = arena[:, 0:32 * KB].bitcast(F32).rearrange("p (c o t) -> p c o t", c=2, o=2)
        ogla = arena[:, 0:32 * KB].rearrange("p (c t) -> p c t", c=8)
        qbuf = arena[:, 32 * KB:40 * KB].rearrange("p (c t) -> p c t", c=2)
        kbuf = arena[:, 40 * KB:48 * KB].rearrange("p (c t) -> p c t", c=2)
        vtm = arena[:, 48 * KB:56 * KB].rearrange("p (b f) -> p b f", b=16)
        oatt = arena[:, 32 * KB:40 * KB].rearrange("p (c t) -> p c t", c=2)
        gvtm = arena[:, 40 * KB:48 * KB].rearrange("p (b f) -> p b f", b=16)
        qdec = arena[:, 48 * KB:52 * KB]
        kinv = arena[:, 52 * KB:56 * KB]
        merged = arena[:, 40 * KB:56 * KB].rearrange("p (c t) -> p c t", c=8)

        ACC_KEYS = [("acc", ch, t) for ch in range(2) for t in range(NTS)]
        Q_KEYS = [("q", ch, t) for ch in range(2) for t in range(NTS)]
        K_KEYS = [("k", ch, t) for ch in range(2) for t in range(NTS)]
        V_KEYS = [("v", tb) for tb in range(16)]
        OATT_KEYS = [("oatt", t) for t in range(NTS)]
        OGLA_KEYS = [("ogla", hh, t) for hh in range(4) for t in range(NTS)]
        GV_KEYS = [("gv", tb) for tb in range(16)]
        QD_KEYS = [("qd", t) for t in range(NTS)]
        KI_KEYS = [("ki", t) for t in range(NTS)]
        KD_KEYS = []
        MG_KEYS = [("mg", dc, t) for dc in range(DC) for t in range(2)]

        def tok_slice(dil, r, n):
            st0 = r + dil * P * n
            return slice(st0, st0 + dil * (P - 1) + 1, dil)

        def blk_ts(dil, r, n):
            if dil == 1:
                return [n // 4]
            return list(range(NTS)) if dil == 16 else [n]

        def attention(b):
            for gi, (win, dil) in enumerate(ATT_GROUPS):
                nb = S // dil // P
                wq_, wqk_ = load_w(kc_view(w_in, OFF_AQ + gi * 256, 256), DC, 256)
                wk_, wkk_ = load_w(kc_view(w_in, OFF_AK + gi * 256, 256), DC, 256)
                items = [(wq_, wqk_, qbuf, qg, "q", ch, ts) for ch in range(2) for ts in range(NTS)] + \
                        [(wk_, wkk_, kbuf, kg, "k", ch, ts) for ch in range(2) for ts in range(NTS)]

                def qk_s1(it):
                    wv, wk, buf, gain, nm, ch, ts = it
                    sl = slice(ts * TS, (ts + 1) * TS)
                    pq, pqk, _ = bank()
                    for kc in range(DC):
                        mm(pq, wv[:, kc, ch * P:(ch + 1) * P], u[:, kc, sl], kc == 0, kc == DC - 1,
                           [wk, ("u", ts)], [pqk])
                    sq, sqk = tmp16()
                    pg.op("act", ("activation", dict(out=sq[:], in_=pq, func=AF.Square)), [pqk], [sqk])
                    return (pq, pqk, sq, sqk)

                def qk_s2(it, ctx):
                    wv, wk, buf, gain, nm, ch, ts = it
                    pq, pqk, sq, sqk = ctx
                    sl = slice(ts * TS, (ts + 1) * TS)
                    pss, pssk, _ = bank()
                    mm(pss, bd_bf[:], sq[:], True, True, [sqk, "bdbf"], [pssk])
                    r32, rk = rstd_from(pss, pssk, 1.0 / 64)
                    pg.op("dve", ("scalar_tensor_tensor", dict(out=buf[:, ch, sl], in0=pq, scalar=gain[:, 0:1], in1=r32[:],
                                                               op0=ALU.mult, op1=ALU.mult)),
                          [pqk, rk, (nm + "g", 0), (nm + "g", 1)], [(nm, ch, ts)])

                ctxs = {0: qk_s1(items[0])}
                for i in range(len(items)):
                    if i + 1 < len(items):
                        ctxs[i + 1] = qk_s1(items[i + 1])
                    qk_s2(items[i], ctxs.pop(i))
                wv, wk = load_w(kc_view(w_in, OFF_AV + gi * 256, 256), DC, 256)
                blocks = [(r, n) for r in range(dil) for n in range(nb)]
                for tb, (r, n) in enumerate(blocks):
                    tsl = tok_slice(dil, r, n)
                    pv, pvk, _ = bank()
                    for kc in range(DC):
                        mm(pv[:, 0:256], u[:, kc, tsl], wv[:, kc, :], kc == 0, kc == DC - 1,
                           [wk] + [("u", t) for t in blk_ts(dil, r, n)], [pvk])
                    copy_op(alt(), vtm[:, tb, :], pv[:, 0:256], [pvk], [("v", tb)])
                work = [(ch, tb) for ch in range(2) for tb in range(len(blocks))]
                pend = {}

                def emit_qk(ch, tb):
                    r, n = blocks[tb]
                    tsl = tok_slice(dil, r, n)
                    tss = blk_ts(dil, r, n)
                    Es = []
                    for hh in range(2):
                        head = gi * 4 + ch * 2 + hh
                        cs = -SLOPES[head] * dil
                        ps_, psk, _ = bank()
                        prt = slice(hh * 64, (hh + 1) * 64)
                        rd = [("q", ch, t) for t in tss] + [("k", ch, t) for t in tss]
                        mm(ps_[:, 128:256], kbuf[prt, ch, tsl], qbuf[prt, ch, tsl], True, True, rd, [psk])
                        lo = 128
                        if n > 0:
                            psl = tok_slice(dil, r, n - 1)
                            rd2 = rd + [("k", ch, t) for t in blk_ts(dil, r, n - 1)]
                            mm(ps_[:, 0:128], kbuf[prt, ch, psl], qbuf[prt, ch, tsl], True, True, rd2, [psk])
                            lo = 0
                        t_, tk = tmp32()
                        pg.op("dve", ("scalar_tensor_tensor", dict(
                            out=t_[:, lo:256], in0=stepsM[:, lo:256], scalar=cs, in1=ps_[:, lo:256],
                            op0=ALU.mult, op1=ALU.add)), [psk, "cst"], [tk])
                        E, Ek = tmp16()
                        pg.op("act", ("activation", dict(out=E[:, lo:256], in_=t_[:, lo:256],
                                                                               func=AF.Exp)), [tk], [Ek])
                        Es.append((E, Ek, lo))
                    pend[(ch, tb)] = Es

                def emit_pv(ch, tb):
                    r, n = blocks[tb]
                    tsl = tok_slice(dil, r, n)
                    tss = blk_ts(dil, r, n)
                    Es = pend.pop((ch, tb))
                    po, pok, _ = bank()
                    for hh in range(2):
                        E, Ek, lo = Es[hh]
                        prt = slice(hh * 64, (hh + 1) * 64)
                        vc = slice(ch * P + hh * 64, ch * P + hh * 64 + 64)
                        mm(po[prt, 0:128], vtm[:, tb, vc], E[:, 128:256], True, n == 0, [Ek, ("v", tb)], [pok])
                        if n > 0:
                            mm(po[prt, 0:128], vtm[:, tb - 1, vc], E[:, 0:128], False, True, [Ek, ("v", tb - 1)], [pok])
                        mm(po[prt, 128:256], ones_bf[:, 0:64], E[:, 128:256], True, n == 0, [Ek, "ones"], [pok])
                        if n > 0:
                            mm(po[prt, 128:256], ones_bf[:, 0:64], E[:, 0:128], False, True, [Ek, "ones"], [pok])
                    pov = po[:, 0:256].rearrange("p (o t) -> p o t", o=2)
                    akeys = [("acc", ch, t) for t in tss]
                    if gi == 0:
                        pg.op("dve", ("tensor_copy", dict(out=acc[:, ch, :, tsl], in_=pov)), [pok], akeys)
                    else:
                        pg.op("dve", ("tensor_tensor", dict(out=acc[:, ch, :, tsl], in0=acc[:, ch, :, tsl], in1=pov,
                                                               op=ALU.add)), [pok] + akeys, akeys)

                emit_qk(*work[0])
                for i in range(len(work)):
                    if i + 1 < len(work):
                        emit_qk(*work[i + 1])
                    emit_pv(*work[i])
            pg.transfer(Q_KEYS, OATT_KEYS)
            for ch in range(2):
                for ts in range(NTS):
                    sl = slice(ts * TS, (ts + 1) * TS)
                    r_, rk = tmp32()
                    pg.op("dve", ("reciprocal", dict(out=r_[:], in_=acc[:, ch, 1, sl])),
                          [("acc", ch, ts)], [rk])
                    pg.op("dve", ("tensor_tensor", dict(out=oatt[:, ch, sl], in0=acc[:, ch, 0, sl],
                                                                              in1=r_[:], op=ALU.mult)),
                          [("acc", ch, ts), rk], [("oatt", ts)])

        def gd_pb(ts):
            return 0 if ts == 3 else 32 * ts

        def gd_ap(ts):
            c0 = TS if ts == 3 else 0
            return gdT[gd_pb(ts):gd_pb(ts) + 16, c0:c0 + TS]

        def gla(b):
            wv, wk = load_w(kc_view(w_in, OFF_GD, 16), DC, 16)
            for ts in range(NTS):
                sl = slice(ts * TS, (ts + 1) * TS)
                pd, pdk, _ = bank()
                for kc in range(DC):
                    mm(pd[gd_pb(ts):gd_pb(ts) + 16, :], wv[:, kc, :], u[:, kc, sl], kc == 0, kc == DC - 1, [wk, ("u", ts)], [pdk])
                copy_op("act", gd_ap(ts), pd[gd_pb(ts):gd_pb(ts) + 16, :], [pdk], [("gd", ts)])

            def outnorm_units(hh):
                units = []
                st_ = {}

                def load():
                    st_["w"] = load_w(kc_view(w_in, OFF_GR + hh * 256, 256), DC, 256)
                units.append(load)
                for ts in range(NTS):
                    sl = slice(ts * TS, (ts + 1) * TS)

                    def u_ss(ts=ts, sl=sl):
                        pss, pssk, _ = bank()
                        for dv in range(2):
                            sq, sqk = tmp16()
                            pg.op("pool", ("tensor_tensor", dict(
                                out=sq[:], in0=ogla[:, hh * 2 + dv, sl], in1=ogla[:, hh * 2 + dv, sl], op=ALU.mult)),
                                [("ogla", hh, ts)], [sqk])
                            mm(pss, ones_bf[:], sq[:], dv == 0, dv == 1, [sqk, "ones"], [pssk])
                        st_[ts] = rstd_from(pss, pssk, 1.0 / 256)
                    units.append(u_ss)
                    for dv in range(2):
                        def u_dv(ts=ts, sl=sl, dv=dv):
                            wv_, wk_ = st_["w"]
                            r32, rk = st_[ts]
                            pr, prk, _ = bank()
                            for kc in range(DC):
                                mm(pr, wv_[:, kc, dv * P:(dv + 1) * P], u[:, kc, sl], kc == 0, kc == DC - 1,
                                   [wk_, ("u", ts)], [prk])
                            sg, sgk = tmp32()
                            pg.op("act", ("activation", dict(out=sg[:], in_=pr, func=AF.Silu)), [prk], [sgk])
                            t1, t1k = tmp32()
                            pg.op("dve", ("scalar_tensor_tensor", dict(
                                out=t1[:], in0=ogla[:, hh * 2 + dv, sl], scalar=gno[:, dv:dv + 1], in1=r32[:],
                                op0=ALU.mult, op1=ALU.mult)), [("ogla", hh, ts), rk, "gno"], [t1k])
                            pg.op("dve", ("tensor_tensor", dict(
                                out=ogla[:, hh * 2 + dv, sl], in0=t1[:], in1=sg[:], op=ALU.mult)),
                                [t1k, sgk, ("ogla", hh, ts)], [("ogla", hh, ts)])
                        units.append(u_dv)
                return units

            pending = []
            for hh in range(4):
                wq, wqk = load_w(kc_view(w_in, OFF_GQ + hh * P, P), DC, P)
                wkk, wkkk = load_w(kc_view(w_in, OFF_GK + hh * P, P), DC, P)
                wvv, wvk = load_w(kc_view(w_in, OFF_GV + hh * 256, 256), DC, 256)
                for ts in range(NTS):
                    sl = slice(ts * TS, (ts + 1) * TS)
                    px, pxk, _ = bank()
                    mm(px, gup[gd_pb(ts):gd_pb(ts) + 16, hh * P:(hh + 1) * P], gd_ap(ts), True, True, ["gup", ("gd", ts)], [pxk])
                    e_, ek = tmp32()
                    pg.op("act", ("activation", dict(out=e_[:], in_=px, func=AF.Exp, bias=negb[:, hh:hh + 1], scale=-1.0)),
                          [pxk, "negb"], [ek])
                    sp_, spk = tmp32()
                    pg.op("act", ("activation", dict(out=sp_[:], in_=e_[:], func=AF.Ln, bias=kst[:, 1:2], scale=1.0)),
                          [ek, "kst1"], [spk])
                    B_, Bk = tmpL()
                    bks = [(Bk, c4) for c4 in range(4)]
                    for c4 in range(4):
                        pg.op("dve", ("tensor_tensor_scan", dict(
                            out=B_[:, c4 * P:(c4 + 1) * P], data0=scanM, data1=sp_[:, c4 * P:(c4 + 1) * P], initial=0.0,
                            op0=ALU.mult, op1=ALU.add)), [spk, "cst"], [bks[c4]])
                    pq, pqk, _ = bank()
                    for kc in range(DC):
                        mm(pq, wq[:, kc, :], u[:, kc, sl], kc == 0, kc == DC - 1, [wqk, ("u", ts)], [pqk])
                    pk_, pkk, _ = bank()
                    for kc in range(DC):
                        mm(pk_, wkk[:, kc, :], u[:, kc, sl], kc == 0, kc == DC - 1, [wkkk, ("u", ts)], [pkk])
                    pg.op("act", ("activation", dict(
                        out=ebl[:, ts * 4:ts * 4 + 4], in_=B_[:, 127:512:128], func=AF.Exp, scale=-1.0 / 16)),
                        bks, [("ebl", ts)])
                    eb, ebk = tmp32()
                    pg.op("act", ("activation", dict(out=eb[:], in_=B_[:], func=AF.Exp, bias=kst[:, 2:3], scale=-1.0 / 16)),
                          bks + ["kst2"], [ebk])
                    pg.op("dve", ("tensor_tensor", dict(out=qdec[:, sl], in0=pq, in1=eb[:], op=ALU.mult)),
                          [pqk, ebk], [("qd", ts)])
                    en, enk = tmp32()
                    pg.op("act", ("activation", dict(out=en[:], in_=B_[:], func=AF.Exp, scale=1.0 / 16)), bks, [enk])
                    pg.op("dve", ("tensor_tensor", dict(out=kinv[:, sl], in0=pk_, in1=en[:], op=ALU.mult)),
                          [pkk, enk], [("ki", ts)])
                    for tb in range(ts * 4, ts * 4 + 4):
                        pv, pvk, _ = bank()
                        for kc in range(DC):
                            mm(pv[:, 0:256], u[:, kc, tb * P:(tb + 1) * P], wvv[:, kc, :], kc == 0, kc == DC - 1,
                               [wvk, ("u", tb // 4)], [pvk])
                        copy_op(alt(), gvtm[:, tb, :], pv[:, 0:256], [pvk], [("gv", tb)])

                def emit_kd(cc):
                    csl = slice(cc * P, (cc + 1) * P)
                    j = cc % 2
                    pg.op("dve", ("tensor_scalar", dict(
                        out=kdT[:, j, :], in0=kinv[:, csl], scalar1=ebl[:, cc:cc + 1], scalar2=None, op0=ALU.mult)),
                        [("ki", cc // 4), ("ebl", cc // 4)], [("kdT", j)])
                    pt, ptk, bi = bank()
                    pt16 = psum16[:, bi * 2 * TS:bi * 2 * TS + P]
                    pg.op("pe", ("transpose", dict(out=pt16, in_=kdT[:, j, :], identity=ident_bf[:])),
                          [("kdT", j), "identbf"], [ptk])
                    copy_op("act", kdtm[:, j, :], pt16, [ptk], [("kd", j)])
                emit_kd(0)
                for cc in range(16):
                    csl = slice(cc * P, (cc + 1) * P)
                    ts = cc // 4
                    pa, pak, _ = bank()
                    mm(pa[:, 0:P], kinv[:, csl], qdec[:, csl], True, True, [("ki", ts), ("qd", ts)], [pak])
                    j = cc % 2
                    if cc + 1 < 15:
                        emit_kd(cc + 1)
                    pg.op("dve", ("tensor_tensor", dict(out=amk[:, j, :], in0=pa[:, 0:P], in1=causM, op=ALU.mult)),
                          [pak, "cst"], [("amk", j)])
                    if cc < 15:
                        pu, puk, _ = bank()
                        mm(pu[:, 0:256], kdtm[:, j, :], gvtm[:, cc, :], True, True, [("kd", j), ("gv", cc)], [puk])
                    po, pok, _ = bank()
                    for dv in range(2):
                        mm(po[:, dv * P:(dv + 1) * P], gvtm[:, cc, dv * P:(dv + 1) * P], amk[:, j, :], True, cc == 0,
                           [("gv", cc), ("amk", j)], [pok])
                        if cc > 0:
                            mm(po[:, dv * P:(dv + 1) * P], Sbf[:, dv * P:(dv + 1) * P], qdec[:, csl], False, True,
                               ["Sbf", ("qd", ts)], [pok])
                    copy_op("act", ogla[:, hh * 2:hh * 2 + 2, csl], po[:, 0:256].rearrange("p (a t) -> p a t", a=2),
                            [pok], [("ogla", hh, ts)])
                    if cc < 15:
                        if cc == 0:
                            pg.op("dve", ("tensor_copy", dict(out=Sst[:], in_=pu[:, 0:256])), [puk], ["Sst"])
                        else:
                            pg.op("dve", ("scalar_tensor_tensor", dict(
                                out=Sst[:], in0=Sst[:], scalar=ebl[:, cc:cc + 1], in1=pu[:, 0:256],
                                op0=ALU.mult, op1=ALU.add)), [puk, "Sst", ("ebl", ts)], ["Sst"])
                        copy_op("act", Sbf[:], Sst[:], ["Sst"], ["Sbf"])
                    if pending:
                        pending.pop(0)()
                while pending:
                    pending.pop(0)()
                pending = outnorm_units(hh)
            while pending:
                pending.pop(0)()

        def merge_out(b):
            wba_all = w_branch_att.rearrange("(kc p) d -> p kc d", p=P)
            for th in range(2):
                for dp in range(4):
                    c0 = dp * 256
                    wga, wgak = load_w(kc_view(w_in, OFF_GA + c0, 256), DC, 256)
                    wba, wbak = load_w(wba_all[:, :, c0:c0 + 256], 2, 256)
                    for j in range(2):
                        dc = dp * 2 + j
                        for t2 in range(2):
                            ts = th * 2 + t2
                            sl = slice(ts * TS, (ts + 1) * TS)
                            p1, p1k, _ = bank()
                            for kc in range(DC):
                                mm(p1, wga[:, kc, j * P:(j + 1) * P], u[:, kc, sl], kc == 0, kc == DC - 1,
                                   [wgak, ("u", ts)], [p1k])
                            sa, sak = tmp32()
                            pg.op("act", ("activation", dict(out=sa[:], in_=p1, func=AF.Sigmoid)),
                                  [p1k], [sak])
                            p2, p2k, _ = bank()
                            for c2 in range(2):
                                mm(p2, wba[:, c2, j * P:(j + 1) * P], oatt[:, c2, sl], c2 == 0, c2 == 1,
                                   [wbak, ("oatt", ts)], [p2k])
                            pg.op("dve", ("tensor_tensor", dict(
                                out=merged[:, dc, t2 * TS:(t2 + 1) * TS], in0=p2, in1=sa[:], op=ALU.mult)),
                                [p2k, sak], [("mg", dc, t2)])
                    wgg, wggk = load_w(kc_view(w_in, OFF_GG + c0, 256), DC, 256)
                    wbg, wbgk = load_w(kc_view(w_branch_gla, c0, 256), DC, 256)
                    for j in range(2):
                        dc = dp * 2 + j
                        for t2 in range(2):
                            ts = th * 2 + t2
                            sl = slice(ts * TS, (ts + 1) * TS)
                            p1, p1k, _ = bank()
                            for kc in range(DC):
                                mm(p1, wgg[:, kc, j * P:(j + 1) * P], u[:, kc, sl], kc == 0, kc == DC - 1,
                                   [wggk, ("u", ts)], [p1k])
                            sa, sak = tmp32()
                            pg.op("act", ("activation", dict(out=sa[:], in_=p1, func=AF.Sigmoid)),
                                  [p1k], [sak])
                            p2, p2k, _ = bank()
                            for kc in range(DC):
                                mm(p2, wbg[:, kc, j * P:(j + 1) * P], ogla[:, kc, sl], kc == 0, kc == DC - 1,
                                   [wbgk, ("ogla", kc // 2, ts)], [p2k])
                            m2, m2k = tmp32()
                            pg.op("dve", ("tensor_tensor", dict(out=m2[:], in0=p2, in1=sa[:],
                                                                                         op=ALU.mult)), [p2k, sak], [m2k])
                            pg.op("dve", ("tensor_tensor", dict(
                                out=merged[:, dc, t2 * TS:(t2 + 1) * TS], in0=merged[:, dc, t2 * TS:(t2 + 1) * TS],
                                in1=m2[:], op=ALU.add)), [m2k, ("mg", dc, t2)], [("mg", dc, t2)])
                for dp in range(4):
                    wo, wok = load_w(kc_view(w_out, dp * 256, 256), DC, 256)
                    for j in range(2):
                        dc = dp * 2 + j
                        for t2 in range(2):
                            ts = th * 2 + t2
                            sl = slice(ts * TS, (ts + 1) * TS)
                            po, pok, _ = bank()
                            for kc in range(DC):
                                mm(po, wo[:, kc, j * P:(j + 1) * P], merged[:, kc, t2 * TS:(t2 + 1) * TS], kc == 0,
                                   kc == DC - 1, [wok, ("mg", kc, t2)], [pok])
                            pg.op("dve", ("scalar_tensor_tensor", dict(
                                out=h[:, dc, sl], in0=po, scalar=Gmod[:, 1, dc, b:b + 1], in1=h[:, dc, sl],
                                op0=ALU.mult, op1=ALU.add)), [pok, ("Gmod", 1, b), ("h", ts)], [("h", ts)])

        def dump(slot_i, b):
            if dbg and b == 0:
                pg.dma(("dma_start", dict(out=dbg_out[slot_i].rearrange("p (c t) -> p c t", c=DC), in_=h[:])), "dbg",
                       reads=[("h", t) for t in range(NTS)])

        MIX_A = ACC_KEYS + Q_KEYS + K_KEYS + V_KEYS
        for b in range(nbr):
            load_x(b)
            norm_mod(0, b)
            if upto >= 1:
                ffn(0, 0, b)
            dump(0, b)
            run_bg(len(bg))
            if upto >= 2:
                norm_mod(1, b)
                pg.transfer(G_KEYS, MIX_A + KD_KEYS)
                attention(b)
                pg.transfer(ACC_KEYS, OGLA_KEYS)
                pg.transfer(K_KEYS, GV_KEYS)
                pg.transfer(V_KEYS, QD_KEYS + KI_KEYS)
                gla(b)
                pg.transfer(GV_KEYS + QD_KEYS + KI_KEYS, MG_KEYS)
                merge_out(b)
                dump(1, b)
            if upto >= 3:
                norm_mod(2, b)
                pg.transfer(OGLA_KEYS + OATT_KEYS + MG_KEYS + KD_KEYS + MIX_A + GV_KEYS + QD_KEYS + KI_KEYS, G_KEYS)
                ffn(1, 2, b)
            elif upto >= 2:
                pg.transfer(OGLA_KEYS + OATT_KEYS + MG_KEYS + KD_KEYS + MIX_A + GV_KEYS + QD_KEYS + KI_KEYS, G_KEYS)
            store_y(b)

        pg.emit(nc, block, sems, dsems, final_waits=["stg0", "stg1", "dbg"])
    return nc


def make_consts():
    cs = np.zeros((P, C_TOT), np.float32)
    cs[:, C_ID:C_ID + P] = np.eye(P, dtype=np.float32)
    kk = np.arange(P)[:, None]
    qq = np.arange(P)[None, :]
    BIG = 1.0e4
    prev = np.where(qq <= kk, (qq + P - kk).astype(np.float32), BIG)
    cur = np.where(qq >= kk, (qq - kk).astype(np.float32), BIG)
    cs[:, C_STEP:C_STEP + P] = prev
    cs[:, C_STEP + P:C_STEP + 2 * P] = cur
    cs[:, C_CAUS:C_CAUS + P] = (kk <= qq).astype(np.float32)
    cs[:, C_SCAN:C_SCAN + P] = 1.0
    bd = np.zeros((P, P), np.float32)
    bd[:64, :64] = 1.0
    bd[64:, 64:] = 1.0
    cs[:, C_BD:C_BD + P] = bd
    return cs


_NC_CACHE = {}


def _run(inputs, upto=99, dbg=False, ncores=8):
    key = (upto, dbg)
    if key not in _NC_CACHE:
        _NC_CACHE[key] = build_nc(upto, dbg)
    nc = _NC_CACHE[key]
    f = lambda a: np.ascontiguousarray(np.asarray(a, dtype=np.float32))
    sq = lambda a: f(a)[0]
    shared = {
        "w_mod": sq(inputs["w_mod"]), "b_mod": sq(inputs["b_mod"]),
        "g_ffn1": sq(inputs["g_ffn1"]), "g_mix": sq(inputs["g_mix"]), "g_ffn2": sq(inputs["g_ffn2"]),
        "ffn1_w1": sq(inputs["ffn1_w1"]), "ffn1_w3": sq(inputs["ffn1_w3"]), "ffn1_w2": sq(inputs["ffn1_w2"]),
        "ffn2_w1": sq(inputs["ffn2_w1"]), "ffn2_w3": sq(inputs["ffn2_w3"]), "ffn2_w2": sq(inputs["ffn2_w2"]),
        "w_in": sq(inputs["w_in"]), "q_norm_g": sq(inputs["q_norm_g"]), "k_norm_g": sq(inputs["k_norm_g"]),
        "gla_gate_up": sq(inputs["gla_gate_up"]), "gla_gate_bias": sq(inputs["gla_gate_bias"]),
        "gla_out_norm_g": sq(inputs["gla_out_norm_g"]), "w_branch_att": sq(inputs["w_branch_att"]),
        "w_branch_gla": sq(inputs["w_branch_gla"]), "w_out": sq(inputs["w_out"]),
        "consts": make_consts(),
    }
    xf = f(inputs["x"])
    cf = f(inputs["c"])
    in_maps = []
    for i in range(ncores):
        m = dict(shared)
        m["x"] = np.ascontiguousarray(xf[i * NB:(i + 1) * NB])
        m["c"] = np.ascontiguousarray(cf[i * NB:(i + 1) * NB])
        in_maps.append(m)
    res = run_bass_kernel_spmd(nc, in_maps, core_ids=list(range(ncores)))
    return res


def kernel(**inputs):
    res = _run(inputs)
    return np.concatenate([np.asarray(r["y"], dtype=np.float32) for r in res.results], axis=0)
```

```python
import math
from contextlib import ExitStack
import numpy as np
import concourse.bass as bass
import concourse.mybir as mybir
from concourse.bass_utils import run_bass_kernel_spmd

F32 = mybir.dt.float32
BF16 = mybir.dt.bfloat16
AF = mybir.ActivationFunctionType
ALU = mybir.AluOpType

P = 128
S = 2048
D = 1024
DC = 8
NTS = 4
TS = 512
FF = 2816
FC = 22
NB = 2
EPS = 1e-6
ATT_GROUPS = ((128, 1), (512, 4), (2048, 16))
OFF_AQ, OFF_AK, OFF_AV = 0, 768, 1536
OFF_GQ, OFF_GK, OFF_GV, OFF_GR, OFF_GD, OFF_GA, OFF_GG = 2304, 2816, 3328, 4352, 5376, 5392, 6416
IN_W = 7440
SLOPES = [2.0 ** (-8.0 * (i + 1) / 12.0) for i in range(12)]
C_ID, C_STEP, C_CAUS, C_BD = 0, 128, 384, 512
C_TOT = 640


class Prog:
    ENG = ("pe", "act", "dve", "pool", "sp")

    def __init__(self):
        self.ins = {e: [] for e in self.ENG}
        self.lastw = {}
        self.readers = {}
        self.waited = {e: {} for e in self.ENG}
        self.dcount = {}
        self.group_slots = set()

    def _deps(self, eng, reads, writes):
        toks = []
        for k in reads:
            t = self.lastw.get(k)
            if t is not None:
                toks.append(t)
        for k in writes:
            t = self.lastw.get(k)
            if t is not None:
                toks.append(t)
            toks.extend(self.readers.get(k, ()))
        need = {}
        for t in toks:
            src = (t[0], t[1])
            if t[0] == "e" and t[1] == eng and eng in ("pe", "sp"):
                continue
            if need.get(src, -1) < t[2]:
                need[src] = t[2]
        waits = []
        wd = self.waited[eng]
        for src, idx in need.items():
            if wd.get(src, -1) >= idx:
                continue
            wd[src] = idx
            if src[0] == "e":
                self.ins[src[1]][idx]["sig"] = True
            waits.append((src, idx))
        return waits

    def _commit(self, tok, reads, writes):
        for k in reads:
            self.readers.setdefault(k, []).append(tok)
        for k in writes:
            self.lastw[k] = tok
            self.readers[k] = []

    def op(self, eng, fn, reads=(), writes=()):
        waits = self._deps(eng, reads, writes)
        idx = len(self.ins[eng])
        self.ins[eng].append(dict(fn=fn, waits=waits, sig=False, dma=None))
        self._commit(("e", eng, idx), reads, writes)

    def dma(self, fn, slot, reads=(), writes=(), group=False, queue="sp"):
        waits = self._deps(queue, reads, writes)
        self.dcount[slot] = self.dcount.get(slot, 0) + 1
        if group:
            self.group_slots.add(slot)
            waits = [w for w in waits if not (w[0][0] == "d" and w[0][1] == slot)]
        self.ins[queue].append(dict(fn=fn, waits=waits, sig=False, dma=slot))
        self._commit(("d", slot, self.dcount[slot]), reads, writes)

    def transfer(self, old_keys, new_keys):
        toks = []
        for k in old_keys:
            t = self.lastw.get(k)
            if t is not None:
                toks.append(t)
            toks.extend(self.readers.get(k, ()))
        best = {}
        for t in toks:
            s = (t[0], t[1])
            if best.get(s, -1) < t[2]:
                best[s] = t[2]
        toks = [(s[0], s[1], i) for s, i in best.items()]
        for k in new_keys:
            self.lastw[k] = None
            self.readers[k] = list(toks)

    def emit(self, nc, block, sems, dsems, final_waits):
        signo = {}
        for e in self.ENG:
            c = 0
            arr = []
            for it in self.ins[e]:
                if it["sig"]:
                    c += 1
                arr.append(c)
            signo[e] = arr

        def run(e, eng):
            for it in self.ins[e]:
                for src, idx in it["waits"]:
                    if src[0] == "e":
                        eng.wait_ge(sems[src[1]], signo[src[1]][idx])
                    else:
                        cnt = self.dcount[src[1]] if src[1] in self.group_slots else idx
                        eng.wait_ge(dsems[src[1]], 16 * cnt)
                r = getattr(eng, it["fn"][0])(**it["fn"][1])
                if it["dma"] is not None:
                    r.then_inc(dsems[it["dma"]], 16)
                elif it["sig"]:
                    r.then_inc(sems[e], 1)
            if e == "sp":
                for slot in final_waits:
                    if self.dcount.get(slot, 0):
                        eng.wait_ge(dsems[slot], 16 * self.dcount[slot])

        @block.tensor
        def _(eng):
            run("pe", eng)

        @block.scalar
        def _(eng):
            run("act", eng)

        @block.vector
        def _(eng):
            run("dve", eng)

        @block.gpsimd
        def _(eng):
            run("pool", eng)

        @block.sync
        def _(eng):
            run("sp", eng)


def build_nc(upto=99, dbg=False, nbr=NB):
    nc = bass.Bass("TRN2", target_bir_lowering=False)

    def din(name, shape):
        return nc.dram_tensor(name, list(shape), F32, kind="ExternalInput").ap()

    x = din("x", [NB, S, D])
    c = din("c", [NB, D])
    w_mod = din("w_mod", [D, 9 * D])
    b_mod = din("b_mod", [9 * D])
    g_l = [din("g_ffn1", [D]), din("g_mix", [D]), din("g_ffn2", [D])]
    ffn_w = [(din("ffn1_w1", [D, FF]), din("ffn1_w3", [D, FF]), din("ffn1_w2", [FF, D])),
             (din("ffn2_w1", [D, FF]), din("ffn2_w3", [D, FF]), din("ffn2_w2", [FF, D]))]
    w_in = din("w_in", [D, IN_W])
    q_norm_g = din("q_norm_g", [64])
    k_norm_g = din("k_norm_g", [64])
    gla_gate_up = din("gla_gate_up", [16, 512])
    gla_gate_bias = din("gla_gate_bias", [512])
    gla_out_norm_g = din("gla_out_norm_g", [256])
    w_branch_att = din("w_branch_att", [256, D])
    w_branch_gla = din("w_branch_gla", [D, D])
    w_out = din("w_out", [D, D])
    consts = din("consts", [P, C_TOT])
    y = nc.dram_tensor("y", [NB, S, D], F32, kind="ExternalOutput").ap()
    dbg_out = None
    if dbg:
        dbg_out = nc.dram_tensor("dbg", [4, P, DC * S], F32, kind="ExternalOutput").ap()

    pg = Prog()
    es = ExitStack()

    def sb(name, shape, dt):
        return es.enter_context(nc.sbuf_tensor(name, list(shape), dt))

    with es:
        h = sb("h", [P, DC, S], F32)
        u = sb("u", [P, DC, S], BF16)
        arena = sb("arena", [P, 28 * 1024], BF16)
        NST = 2
        stg = [sb(f"stg{i}", [P, 2048], F32) for i in range(NST)]
        NWB = 4
        wbs = [sb(f"wb{i}", [P, 2048], BF16) for i in range(NWB)]
        cst = sb("cst", [P, C_TOT], F32)
        NT32 = 4
        t32 = [sb(f"t32_{i}", [P, TS], F32) for i in range(NT32)]
        NT16 = 4
        t16 = [sb(f"t16_{i}", [P, TS], BF16) for i in range(NT16)]
        ident_bf = sb("ident_bf", [P, P], BF16)
        ones_bf = sb("ones_bf", [P, P], BF16)
        bd_bf = sb("bd_bf", [P, P], BF16)
        kst = sb("kst", [P, 8], F32)
        cT = sb("cT", [P, DC, NB], F32)
        cact = sb("cact", [P, DC, NB], BF16)
        bmodT = sb("bmodT", [P, 72], F32)
        modT = sb("modT", [P, 72, NB], F32)
        gT = sb("gT", [P, 3, DC], F32)
        Amod = sb("Amod", [P, 3, DC, NB], F32)
        Gmod = sb("Gmod", [P, 3, DC, NB], F32)
        qg = sb("qg", [P, 1], F32)
        kg = sb("kg", [P, 1], F32)
        negb = sb("negb", [P, 4], F32)
        gno = sb("gno", [P, 2], F32)
        gup = sb("gup", [80, 512], BF16)
        gdT = sb("gdT", [80, 2 * TS], BF16)
        Sst = sb("Sst", [P, 256], F32)
        Sbf = sb("Sbf", [P, 2, 256], BF16)
        ebl = sb("ebl", [P, 16], F32)
        kdT = sb("kdT", [P, 2, P], BF16)
        kdtm = sb("kdtm", [P, 2, P], BF16)
        amk = sb("amk", [P, 2, P], BF16)
        psum = es.enter_context(nc.psum_tensor("ps", [P, 8 * TS], F32))
        psum16 = psum.bitcast(BF16) if hasattr(psum, "bitcast") else None

        sems = {e: es.enter_context(nc.semaphore(f"s_{e}")) for e in ("pe", "act", "dve", "pool")}
        dslots = ["pro", "stg0", "stg1", "dbg"]
        dsems = {s_: es.enter_context(nc.semaphore(f"d_{s_}")) for s_ in dslots}
        block = es.enter_context(nc.Block())

        cnt = dict(bank=0, t32=0, tL=0, t16=0, stg=0, wb=0, alt=0)

        def bank():
            i = cnt["bank"] % 8
            cnt["bank"] += 1
            return psum[:, i * TS:(i + 1) * TS], ("ps", i), i

        def tmp32():
            i = cnt["t32"] % 2
            cnt["t32"] += 1
            return t32[i], ("t32", i)

        def tmpL():
            i = 2 + cnt["tL"] % 2
            cnt["tL"] += 1
            return t32[i], ("t32", i)

        def tmp16():
            i = cnt["t16"] % NT16
            cnt["t16"] += 1
            return t16[i], ("t16", i)

        def stage():
            i = cnt["stg"] % NST
            cnt["stg"] += 1
            return stg[i], ("stg", i), f"stg{i}"

        def alt():
            cnt["alt"] += 1
            return "act" if cnt["alt"] % 2 else "dve"

        def copy_op(eng, out, in_, reads, writes):
            if eng == "act":
                pg.op("act", ("copy", dict(out=out, in_=in_)), reads, writes)
            else:
                pg.op(eng, ("tensor_copy", dict(out=out, in_=in_)), reads, writes)

        def load_w(src, a, b_):
            st, skey, sslot = stage()
            n = a * b_
            stv = st[:, 0:n].rearrange("p (a b) -> p a b", a=a)
            pg.dma(("dma_start", dict(out=stv, in_=src)), sslot, writes=[skey])
            i = cnt["wb"] % NWB
            cnt["wb"] += 1
            wv = wbs[i][:, 0:n].rearrange("p (a b) -> p a b", a=a)
            pg.op("pool", ("tensor_copy", dict(out=wv, in_=stv)), [skey], [("wb", i)])
            return wv, ("wb", i)

        def kc_view(w, c0, ncol):
            return w.rearrange("(kc p) f -> p kc f", p=P)[:, :, c0:c0 + ncol]

        def mm(out, lhsT, rhs, start, stop, reads, writes):
            pg.op("pe", ("matmul", dict(out=out, lhsT=lhsT, rhs=rhs, start=start, stop=stop)),
                  reads, writes)

        ukeys = [("u", t) for t in range(NTS)]

        def pro(out, in_, key, slow=False):
            if slow:
                pg.dma(("dma_start", dict(out=out, in_=in_, allow_slow_non_contiguous=True)), "pro",
                       writes=[key], group=True)
            else:
                pg.dma(("dma_start", dict(out=out, in_=in_)), "pro", writes=[key], group=True)

        pro(cst[:], consts, "cst")
        for b in range(NB):
            pro(cT[:, :, b], c[b].rearrange("(kc p) -> p kc", p=P), ("cT", b), slow=True)
        pro(bmodT[:], b_mod.rearrange("(n p) -> p n", p=P), "bmodT", slow=True)
        for l in range(3):
            pro(gT[:, l, :], g_l[l].rearrange("(n p) -> p n", p=P), ("gT", l), slow=True)
        for hh in range(2):
            pro(qg[hh * 64:(hh + 1) * 64, :], q_norm_g.rearrange("(p o) -> p o", o=1), ("qg", hh), slow=True)
            pro(kg[hh * 64:(hh + 1) * 64, :], k_norm_g.rearrange("(p o) -> p o", o=1), ("kg", hh), slow=True)
        pro(negb[:], gla_gate_bias.rearrange("(n p) -> p n", p=P), "negb", slow=True)
        pro(gno[:], gla_out_norm_g.rearrange("(n p) -> p n", p=P), "gno", slow=True)

        pg.op("dve", ("memset", dict(ap=ones_bf[:], constant=1.0)), [], ["ones"])
        pg.op("dve", ("memset", dict(ap=kst[:, 0:1], constant=EPS)), [], ["kst0"])
        pg.op("dve", ("memset", dict(ap=kst[:, 1:2], constant=1.0)), [], ["kst1"])
        pg.op("dve", ("memset", dict(ap=kst[:, 2:3], constant=math.log(128.0 ** -0.5))), [], ["kst2"])
        pg.op("dve", ("tensor_copy", dict(out=ident_bf[:], in_=cst[:, C_ID:C_ID + P])), ["cst"], ["identbf"])
        pg.op("dve", ("tensor_copy", dict(out=bd_bf[:], in_=cst[:, C_BD:C_BD + P])), ["cst"], ["bdbf"])
        for pb_ in (0, 32, 64):
            pro(t32[0][pb_:pb_ + 16, :], gla_gate_up, ("gupst", pb_))
        for pb_ in (0, 32, 64):
            pg.op("dve", ("tensor_copy", dict(out=gup[pb_:pb_ + 16, :], in_=t32[0][pb_:pb_ + 16, :])),
                  [("gupst", pb_)], ["gup", ("t32", 0)])
        pg.op("dve", ("tensor_scalar", dict(out=negb[:], in0=negb[:], scalar1=-1.0, scalar2=None, op0=ALU.mult)),
              ["negb"], ["negb"])
        pg.op("dve", ("tensor_scalar", dict(out=qg[:], in0=qg[:], scalar1=0.125, scalar2=None, op0=ALU.mult)),
              [("qg", 0), ("qg", 1)], [("qg", 0), ("qg", 1)])
        pg.op("act", ("activation", dict(out=cact[:], in_=cT[:], func=AF.Silu)), [("cT", 0), ("cT", 1)], ["cact"])
        ident = cst[:, C_ID:C_ID + P]
        stepsM = cst[:, C_STEP:C_STEP + 256]
        causM = cst[:, C_CAUS:C_CAUS + P]
        scanM = ones_bf[:]

        bg = []

        def run_bg(n=1):
            for _ in range(n):
                if bg:
                    bg.pop(0)()

        def mod_tile(t):
            l = t // 12
            wv, wk = load_w(kc_view(w_mod, t * 256, 256), DC, 256)
            pm, pmk, _ = bank()
            for j in range(2):
                for kc in range(DC):
                    mm(pm[:, j * 2:j * 2 + 2], wv[:, kc, j * P:(j + 1) * P], cact[:, kc, :],
                       kc == 0, kc == DC - 1, [wk, "cact"], [pmk])
            for b in range(NB):
                pg.op("dve", ("tensor_tensor", dict(out=modT[:, 2 * t:2 * t + 2, b], in0=pm[:, b:4:2],
                                                    in1=bmodT[:, 2 * t:2 * t + 2], op=ALU.add)),
                      [pmk, "bmodT"], [("modT", l, b)])

        def mod_fin(l):
            coef = 1.0 if l == 1 else 0.5
            for b in range(NB):
                pg.op("dve", ("scalar_tensor_tensor", dict(
                    out=Amod[:, l, :, b], in0=modT[:, l * 24 + 8:l * 24 + 16, b], scalar=1.0, in1=gT[:, l, :],
                    op0=ALU.add, op1=ALU.mult)), [("modT", l, b), ("gT", l)], [("Amod", l, b)])
                pg.op("dve", ("tensor_scalar", dict(
                    out=Gmod[:, l, :, b], in0=modT[:, l * 24 + 16:l * 24 + 24, b], scalar1=1.0, scalar2=coef,
                    op0=ALU.add, op1=ALU.mult)), [("modT", l, b)], [("Gmod", l, b)])

        HOIST = []

        def prologue_mod():
            for t in range(12):
                mod_tile(t)
            mod_fin(0)
        for l in (1, 2):
            for t in range(12 * l, 12 * l + 12):
                bg.append(lambda t=t: mod_tile(t))
            bg.append(lambda l=l: mod_fin(l))

        def load_x(b):
            for tt in range(16):
                st, skey, sslot = stage()
                xs = st[:, 0:D]
                pg.dma(("dma_start", dict(out=xs, in_=x[b, tt * P:(tt + 1) * P, :])), sslot,
                       writes=[skey])
                for half in range(2):
                    pb, pk, _ = bank()
                    for j in range(4):
                        dc = half * 4 + j
                        pg.op("pe", ("transpose", dict(
                            out=pb[:, j * P:(j + 1) * P], in_=xs[:, dc * P:(dc + 1) * P], identity=ident)),
                            [skey, "cst"], [pk])
                    copy_op(alt(), h[:, half * 4:half * 4 + 4, tt * P:(tt + 1) * P],
                            pb.rearrange("p (a t) -> p a t", a=4), [pk], [("h", tt // 4)])

        def store_y(b):
            for tt in range(16):
                st, skey, sslot = stage()
                ys = st[:, 0:D]
                for half in range(2):
                    pb, pk, _ = bank()
                    for j in range(4):
                        dc = half * 4 + j
                        pg.op("pe", ("transpose", dict(
                            out=pb[:, j * P:(j + 1) * P], in_=h[:, dc, tt * P:(tt + 1) * P], identity=ident)),
                            [("h", tt // 4), "cst"], [pk])
                    copy_op(alt(), ys[:, half * 512:(half + 1) * 512], pb, [pk], [skey])
                pg.dma(("dma_start", dict(out=y[b, tt * P:(tt + 1) * P, :], in_=ys)), sslot,
                       reads=[skey])

        def rstd_from(pss, pssk, scale):
            r32, rk = tmpL()
            pg.op("act", ("activation", dict(out=r32[:], in_=pss, func=AF.Ln, bias=kst[:, 0:1], scale=scale)),
                  [pssk, "kst0"], [rk])
            pg.op("act", ("activation", dict(out=r32[:], in_=r32[:], func=AF.Exp, scale=-0.5)), [rk], [rk])
            return r32, rk

        SQ_ENG = ("dve", "dve", "act", "dve", "dve", "act", "dve", "dve")

        def norm_mod(l, b, tsl=(0, 1, 2, 3)):
            def s1(ts):
                sl = slice(ts * TS, (ts + 1) * TS)
                pss, pssk, _ = bank()
                for dc in range(DC):
                    sq, sqk = tmp16()
                    e_ = SQ_ENG[dc]
                    if e_ == "act":
                        pg.op("act", ("activation", dict(out=sq[:], in_=h[:, dc, sl], func=AF.Square)), [("h", ts)], [sqk])
                    else:
                        pg.op(e_, ("tensor_tensor", dict(out=sq[:], in0=h[:, dc, sl], in1=h[:, dc, sl], op=ALU.mult)),
                              [("h", ts)], [sqk])
                    mm(pss, ones_bf[:], sq[:], dc == 0, dc == DC - 1, [sqk, "ones"], [pssk])
                return rstd_from(pss, pssk, 1.0 / D)

            def s2(ts, r32, rk):
                sl = slice(ts * TS, (ts + 1) * TS)
                for dc in range(DC):
                    tt_, tk = tmp32()
                    pg.op("dve", ("tensor_tensor", dict(out=tt_[:], in0=h[:, dc, sl], in1=r32[:], op=ALU.mult)),
                          [("h", ts), rk], [tk])
                    pg.op("act", ("activation", dict(
                        out=u[:, dc, sl], in_=tt_[:], func=AF.Identity, bias=modT[:, l * 24 + dc, b:b + 1],
                        scale=Amod[:, l, dc, b:b + 1])), [tk, ("modT", l, b), ("Amod", l, b)], [("u", ts)])

            rs = {tsl[0]: s1(tsl[0])}
            for i_, ts in enumerate(tsl):
                if i_ + 1 < len(tsl):
                    rs[tsl[i_ + 1]] = s1(tsl[i_ + 1])
                s2(ts, *rs.pop(ts))

        def gkey(f, ts):
            return ("g", f, ts)

        def ffn(fi, l, b):
            w1, w3, w2 = ffn_w[fi]
            gbuf = arena[:, 0:12 * S].rearrange("p (f t) -> p f t", f=12)
            for (f0, nf) in ((0, 12), (12, 10)):
                for tl in range(nf // 2):
                    c0 = (f0 + tl * 2) * P
                    w1v, w1k = load_w(kc_view(w1, c0, 256), DC, 256)
                    w3v, w3k = load_w(kc_view(w3, c0, 256), DC, 256)
                    for j in range(2):
                        fl = tl * 2 + j
                        for ts in range(NTS):
                            sl = slice(ts * TS, (ts + 1) * TS)
                            pa, pak, _ = bank()
                            pb, pbk, _ = bank()
                            for kc in range(DC):
                                mm(pa, w1v[:, kc, j * P:(j + 1) * P], u[:, kc, sl], kc == 0, kc == DC - 1,
                                   [w1k, ("u", ts)], [pak])
                            for kc in range(DC):
                                mm(pb, w3v[:, kc, j * P:(j + 1) * P], u[:, kc, sl], kc == 0, kc == DC - 1,
                                   [w3k, ("u", ts)], [pbk])
                            s1, s1k = tmp16()
                            pg.op("act", ("activation", dict(out=s1[:], in_=pa, func=AF.Silu)),
                                  [pak], [s1k])
                            pg.op("dve", ("tensor_tensor", dict(
                                out=gbuf[:, fl, sl], in0=pb, in1=s1[:], op=ALU.mult)), [pbk, s1k], [gkey(fl, ts)])
                    run_bg()
                w2v_all = w2.rearrange("(fc p) d -> p fc d", p=P)
                for dc in range(DC):
                    w2v, w2k = load_w(w2v_all[:, f0:f0 + nf, dc * P:(dc + 1) * P], nf, P)
                    for ts in range(NTS):
                        sl = slice(ts * TS, (ts + 1) * TS)
                        po, pok, _ = bank()
                        for fl in range(nf):
                            mm(po, w2v[:, fl, :], gbuf[:, fl, sl], fl == 0, fl == nf - 1, [w2k, gkey(fl, ts)], [pok])
                        pg.op("dve", ("scalar_tensor_tensor", dict(
                            out=h[:, dc, sl], in0=po, scalar=Gmod[:, l, dc, b:b + 1], in1=h[:, dc, sl],
                            op0=ALU.mult, op1=ALU.add)), [pok, ("Gmod", l, b), ("h", ts)], [("h", ts)])
                    run_bg()

        G_KEYS = [gkey(f, t) for f in range(12) for t in range(NTS)]

        KB = 512
        acc = arena[:, 0:32 * KB].bitcast(F32).rearrange("p (c o t) -> p c o t", c=2, o=2)
        ogla = arena[:, 0:32 * KB].rearrange("p (c t) -> p c t", c=8)
        qbuf = arena[:, 32 * KB:40 * KB].rearrange("p (c t) -> p c t", c=2)
        kbuf = arena[:, 40 * KB:48 * KB].rearrange("p (c t) -> p c t", c=2)
        vtm = arena[:, 48 * KB:56 * KB].rearrange("p (b f) -> p b f", b=16)
        oatt = arena[:, 32 * KB:40 * KB].rearrange("p (c t) -> p c t", c=2)
        gvtm = arena[:, 40 * KB:48 * KB].rearrange("p (b f) -> p b f", b=16)
        qdec = arena[:, 48 * KB:52 * KB]
        kinv = arena[:, 52 * KB:56 * KB]
        merged = arena[:, 40 * KB:56 * KB].rearrange("p (c t) -> p c t", c=8)

        ACC_KEYS = [("acc", ch, t) for ch in range(2) for t in range(NTS)]
        Q_KEYS = [("q", ch, t) for ch in range(2) for t in range(NTS)]
        K_KEYS = [("k", ch, t) for ch in range(2) for t in range(NTS)]
        V_KEYS = [("v", tb) for tb in range(16)]
        OATT_KEYS = [("oatt", t) for t in range(NTS)]
        OGLA_KEYS = [("ogla", hh, t) for hh in range(4) for t in range(NTS)]
        GV_KEYS = [("gv", tb) for tb in range(16)]
        QD_KEYS = [("qd", t) for t in range(NTS)]
        KI_KEYS = [("ki", t) for t in range(NTS)]
        KD_KEYS = []
        MG_KEYS = [("mg", dc, t) for dc in range(DC) for t in range(2)]

        def tok_slice(dil, r, n):
            st0 = r + dil * P * n
            return slice(st0, st0 + dil * (P - 1) + 1, dil)

        def blk_ts(dil, r, n):
            if dil == 1:
                return [n // 4]
            return list(range(NTS)) if dil == 16 else [n]

        def attention(b):
            for gi, (win, dil) in enumerate(ATT_GROUPS):
                nb = S // dil // P
                wq_, wqk_ = load_w(kc_view(w_in, OFF_AQ + gi * 256, 256), DC, 256)
                wk_, wkk_ = load_w(kc_view(w_in, OFF_AK + gi * 256, 256), DC, 256)
                items = [(wq_, wqk_, qbuf, qg, "q", ch, ts) for ch in range(2) for ts in range(NTS)] + \
                        [(wk_, wkk_, kbuf, kg, "k", ch, ts) for ch in range(2) for ts in range(NTS)]

                def qk_s1(it):
                    wv, wk, buf, gain, nm, ch, ts = it
                    sl = slice(ts * TS, (ts + 1) * TS)
                    pq, pqk, _ = bank()
                    for kc in range(DC):
                        mm(pq, wv[:, kc, ch * P:(ch + 1) * P], u[:, kc, sl], kc == 0, kc == DC - 1,
                           [wk, ("u", ts)], [pqk])
                    sq, sqk = tmp16()
                    pg.op("act", ("activation", dict(out=sq[:], in_=pq, func=AF.Square)), [pqk], [sqk])
                    return (pq, pqk, sq, sqk)

                def qk_s2(it, ctx):
                    wv, wk, buf, gain, nm, ch, ts = it
                    pq, pqk, sq, sqk = ctx
                    sl = slice(ts * TS, (ts + 1) * TS)
                    pss, pssk, _ = bank()
                    mm(pss, bd_bf[:], sq[:], True, True, [sqk, "bdbf"], [pssk])
                    r32, rk = rstd_from(pss, pssk, 1.0 / 64)
                    pg.op("dve", ("scalar_tensor_tensor", dict(out=buf[:, ch, sl], in0=pq, scalar=gain[:, 0:1], in1=r32[:],
                                                               op0=ALU.mult, op1=ALU.mult)),
                          [pqk, rk, (nm + "g", 0), (nm + "g", 1)], [(nm, ch, ts)])

                ctxs = {0: qk_s1(items[0])}
                for i in range(len(items)):
                    if i + 1 < len(items):
                        ctxs[i + 1] = qk_s1(items[i + 1])
                    qk_s2(items[i], ctxs.pop(i))
                wv, wk = load_w(kc_view(w_in, OFF_AV + gi * 256, 256), DC, 256)
                blocks = [(r, n) for r in range(dil) for n in range(nb)]
                for tb, (r, n) in enumerate(blocks):
                    tsl = tok_slice(dil, r, n)
                    pv, pvk, _ = bank()
                    for kc in range(DC):
                        mm(pv[:, 0:256], u[:, kc, tsl], wv[:, kc, :], kc == 0, kc == DC - 1,
                           [wk] + [("u", t) for t in blk_ts(dil, r, n)], [pvk])
                    copy_op(alt(), vtm[:, tb, :], pv[:, 0:256], [pvk], [("v", tb)])
                work = [(ch, tb) for ch in range(2) for tb in range(len(blocks))]
                pend = {}

                def emit_qk(ch, tb):
                    r, n = blocks[tb]
                    tsl = tok_slice(dil, r, n)
                    tss = blk_ts(dil, r, n)
                    Es = []
                    for hh in range(2):
                        head = gi * 4 + ch * 2 + hh
                        cs = -SLOPES[head] * dil
                        ps_, psk, _ = bank()
                        prt = slice(hh * 64, (hh + 1) * 64)
                        rd = [("q", ch, t) for t in tss] + [("k", ch, t) for t in tss]
                        mm(ps_[:, 128:256], kbuf[prt, ch, tsl], qbuf[prt, ch, tsl], True, True, rd, [psk])
                        lo = 128
                        if n > 0:
                            psl = tok_slice(dil, r, n - 1)
                            rd2 = rd + [("k", ch, t) for t in blk_ts(dil, r, n - 1)]
                            mm(ps_[:, 0:128], kbuf[prt, ch, psl], qbuf[prt, ch, tsl], True, True, rd2, [psk])
                            lo = 0
                        t_, tk = tmp32()
                        pg.op("dve", ("scalar_tensor_tensor", dict(
                            out=t_[:, lo:256], in0=stepsM[:, lo:256], scalar=cs, in1=ps_[:, lo:256],
                            op0=ALU.mult, op1=ALU.add)), [psk, "cst"], [tk])
                        E, Ek = tmp16()
                        pg.op("act", ("activation", dict(out=E[:, lo:256], in_=t_[:, lo:256],
                                                                               func=AF.Exp)), [tk], [Ek])
                        Es.append((E, Ek, lo))
                    pend[(ch, tb)] = Es

                def emit_pv(ch, tb):
                    r, n = blocks[tb]
                    tsl = tok_slice(dil, r, n)
                    tss = blk_ts(dil, r, n)
                    Es = pend.pop((ch, tb))
                    po, pok, _ = bank()
                    for hh in range(2):
                        E, Ek, lo = Es[hh]
                        prt = slice(hh * 64, (hh + 1) * 64)
                        vc = slice(ch * P + hh * 64, ch * P + hh * 64 + 64)
                        mm(po[prt, 0:128], vtm[:, tb, vc], E[:, 128:256], True, n == 0, [Ek, ("v", tb)], [pok])
                        if n > 0:
                            mm(po[prt, 0:128], vtm[:, tb - 1, vc], E[:, 0:128], False, True, [Ek, ("v", tb - 1)], [pok])
                        mm(po[prt, 128:256], ones_bf[:, 0:64], E[:, 128:256], True, n == 0, [Ek, "ones"], [pok])
                        if n > 0:
                            mm(po[prt, 128:256], ones_bf[:, 0:64], E[:, 0:128], False, True, [Ek, "ones"], [pok])
                    pov = po[:, 0:256].rearrange("p (o t) -> p o t", o=2)
                    akeys = [("acc", ch, t) for t in tss]
                    if gi == 0:
                        pg.op("dve", ("tensor_copy", dict(out=acc[:, ch, :, tsl], in_=pov)), [pok], akeys)
                    else:
                        pg.op("dve", ("tensor_tensor", dict(out=acc[:, ch, :, tsl], in0=acc[:, ch, :, tsl], in1=pov,
                                                               op=ALU.add)), [pok] + akeys, akeys)

                emit_qk(*work[0])
                for i in range(len(work)):
                    if i + 1 < len(work):
                        emit_qk(*work[i + 1])
                    emit_pv(*work[i])
            pg.transfer(Q_KEYS, OATT_KEYS)
            for ch in range(2):
                for ts in range(NTS):
                    sl = slice(ts * TS, (ts + 1) * TS)
                    r_, rk = tmp32()
                    pg.op("dve", ("reciprocal", dict(out=r_[:], in_=acc[:, ch, 1, sl])),
                          [("acc", ch, ts)], [rk])
                    pg.op("dve", ("tensor_tensor", dict(out=oatt[:, ch, sl], in0=acc[:, ch, 0, sl],
                                                                              in1=r_[:], op=ALU.mult)),
                          [("acc", ch, ts), rk], [("oatt", ts)])

        def gd_pb(ts):
            return 0 if ts == 3 else 32 * ts

        def gd_ap(ts):
            c0 = TS if ts == 3 else 0
            return gdT[gd_pb(ts):gd_pb(ts) + 16, c0:c0 + TS]

        def gla(b):
            wv, wk = load_w(kc_view(w_in, OFF_GD, 16), DC, 16)
            for ts in range(NTS):
                sl = slice(ts * TS, (ts + 1) * TS)
                pd, pdk, _ = bank()
                for kc in range(DC):
                    mm(pd[gd_pb(ts):gd_pb(ts) + 16, :], wv[:, kc, :], u[:, kc, sl], kc == 0, kc == DC - 1, [wk, ("u", ts)], [pdk])
                copy_op("act", gd_ap(ts), pd[gd_pb(ts):gd_pb(ts) + 16, :], [pdk], [("gd", ts)])

            def outnorm_units(hh):
                units = []
                st_ = {}

                def load():
                    st_["w"] = load_w(kc_view(w_in, OFF_GR + hh * 256, 256), DC, 256)
                units.append(load)
                for ts in range(NTS):
                    sl = slice(ts * TS, (ts + 1) * TS)

                    def u_ss(ts=ts, sl=sl):
                        pss, pssk, _ = bank()
                        for dv in range(2):
                            sq, sqk = tmp16()
                            pg.op("pool", ("tensor_tensor", dict(
                                out=sq[:], in0=ogla[:, hh * 2 + dv, sl], in1=ogla[:, hh * 2 + dv, sl], op=ALU.mult)),
                                [("ogla", hh, ts)], [sqk])
                            mm(pss, ones_bf[:], sq[:], dv == 0, dv == 1, [sqk, "ones"], [pssk])
                        st_[ts] = rstd_from(pss, pssk, 1.0 / 256)
                    units.append(u_ss)
                    for dv in range(2):
                        def u_dv(ts=ts, sl=sl, dv=dv):
                            wv_, wk_ = st_["w"]
                            r32, rk = st_[ts]
                            pr, prk, _ = bank()
                            for kc in range(DC):
                                mm(pr, wv_[:, kc, dv * P:(dv + 1) * P], u[:, kc, sl], kc == 0, kc == DC - 1,
                                   [wk_, ("u", ts)], [prk])
                            sg, sgk = tmp32()
                            pg.op("act", ("activation", dict(out=sg[:], in_=pr, func=AF.Silu)), [prk], [sgk])
                            t1, t1k = tmp32()
                            pg.op("dve", ("scalar_tensor_tensor", dict(
                                out=t1[:], in0=ogla[:, hh * 2 + dv, sl], scalar=gno[:, dv:dv + 1], in1=r32[:],
                                op0=ALU.mult, op1=ALU.mult)), [("ogla", hh, ts), rk, "gno"], [t1k])
                            pg.op("dve", ("tensor_tensor", dict(
                                out=ogla[:, hh * 2 + dv, sl], in0=t1[:], in1=sg[:], op=ALU.mult)),
                                [t1k, sgk, ("ogla", hh, ts)], [("ogla", hh, ts)])
                        units.append(u_dv)
                return units

            pending = []
            for hh in range(4):
                wq, wqk = load_w(kc_view(w_in, OFF_GQ + hh * P, P), DC, P)
                wkk, wkkk = load_w(kc_view(w_in, OFF_GK + hh * P, P), DC, P)
                wvv, wvk = load_w(kc_view(w_in, OFF_GV + hh * 256, 256), DC, 256)
                for ts in range(NTS):
                    sl = slice(ts * TS, (ts + 1) * TS)
                    px, pxk, _ = bank()
                    mm(px, gup[gd_pb(ts):gd_pb(ts) + 16, hh * P:(hh + 1) * P], gd_ap(ts), True, True, ["gup", ("gd", ts)], [pxk])
                    e_, ek = tmp32()
                    pg.op("act", ("activation", dict(out=e_[:], in_=px, func=AF.Exp, bias=negb[:, hh:hh + 1], scale=-1.0)),
                          [pxk, "negb"], [ek])
                    sp_, spk = tmp32()
                    pg.op("act", ("activation", dict(out=sp_[:], in_=e_[:], func=AF.Ln, bias=kst[:, 1:2], scale=1.0)),
                          [ek, "kst1"], [spk])
                    B_, Bk = tmpL()
                    bks = [(Bk, c4) for c4 in range(4)]
                    for c4 in range(4):
                        pg.op("dve", ("tensor_tensor_scan", dict(
                            out=B_[:, c4 * P:(c4 + 1) * P], data0=scanM, data1=sp_[:, c4 * P:(c4 + 1) * P], initial=0.0,
                            op0=ALU.mult, op1=ALU.add)), [spk, "ones"], [bks[c4]])
                    pq, pqk, _ = bank()
                    for kc in range(DC):
                        mm(pq, wq[:, kc, :], u[:, kc, sl], kc == 0, kc == DC - 1, [wqk, ("u", ts)], [pqk])
                    pk_, pkk, _ = bank()
                    for kc in range(DC):
                        mm(pk_, wkk[:, kc, :], u[:, kc, sl], kc == 0, kc == DC - 1, [wkkk, ("u", ts)], [pkk])
                    pg.op("act", ("activation", dict(
                        out=ebl[:, ts * 4:ts * 4 + 4], in_=B_[:, 127:512:128], func=AF.Exp, scale=-1.0 / 16)),
                        bks, [("ebl", ts)])
                    eb, ebk = tmp32()
                    pg.op("act", ("activation", dict(out=eb[:], in_=B_[:], func=AF.Exp, bias=kst[:, 2:3], scale=-1.0 / 16)),
                          bks + ["kst2"], [ebk])
                    pg.op("dve", ("tensor_tensor", dict(out=qdec[:, sl], in0=pq, in1=eb[:], op=ALU.mult)),
                          [pqk, ebk], [("qd", ts)])
                    en, enk = tmp32()
                    pg.op("act", ("activation", dict(out=en[:], in_=B_[:], func=AF.Exp, scale=1.0 / 16)), bks, [enk])
                    pg.op("dve", ("tensor_tensor", dict(out=kinv[:, sl], in0=pk_, in1=en[:], op=ALU.mult)),
                          [pkk, enk], [("ki", ts)])
                    for tb in range(ts * 4, ts * 4 + 4):
                        pv, pvk, _ = bank()
                        for kc in range(DC):
                            mm(pv[:, 0:256], u[:, kc, tb * P:(tb + 1) * P], wvv[:, kc, :], kc == 0, kc == DC - 1,
                               [wvk, ("u", tb // 4)], [pvk])
                        copy_op(alt(), gvtm[:, tb, :], pv[:, 0:256], [pvk], [("gv", tb)])

                def emit_kd(cc):
                    csl = slice(cc * P, (cc + 1) * P)
                    j = cc % 2
                    pg.op("dve", ("tensor_scalar", dict(
                        out=kdT[:, j, :], in0=kinv[:, csl], scalar1=ebl[:, cc:cc + 1], scalar2=None, op0=ALU.mult)),
                        [("ki", cc // 4), ("ebl", cc // 4)], [("kdT", j)])
                    pt, ptk, bi = bank()
                    pt16 = psum16[:, bi * 2 * TS:bi * 2 * TS + P]
                    pg.op("pe", ("transpose", dict(out=pt16, in_=kdT[:, j, :], identity=ident_bf[:])),
                          [("kdT", j), "identbf"], [ptk])
                    copy_op("act", kdtm[:, j, :], pt16, [ptk], [("kd", j)])
                emit_kd(0)
                for cc in range(16):
                    csl = slice(cc * P, (cc + 1) * P)
                    ts = cc // 4
                    pa, pak, _ = bank()
                    mm(pa[:, 0:P], kinv[:, csl], qdec[:, csl], True, True, [("ki", ts), ("qd", ts)], [pak])
                    j = cc % 2
                    if cc + 1 < 15:
                        emit_kd(cc + 1)
                    pg.op("dve", ("tensor_tensor", dict(out=amk[:, j, :], in0=pa[:, 0:P], in1=causM, op=ALU.mult)),
                          [pak, "cst"], [("amk", j)])
                    if cc < 15:
                        pu, puk, _ = bank()
                        mm(pu[:, 0:256], kdtm[:, j, :], gvtm[:, cc, :], True, True, [("kd", j), ("gv", cc)], [puk])
                    po, pok, _ = bank()
                    for dv in range(2):
                        mm(po[:, dv * P:(dv + 1) * P], gvtm[:, cc, dv * P:(dv + 1) * P], amk[:, j, :], True, cc == 0,
                           [("gv", cc), ("amk", j)], [pok])
                        if cc > 0:
                            mm(po[:, dv * P:(dv + 1) * P], Sbf[:, (cc - 1) % 2, dv * P:(dv + 1) * P], qdec[:, csl], False, True,
                               [("Sbf", (cc - 1) % 2), ("qd", ts)], [pok])
                    if cc < 15:
                        if cc == 0:
                            pg.op("dve", ("tensor_copy", dict(out=Sst[:], in_=pu[:, 0:256])), [puk], ["Sst"])
                        else:
                            pg.op("dve", ("scalar_tensor_tensor", dict(
                                out=Sst[:], in0=Sst[:], scalar=ebl[:, cc:cc + 1], in1=pu[:, 0:256],
                                op0=ALU.mult, op1=ALU.add)), [puk, "Sst", ("ebl", ts)], ["Sst"])
                        copy_op("act", Sbf[:, cc % 2, :], Sst[:], ["Sst"], [("Sbf", cc % 2)])
                    copy_op("act", ogla[:, hh * 2:hh * 2 + 2, csl], po[:, 0:256].rearrange("p (a t) -> p a t", a=2),
                            [pok], [("ogla", hh, ts)])
                    if pending:
                        pending.pop(0)()
                while pending:
                    pending.pop(0)()
                pending = outnorm_units(hh)
            while pending:
                pending.pop(0)()

        def merge_out(b, after_half=None):
            wba_all = w_branch_att.rearrange("(kc p) d -> p kc d", p=P)
            for th in range(2):
                for dp in range(4):
                    c0 = dp * 256
                    wga, wgak = load_w(kc_view(w_in, OFF_GA + c0, 256), DC, 256)
                    wba, wbak = load_w(wba_all[:, :, c0:c0 + 256], 2, 256)
                    for j in range(2):
                        dc = dp * 2 + j
                        for t2 in range(2):
                            ts = th * 2 + t2
                            sl = slice(ts * TS, (ts + 1) * TS)
                            p1, p1k, _ = bank()
                            for kc in range(DC):
                                mm(p1, wga[:, kc, j * P:(j + 1) * P], u[:, kc, sl], kc == 0, kc == DC - 1,
                                   [wgak, ("u", ts)], [p1k])
                            sa, sak = tmp32()
                            pg.op("act", ("activation", dict(out=sa[:], in_=p1, func=AF.Sigmoid)),
                                  [p1k], [sak])
                            p2, p2k, _ = bank()
                            for c2 in range(2):
                                mm(p2, wba[:, c2, j * P:(j + 1) * P], oatt[:, c2, sl], c2 == 0, c2 == 1,
                                   [wbak, ("oatt", ts)], [p2k])
                            pg.op("dve", ("tensor_tensor", dict(
                                out=merged[:, dc, t2 * TS:(t2 + 1) * TS], in0=p2, in1=sa[:], op=ALU.mult)),
                                [p2k, sak], [("mg", dc, t2)])
                    wgg, wggk = load_w(kc_view(w_in, OFF_GG + c0, 256), DC, 256)
                    wbg, wbgk = load_w(kc_view(w_branch_gla, c0, 256), DC, 256)
                    for j in range(2):
                        dc = dp * 2 + j
                        for t2 in range(2):
                            ts = th * 2 + t2
                            sl = slice(ts * TS, (ts + 1) * TS)
                            p1, p1k, _ = bank()
                            for kc in range(DC):
                                mm(p1, wgg[:, kc, j * P:(j + 1) * P], u[:, kc, sl], kc == 0, kc == DC - 1,
                                   [wggk, ("u", ts)], [p1k])
                            sa, sak = tmp32()
                            pg.op("act", ("activation", dict(out=sa[:], in_=p1, func=AF.Sigmoid)),
                                  [p1k], [sak])
                            p2, p2k, _ = bank()
                            for kc in range(DC):
                                mm(p2, wbg[:, kc, j * P:(j + 1) * P], ogla[:, kc, sl], kc == 0, kc == DC - 1,
                                   [wbgk, ("ogla", kc // 2, ts)], [p2k])
                            m2, m2k = tmp32()
                            pg.op("dve", ("tensor_tensor", dict(out=m2[:], in0=p2, in1=sa[:],
                                                                                         op=ALU.mult)), [p2k, sak], [m2k])
                            pg.op("dve", ("tensor_tensor", dict(
                                out=merged[:, dc, t2 * TS:(t2 + 1) * TS], in0=merged[:, dc, t2 * TS:(t2 + 1) * TS],
                                in1=m2[:], op=ALU.add)), [m2k, ("mg", dc, t2)], [("mg", dc, t2)])
                for dp in range(4):
                    wo, wok = load_w(kc_view(w_out, dp * 256, 256), DC, 256)
                    for j in range(2):
                        dc = dp * 2 + j
                        for t2 in range(2):
                            ts = th * 2 + t2
                            sl = slice(ts * TS, (ts + 1) * TS)
                            po, pok, _ = bank()
                            for kc in range(DC):
                                mm(po, wo[:, kc, j * P:(j + 1) * P], merged[:, kc, t2 * TS:(t2 + 1) * TS], kc == 0,
                                   kc == DC - 1, [wok, ("mg", kc, t2)], [pok])
                            pg.op("dve", ("scalar_tensor_tensor", dict(
                                out=h[:, dc, sl], in0=po, scalar=Gmod[:, 1, dc, b:b + 1], in1=h[:, dc, sl],
                                op0=ALU.mult, op1=ALU.add)), [pok, ("Gmod", 1, b), ("h", ts)], [("h", ts)])
                if after_half is not None:
                    after_half(th)

        def dump(slot_i, b):
            if dbg and b == 0:
                pg.dma(("dma_start", dict(out=dbg_out[slot_i].rearrange("p (c t) -> p c t", c=DC), in_=h[:])), "dbg",
                       reads=[("h", t) for t in range(NTS)])

        MIX_A = ACC_KEYS + Q_KEYS + K_KEYS + V_KEYS
        for b in range(nbr):
            load_x(b)
            if b == 0:
                prologue_mod()
            norm_mod(0, b)
            if upto >= 1:
                ffn(0, 0, b)
            dump(0, b)
            run_bg(len(bg))
            if upto >= 2:
                norm_mod(1, b)
                pg.transfer(G_KEYS, MIX_A + KD_KEYS)
                attention(b)
                pg.transfer(ACC_KEYS, OGLA_KEYS)
                pg.transfer(K_KEYS, GV_KEYS)
                pg.transfer(V_KEYS, QD_KEYS + KI_KEYS)
                gla(b)
                pg.transfer(GV_KEYS + QD_KEYS + KI_KEYS, MG_KEYS)
                if upto >= 3:
                    merge_out(b, after_half=lambda th: norm_mod(2, b, (2 * th, 2 * th + 1)))
                else:
                    merge_out(b)
                dump(1, b)
            if upto >= 3:
                pg.transfer(OGLA_KEYS + OATT_KEYS + MG_KEYS + KD_KEYS + MIX_A + GV_KEYS + QD_KEYS + KI_KEYS, G_KEYS)
                ffn(1, 2, b)
            elif upto >= 2:
                pg.transfer(OGLA_KEYS + OATT_KEYS + MG_KEYS + KD_KEYS + MIX_A + GV_KEYS + QD_KEYS + KI_KEYS, G_KEYS)
            store_y(b)

        pg.emit(nc, block, sems, dsems, final_waits=["stg0", "stg1", "dbg"])
    return nc


def make_consts():
    cs = np.zeros((P, C_TOT), np.float32)
    cs[:, C_ID:C_ID + P] = np.eye(P, dtype=np.float32)
    kk = np.arange(P)[:, None]
    qq = np.arange(P)[None, :]
    BIG = 1.0e4
    prev = np.where(qq <= kk, (qq + P - kk).astype(np.float32), BIG)
    cur = np.where(qq >= kk, (qq - kk).astype(np.float32), BIG)
    cs[:, C_STEP:C_STEP + P] = prev
    cs[:, C_STEP + P:C_STEP + 2 * P] = cur
    cs[:, C_CAUS:C_CAUS + P] = (kk <= qq).astype(np.float32)
    bd = np.zeros((P, P), np.float32)
    bd[:64, :64] = 1.0
    bd[64:, 64:] = 1.0
    cs[:, C_BD:C_BD + P] = bd
    return cs


_NC_CACHE = {}


def _run(inputs, upto=99, dbg=False, ncores=8):
    key = (upto, dbg)
    if key not in _NC_CACHE:
        _NC_CACHE[key] = build_nc(upto, dbg)
    nc = _NC_CACHE[key]
    f = lambda a: np.ascontiguousarray(np.asarray(a, dtype=np.float32))
    sq = lambda a: f(a)[0]
    shared = {
        "w_mod": sq(inputs["w_mod"]), "b_mod": sq(inputs["b_mod"]),
        "g_ffn1": sq(inputs["g_ffn1"]), "g_mix": sq(inputs["g_mix"]), "g_ffn2": sq(inputs["g_ffn2"]),
        "ffn1_w1": sq(inputs["ffn1_w1"]), "ffn1_w3": sq(inputs["ffn1_w3"]), "ffn1_w2": sq(inputs["ffn1_w2"]),
        "ffn2_w1": sq(inputs["ffn2_w1"]), "ffn2_w3": sq(inputs["ffn2_w3"]), "ffn2_w2": sq(inputs["ffn2_w2"]),
        "w_in": sq(inputs["w_in"]), "q_norm_g": sq(inputs["q_norm_g"]), "k_norm_g": sq(inputs["k_norm_g"]),
        "gla_gate_up": sq(inputs["gla_gate_up"]), "gla_gate_bias": sq(inputs["gla_gate_bias"]),
        "gla_out_norm_g": sq(inputs["gla_out_norm_g"]), "w_branch_att": sq(inputs["w_branch_att"]),
        "w_branch_gla": sq(inputs["w_branch_gla"]), "w_out": sq(inputs["w_out"]),
        "consts": make_consts(),
    }
    xf = f(inputs["x"])
    cf = f(inputs["c"])
    in_maps = []
    for i in range(ncores):
        m = dict(shared)
        m["x"] = np.ascontiguousarray(xf[i * NB:(i + 1) * NB])
        m["c"] = np.ascontiguousarray(cf[i * NB:(i + 1) * NB])
        in_maps.append(m)
    res = run_bass_kernel_spmd(nc, in_maps, core_ids=list(range(ncores)))
    return res


def kernel(**inputs):
    res = _run(inputs)
    return np.concatenate([np.asarray(r["y"], dtype=np.float32) for r in res.results], axis=0)
```

```python
import math
from contextlib import ExitStack
import numpy as np
import concourse.bass as bass
import concourse.mybir as mybir
from concourse.bass_utils import run_bass_kernel_spmd

F32 = mybir.dt.float32
BF16 = mybir.dt.bfloat16
AF = mybir.ActivationFunctionType
ALU = mybir.AluOpType

P = 128
S = 2048
D = 1024
DC = 8
NTS = 4
TS = 512
FF = 2816
FC = 22
NB = 2
EPS = 1e-6
ATT_GROUPS = ((128, 1), (512, 4), (2048, 16))
OFF_AQ, OFF_AK, OFF_AV = 0, 768, 1536
OFF_GQ, OFF_GK, OFF_GV, OFF_GR, OFF_GD, OFF_GA, OFF_GG = 2304, 2816, 3328, 4352, 5376, 5392, 6416
IN_W = 7440
SLOPES = [2.0 ** (-8.0 * (i + 1) / 12.0) for i in range(12)]
C_ID, C_STEP, C_CAUS, C_BD = 0, 128, 384, 512
C_TOT = 640


class Prog:
    ENG = ("pe", "act", "dve", "pool", "sp")

    def __init__(self):
        self.ins = {e: [] for e in self.ENG}
        self.lastw = {}
        self.readers = {}
        self.waited = {e: {} for e in self.ENG}
        self.dcount = {}
        self.group_slots = set()

    def _deps(self, eng, reads, writes):
        toks = []
        for k in reads:
            t = self.lastw.get(k)
            if t is not None:
                toks.append(t)
        for k in writes:
            t = self.lastw.get(k)
            if t is not None:
                toks.append(t)
            toks.extend(self.readers.get(k, ()))
        need = {}
        for t in toks:
            src = (t[0], t[1])
            if t[0] == "e" and t[1] == eng and eng in ("pe", "sp"):
                continue
            if need.get(src, -1) < t[2]:
                need[src] = t[2]
        waits = []
        wd = self.waited[eng]
        for src, idx in need.items():
            if wd.get(src, -1) >= idx:
                continue
            wd[src] = idx
            if src[0] == "e":
                self.ins[src[1]][idx]["sig"] = True
            waits.append((src, idx))
        return waits

    def _commit(self, tok, reads, writes):
        for k in reads:
            self.readers.setdefault(k, []).append(tok)
        for k in writes:
            self.lastw[k] = tok
            self.readers[k] = []

    def op(self, eng, fn, reads=(), writes=()):
        waits = self._deps(eng, reads, writes)
        idx = len(self.ins[eng])
        self.ins[eng].append(dict(fn=fn, waits=waits, sig=False, dma=None))
        self._commit(("e", eng, idx), reads, writes)

    def dma(self, fn, slot, reads=(), writes=(), group=False, queue="sp"):
        waits = self._deps(queue, reads, writes)
        self.dcount[slot] = self.dcount.get(slot, 0) + 1
        if group:
            self.group_slots.add(slot)
            waits = [w for w in waits if not (w[0][0] == "d" and w[0][1] == slot)]
        self.ins[queue].append(dict(fn=fn, waits=waits, sig=False, dma=slot))
        self._commit(("d", slot, self.dcount[slot]), reads, writes)

    def transfer(self, old_keys, new_keys):
        toks = []
        for k in old_keys:
            t = self.lastw.get(k)
            if t is not None:
                toks.append(t)
            toks.extend(self.readers.get(k, ()))
        best = {}
        for t in toks:
            s = (t[0], t[1])
            if best.get(s, -1) < t[2]:
                best[s] = t[2]
        toks = [(s[0], s[1], i) for s, i in best.items()]
        for k in new_keys:
            self.lastw[k] = None
            self.readers[k] = list(toks)

    def emit(self, nc, block, sems, dsems, final_waits):
        signo = {}
        for e in self.ENG:
            c = 0
            arr = []
            for it in self.ins[e]:
                if it["sig"]:
                    c += 1
                arr.append(c)
            signo[e] = arr

        def run(e, eng):
            for it in self.ins[e]:
                for src, idx in it["waits"]:
                    if src[0] == "e":
                        eng.wait_ge(sems[src[1]], signo[src[1]][idx])
                    else:
                        cnt = self.dcount[src[1]] if src[1] in self.group_slots else idx
                        eng.wait_ge(dsems[src[1]], 16 * cnt)
                r = getattr(eng, it["fn"][0])(**it["fn"][1])
                if it["dma"] is not None:
                    r.then_inc(dsems[it["dma"]], 16)
                elif it["sig"]:
                    r.then_inc(sems[e], 1)
            if e == "sp":
                for slot in final_waits:
                    if self.dcount.get(slot, 0):
                        eng.wait_ge(dsems[slot], 16 * self.dcount[slot])

        @block.tensor
        def _(eng):
            run("pe", eng)

        @block.scalar
        def _(eng):
            run("act", eng)

        @block.vector
        def _(eng):
            run("dve", eng)

        @block.gpsimd
        def _(eng):
            run("pool", eng)

        @block.sync
        def _(eng):
            run("sp", eng)


def build_nc(upto=99, dbg=False, nbr=NB):
    nc = bass.Bass("TRN2", target_bir_lowering=False)

    def din(name, shape):
        return nc.dram_tensor(name, list(shape), F32, kind="ExternalInput").ap()

    x = din("x", [NB, S, D])
    c = din("c", [NB, D])
    w_mod = din("w_mod", [D, 9 * D])
    b_mod = din("b_mod", [9 * D])
    g_l = [din("g_ffn1", [D]), din("g_mix", [D]), din("g_ffn2", [D])]
    ffn_w = [(din("ffn1_w1", [D, FF]), din("ffn1_w3", [D, FF]), din("ffn1_w2", [FF, D])),
             (din("ffn2_w1", [D, FF]), din("ffn2_w3", [D, FF]), din("ffn2_w2", [FF, D]))]
    w_in = din("w_in", [D, IN_W])
    q_norm_g = din("q_norm_g", [64])
    k_norm_g = din("k_norm_g", [64])
    gla_gate_up = din("gla_gate_up", [16, 512])
    gla_gate_bias = din("gla_gate_bias", [512])
    gla_out_norm_g = din("gla_out_norm_g", [256])
    w_branch_att = din("w_branch_att", [256, D])
    w_branch_gla = din("w_branch_gla", [D, D])
    w_out = din("w_out", [D, D])
    consts = din("consts", [P, C_TOT])
    y = nc.dram_tensor("y", [NB, S, D], F32, kind="ExternalOutput").ap()
    dbg_out = None
    if dbg:
        dbg_out = nc.dram_tensor("dbg", [4, P, DC * S], F32, kind="ExternalOutput").ap()

    pg = Prog()
    es = ExitStack()

    def sb(name, shape, dt):
        return es.enter_context(nc.sbuf_tensor(name, list(shape), dt))

    with es:
        h = sb("h", [P, DC, S], F32)
        u = sb("u", [P, DC, S], BF16)
        arena = sb("arena", [P, 28 * 1024], BF16)
        NST = 2
        stg = [sb(f"stg{i}", [P, 2048], F32) for i in range(NST)]
        NWB = 4
        wbs = [sb(f"wb{i}", [P, 2048], BF16) for i in range(NWB)]
        cst = sb("cst", [P, C_TOT], F32)
        NT32 = 4
        t32 = [sb(f"t32_{i}", [P, TS], F32) for i in range(NT32)]
        NT16 = 4
        t16 = [sb(f"t16_{i}", [P, TS], BF16) for i in range(NT16)]
        ident_bf = sb("ident_bf", [P, P], BF16)
        ones_bf = sb("ones_bf", [P, P], BF16)
        bd_bf = sb("bd_bf", [P, P], BF16)
        kst = sb("kst", [P, 8], F32)
        cT = sb("cT", [P, DC, NB], F32)
        cact = sb("cact", [P, DC, NB], BF16)
        bmodT = sb("bmodT", [P, 72], F32)
        modT = sb("modT", [P, 72, NB], F32)
        gT = sb("gT", [P, 3, DC], F32)
        Amod = sb("Amod", [P, 3, DC, NB], F32)
        Gmod = sb("Gmod", [P, 3, DC, NB], F32)
        qg = sb("qg", [P, 1], F32)
        kg = sb("kg", [P, 1], F32)
        negb = sb("negb", [P, 4], F32)
        gno = sb("gno", [P, 2], F32)
        gup = sb("gup", [80, 512], BF16)
        gdT = sb("gdT", [80, 2 * TS], BF16)
        Sst = sb("Sst", [P, 256], F32)
        Sbf = sb("Sbf", [P, 2, 256], BF16)
        ebl = sb("ebl", [P, 16], F32)
        kdT = sb("kdT", [P, 2, P], BF16)
        kdtm = sb("kdtm", [P, 2, P], BF16)
        amk = sb("amk", [P, 2, P], BF16)
        psum = es.enter_context(nc.psum_tensor("ps", [P, 8 * TS], F32))
        psum16 = psum.bitcast(BF16) if hasattr(psum, "bitcast") else None

        sems = {e: es.enter_context(nc.semaphore(f"s_{e}")) for e in ("pe", "act", "dve", "pool")}
        dslots = ["pro", "stg0", "stg1", "dbg"]
        dsems = {s_: es.enter_context(nc.semaphore(f"d_{s_}")) for s_ in dslots}
        block = es.enter_context(nc.Block())

        cnt = dict(bank=0, t32=0, tL=0, t16=0, stg=0, wb=0, alt=0)

        def bank():
            i = cnt["bank"] % 8
            cnt["bank"] += 1
            return psum[:, i * TS:(i + 1) * TS], ("ps", i), i

        def tmp32():
            i = cnt["t32"] % 2
            cnt["t32"] += 1
            return t32[i], ("t32", i)

        def tmpL():
            i = 2 + cnt["tL"] % 2
            cnt["tL"] += 1
            return t32[i], ("t32", i)

        def tmp16():
            i = cnt["t16"] % NT16
            cnt["t16"] += 1
            return t16[i], ("t16", i)

        def stage():
            i = cnt["stg"] % NST
            cnt["stg"] += 1
            return stg[i], ("stg", i), f"stg{i}"

        def alt():
            cnt["alt"] += 1
            return "act" if cnt["alt"] % 2 else "dve"

        def copy_op(eng, out, in_, reads, writes):
            if eng == "act":
                pg.op("act", ("copy", dict(out=out, in_=in_)), reads, writes)
            else:
                pg.op(eng, ("tensor_copy", dict(out=out, in_=in_)), reads, writes)

        def load_w(src, a, b_, ceng="pool"):
            st, skey, sslot = stage()
            n = a * b_
            stv = st[:, 0:n].rearrange("p (a b) -> p a b", a=a)
            pg.dma(("dma_start", dict(out=stv, in_=src)), sslot, writes=[skey])
            i = cnt["wb"] % NWB
            cnt["wb"] += 1
            wv = wbs[i][:, 0:n].rearrange("p (a b) -> p a b", a=a)
            copy_op(ceng, wv, stv, [skey], [("wb", i)])
            return wv, ("wb", i)

        def kc_view(w, c0, ncol):
            return w.rearrange("(kc p) f -> p kc f", p=P)[:, :, c0:c0 + ncol]

        def mm(out, lhsT, rhs, start, stop, reads, writes):
            pg.op("pe", ("matmul", dict(out=out, lhsT=lhsT, rhs=rhs, start=start, stop=stop)),
                  reads, writes)

        ukeys = [("u", t) for t in range(NTS)]

        def pro(out, in_, key, slow=False):
            if slow:
                pg.dma(("dma_start", dict(out=out, in_=in_, allow_slow_non_contiguous=True)), "pro",
                       writes=[key], group=True)
            else:
                pg.dma(("dma_start", dict(out=out, in_=in_)), "pro", writes=[key], group=True)

        pro(cst[:], consts, "cst")
        for b in range(NB):
            pro(cT[:, :, b], c[b].rearrange("(kc p) -> p kc", p=P), ("cT", b), slow=True)
        pro(bmodT[:], b_mod.rearrange("(n p) -> p n", p=P), "bmodT", slow=True)
        for l in range(3):
            pro(gT[:, l, :], g_l[l].rearrange("(n p) -> p n", p=P), ("gT", l), slow=True)
        for hh in range(2):
            pro(qg[hh * 64:(hh + 1) * 64, :], q_norm_g.rearrange("(p o) -> p o", o=1), ("qg", hh), slow=True)
            pro(kg[hh * 64:(hh + 1) * 64, :], k_norm_g.rearrange("(p o) -> p o", o=1), ("kg", hh), slow=True)
        pro(negb[:], gla_gate_bias.rearrange("(n p) -> p n", p=P), "negb", slow=True)
        pro(gno[:], gla_out_norm_g.rearrange("(n p) -> p n", p=P), "gno", slow=True)

        pg.op("dve", ("memset", dict(ap=ones_bf[:], constant=1.0)), [], ["ones"])
        pg.op("dve", ("memset", dict(ap=kst[:, 0:1], constant=EPS)), [], ["kst0"])
        pg.op("dve", ("memset", dict(ap=kst[:, 1:2], constant=1.0)), [], ["kst1"])
        pg.op("dve", ("memset", dict(ap=kst[:, 2:3], constant=math.log(128.0 ** -0.5))), [], ["kst2"])
        pg.op("dve", ("tensor_copy", dict(out=ident_bf[:], in_=cst[:, C_ID:C_ID + P])), ["cst"], ["identbf"])
        pg.op("dve", ("tensor_copy", dict(out=bd_bf[:], in_=cst[:, C_BD:C_BD + P])), ["cst"], ["bdbf"])
        for pb_ in (0, 32, 64):
            pro(t32[0][pb_:pb_ + 16, :], gla_gate_up, ("gupst", pb_))
        for pb_ in (0, 32, 64):
            pg.op("dve", ("tensor_copy", dict(out=gup[pb_:pb_ + 16, :], in_=t32[0][pb_:pb_ + 16, :])),
                  [("gupst", pb_)], ["gup", ("t32", 0)])
        pg.op("dve", ("tensor_scalar", dict(out=negb[:], in0=negb[:], scalar1=-1.0, scalar2=None, op0=ALU.mult)),
              ["negb"], ["negb"])
        pg.op("dve", ("tensor_scalar", dict(out=qg[:], in0=qg[:], scalar1=0.125, scalar2=None, op0=ALU.mult)),
              [("qg", 0), ("qg", 1)], [("qg", 0), ("qg", 1)])
        pg.op("act", ("activation", dict(out=cact[:], in_=cT[:], func=AF.Silu)), [("cT", 0), ("cT", 1)], ["cact"])
        ident = cst[:, C_ID:C_ID + P]
        stepsM = cst[:, C_STEP:C_STEP + 256]
        causM = cst[:, C_CAUS:C_CAUS + P]
        scanM = ones_bf[:]

        bg = []

        def run_bg(n=1):
            for _ in range(n):
                if bg:
                    bg.pop(0)()

        def mod_tile(t, ceng="pool"):
            l = t // 12
            wv, wk = load_w(kc_view(w_mod, t * 256, 256), DC, 256, ceng)
            pm, pmk, _ = bank()
            for j in range(2):
                for kc in range(DC):
                    mm(pm[:, j * 2:j * 2 + 2], wv[:, kc, j * P:(j + 1) * P], cact[:, kc, :],
                       kc == 0, kc == DC - 1, [wk, "cact"], [pmk])
            for b in range(NB):
                pg.op("dve", ("tensor_tensor", dict(out=modT[:, 2 * t:2 * t + 2, b], in0=pm[:, b:4:2],
                                                    in1=bmodT[:, 2 * t:2 * t + 2], op=ALU.add)),
                      [pmk, "bmodT"], [("modT", l, b)])

        def mod_fin(l):
            coef = 1.0 if l == 1 else 0.5
            for b in range(NB):
                pg.op("dve", ("scalar_tensor_tensor", dict(
                    out=Amod[:, l, :, b], in0=modT[:, l * 24 + 8:l * 24 + 16, b], scalar=1.0, in1=gT[:, l, :],
                    op0=ALU.add, op1=ALU.mult)), [("modT", l, b), ("gT", l)], [("Amod", l, b)])
                pg.op("dve", ("tensor_scalar", dict(
                    out=Gmod[:, l, :, b], in0=modT[:, l * 24 + 16:l * 24 + 24, b], scalar1=1.0, scalar2=coef,
                    op0=ALU.add, op1=ALU.mult)), [("modT", l, b)], [("Gmod", l, b)])

        HOIST = []

        PRO_ENG = ("dve", "act", "pool")

        def prologue_between(tt):
            if tt < 12:
                mod_tile(tt, PRO_ENG[tt % 3])
        for l in (1, 2):
            for t in range(12 * l, 12 * l + 12):
                bg.append(lambda t=t: mod_tile(t))
            bg.append(lambda l=l: mod_fin(l))

        def load_x(b, tts=range(16), between=None):
            for tt in tts:
                if between is not None:
                    between(tt)
                st, skey, sslot = stage()
                xs = st[:, 0:D]
                pg.dma(("dma_start", dict(out=xs, in_=x[b, tt * P:(tt + 1) * P, :])), sslot,
                       writes=[skey])
                for half in range(2):
                    pb, pk, _ = bank()
                    for j in range(4):
                        dc = half * 4 + j
                        pg.op("pe", ("transpose", dict(
                            out=pb[:, j * P:(j + 1) * P], in_=xs[:, dc * P:(dc + 1) * P], identity=ident)),
                            [skey, "cst"], [pk])
                    copy_op(alt(), h[:, half * 4:half * 4 + 4, tt * P:(tt + 1) * P],
                            pb.rearrange("p (a t) -> p a t", a=4), [pk], [("h", tt // 4)])

        def store_y(b, tts=range(16)):
            for tt in tts:
                st, skey, sslot = stage()
                ys = st[:, 0:D]
                for half in range(2):
                    pb, pk, _ = bank()
                    for j in range(4):
                        dc = half * 4 + j
                        pg.op("pe", ("transpose", dict(
                            out=pb[:, j * P:(j + 1) * P], in_=h[:, dc, tt * P:(tt + 1) * P], identity=ident)),
                            [("h", tt // 4), "cst"], [pk])
                    copy_op(alt(), ys[:, half * 512:(half + 1) * 512], pb, [pk], [skey])
                pg.dma(("dma_start", dict(out=y[b, tt * P:(tt + 1) * P, :], in_=ys)), sslot,
                       reads=[skey])

        def rstd_from(pss, pssk, scale):
            r32, rk = tmpL()
            pg.op("act", ("activation", dict(out=r32[:], in_=pss, func=AF.Ln, bias=kst[:, 0:1], scale=scale)),
                  [pssk, "kst0"], [rk])
            pg.op("act", ("activation", dict(out=r32[:], in_=r32[:], func=AF.Exp, scale=-0.5)), [rk], [rk])
            return r32, rk

        SQ_ENG = ("dve", "dve", "act", "dve", "dve", "act", "dve", "dve")

        def norm_mod(l, b, tsl=(0, 1, 2, 3)):
            def s1(ts):
                sl = slice(ts * TS, (ts + 1) * TS)
                pss, pssk, _ = bank()
                for dc in range(DC):
                    sq, sqk = tmp16()
                    e_ = SQ_ENG[dc]
                    if e_ == "act":
                        pg.op("act", ("activation", dict(out=sq[:], in_=h[:, dc, sl], func=AF.Square)), [("h", ts)], [sqk])
                    else:
                        pg.op(e_, ("tensor_tensor", dict(out=sq[:], in0=h[:, dc, sl], in1=h[:, dc, sl], op=ALU.mult)),
                              [("h", ts)], [sqk])
                    mm(pss, ones_bf[:], sq[:], dc == 0, dc == DC - 1, [sqk, "ones"], [pssk])
                return rstd_from(pss, pssk, 1.0 / D)

            def s2(ts, r32, rk):
                sl = slice(ts * TS, (ts + 1) * TS)
                for dc in range(DC):
                    tt_, tk = tmp32()
                    pg.op("dve", ("tensor_tensor", dict(out=tt_[:], in0=h[:, dc, sl], in1=r32[:], op=ALU.mult)),
                          [("h", ts), rk], [tk])
                    pg.op("act", ("activation", dict(
                        out=u[:, dc, sl], in_=tt_[:], func=AF.Identity, bias=modT[:, l * 24 + dc, b:b + 1],
                        scale=Amod[:, l, dc, b:b + 1])), [tk, ("modT", l, b), ("Amod", l, b)], [("u", ts)])

            rs = {tsl[0]: s1(tsl[0])}
            for i_, ts in enumerate(tsl):
                if i_ + 1 < len(tsl):
                    rs[tsl[i_ + 1]] = s1(tsl[i_ + 1])
                s2(ts, *rs.pop(ts))

        def gkey(f, ts):
            return ("g", f, ts)

        def ffn(fi, l, b):
            w1, w3, w2 = ffn_w[fi]
            gbuf = arena[:, 0:12 * S].rearrange("p (f t) -> p f t", f=12)
            for (f0, nf) in ((0, 12), (12, 10)):
                for tl in range(nf // 2):
                    c0 = (f0 + tl * 2) * P
                    w1v, w1k = load_w(kc_view(w1, c0, 256), DC, 256)
                    w3v, w3k = load_w(kc_view(w3, c0, 256), DC, 256)
                    for j in range(2):
                        fl = tl * 2 + j
                        for ts in range(NTS):
                            sl = slice(ts * TS, (ts + 1) * TS)
                            pa, pak, _ = bank()
                            pb, pbk, _ = bank()
                            for kc in range(DC):
                                mm(pa, w1v[:, kc, j * P:(j + 1) * P], u[:, kc, sl], kc == 0, kc == DC - 1,
                                   [w1k, ("u", ts)], [pak])
                            for kc in range(DC):
                                mm(pb, w3v[:, kc, j * P:(j + 1) * P], u[:, kc, sl], kc == 0, kc == DC - 1,
                                   [w3k, ("u", ts)], [pbk])
                            s1, s1k = tmp16()
                            pg.op("act", ("activation", dict(out=s1[:], in_=pa, func=AF.Silu)),
                                  [pak], [s1k])
                            pg.op("dve", ("tensor_tensor", dict(
                                out=gbuf[:, fl, sl], in0=pb, in1=s1[:], op=ALU.mult)), [pbk, s1k], [gkey(fl, ts)])
                    run_bg()
                w2v_all = w2.rearrange("(fc p) d -> p fc d", p=P)
                for dc in range(DC):
                    w2v, w2k = load_w(w2v_all[:, f0:f0 + nf, dc * P:(dc + 1) * P], nf, P)
                    for ts in range(NTS):
                        sl = slice(ts * TS, (ts + 1) * TS)
                        po, pok, _ = bank()
                        for fl in range(nf):
                            mm(po, w2v[:, fl, :], gbuf[:, fl, sl], fl == 0, fl == nf - 1, [w2k, gkey(fl, ts)], [pok])
                        pg.op("dve", ("scalar_tensor_tensor", dict(
                            out=h[:, dc, sl], in0=po, scalar=Gmod[:, l, dc, b:b + 1], in1=h[:, dc, sl],
                            op0=ALU.mult, op1=ALU.add)), [pok, ("Gmod", l, b), ("h", ts)], [("h", ts)])
                    run_bg()

        G_KEYS = [gkey(f, t) for f in range(12) for t in range(NTS)]

        KB = 512
        acc = arena[:, 0:32 * KB].bitcast(F32).rearrange("p (c o t) -> p c o t", c=2, o=2)
        ogla = arena[:, 0:32 * KB].rearrange("p (c t) -> p c t", c=8)
        qbuf = arena[:, 32 * KB:40 * KB].rearrange("p (c t) -> p c t", c=2)
        kbuf = arena[:, 40 * KB:48 * KB].rearrange("p (c t) -> p c t", c=2)
        vtm = arena[:, 48 * KB:56 * KB].rearrange("p (b f) -> p b f", b=16)
        oatt = arena[:, 32 * KB:40 * KB].rearrange("p (c t) -> p c t", c=2)
        gvtm = arena[:, 40 * KB:48 * KB].rearrange("p (b f) -> p b f", b=16)
        qdec = arena[:, 48 * KB:52 * KB]
        kinv = arena[:, 52 * KB:56 * KB]
        merged = arena[:, 40 * KB:56 * KB].rearrange("p (c t) -> p c t", c=8)

        ACC_KEYS = [("acc", ch, t) for ch in range(2) for t in range(NTS)]
        Q_KEYS = [("q", ch, t) for ch in range(2) for t in range(NTS)]
        K_KEYS = [("k", ch, t) for ch in range(2) for t in range(NTS)]
        V_KEYS = [("v", tb) for tb in range(16)]
        OATT_KEYS = [("oatt", t) for t in range(NTS)]
        OGLA_KEYS = [("ogla", hh, t) for hh in range(4) for t in range(NTS)]
        GV_KEYS = [("gv", tb) for tb in range(16)]
        QD_KEYS = [("qd", t) for t in range(NTS)]
        KI_KEYS = [("ki", t) for t in range(NTS)]
        KD_KEYS = []
        MG_KEYS = [("mg", dc, t) for dc in range(DC) for t in range(2)]

        def tok_slice(dil, r, n):
            st0 = r + dil * P * n
            return slice(st0, st0 + dil * (P - 1) + 1, dil)

        def blk_ts(dil, r, n):
            if dil == 1:
                return [n // 4]
            return list(range(NTS)) if dil == 16 else [n]

        def attention(b):
            for gi, (win, dil) in enumerate(ATT_GROUPS):
                nb = S // dil // P
                wq_, wqk_ = load_w(kc_view(w_in, OFF_AQ + gi * 256, 256), DC, 256)
                wk_, wkk_ = load_w(kc_view(w_in, OFF_AK + gi * 256, 256), DC, 256)
                items = [(wq_, wqk_, qbuf, qg, "q", ch, ts) for ch in range(2) for ts in range(NTS)] + \
                        [(wk_, wkk_, kbuf, kg, "k", ch, ts) for ch in range(2) for ts in range(NTS)]

                def qk_s1(it):
                    wv, wk, buf, gain, nm, ch, ts = it
                    sl = slice(ts * TS, (ts + 1) * TS)
                    pq, pqk, _ = bank()
                    for kc in range(DC):
                        mm(pq, wv[:, kc, ch * P:(ch + 1) * P], u[:, kc, sl], kc == 0, kc == DC - 1,
                           [wk, ("u", ts)], [pqk])
                    sq, sqk = tmp16()
                    pg.op("act", ("activation", dict(out=sq[:], in_=pq, func=AF.Square)), [pqk], [sqk])
                    return (pq, pqk, sq, sqk)

                def qk_s2(it, ctx):
                    wv, wk, buf, gain, nm, ch, ts = it
                    pq, pqk, sq, sqk = ctx
                    sl = slice(ts * TS, (ts + 1) * TS)
                    pss, pssk, _ = bank()
                    mm(pss, bd_bf[:], sq[:], True, True, [sqk, "bdbf"], [pssk])
                    r32, rk = rstd_from(pss, pssk, 1.0 / 64)
                    pg.op("dve", ("scalar_tensor_tensor", dict(out=buf[:, ch, sl], in0=pq, scalar=gain[:, 0:1], in1=r32[:],
                                                               op0=ALU.mult, op1=ALU.mult)),
                          [pqk, rk, (nm + "g", 0), (nm + "g", 1)], [(nm, ch, ts)])

                ctxs = {0: qk_s1(items[0])}
                for i in range(len(items)):
                    if i + 1 < len(items):
                        ctxs[i + 1] = qk_s1(items[i + 1])
                    qk_s2(items[i], ctxs.pop(i))
                wv, wk = load_w(kc_view(w_in, OFF_AV + gi * 256, 256), DC, 256)
                blocks = [(r, n) for r in range(dil) for n in range(nb)]
                for tb, (r, n) in enumerate(blocks):
                    tsl = tok_slice(dil, r, n)
                    pv, pvk, _ = bank()
                    for kc in range(DC):
                        mm(pv[:, 0:256], u[:, kc, tsl], wv[:, kc, :], kc == 0, kc == DC - 1,
                           [wk] + [("u", t) for t in blk_ts(dil, r, n)], [pvk])
                    copy_op(alt(), vtm[:, tb, :], pv[:, 0:256], [pvk], [("v", tb)])
                work = [(ch, tb) for ch in range(2) for tb in range(len(blocks))]
                pend = {}

                def emit_qk(ch, tb):
                    r, n = blocks[tb]
                    tsl = tok_slice(dil, r, n)
                    tss = blk_ts(dil, r, n)
                    Es = []
                    for hh in range(2):
                        head = gi * 4 + ch * 2 + hh
                        cs = -SLOPES[head] * dil
                        ps_, psk, _ = bank()
                        prt = slice(hh * 64, (hh + 1) * 64)
                        rd = [("q", ch, t) for t in tss] + [("k", ch, t) for t in tss]
                        mm(ps_[:, 128:256], kbuf[prt, ch, tsl], qbuf[prt, ch, tsl], True, True, rd, [psk])
                        lo = 128
                        if n > 0:
                            psl = tok_slice(dil, r, n - 1)
                            rd2 = rd + [("k", ch, t) for t in blk_ts(dil, r, n - 1)]
                            mm(ps_[:, 0:128], kbuf[prt, ch, psl], qbuf[prt, ch, tsl], True, True, rd2, [psk])
                            lo = 0
                        t_, tk = tmp32()
                        pg.op("dve", ("scalar_tensor_tensor", dict(
                            out=t_[:, lo:256], in0=stepsM[:, lo:256], scalar=cs, in1=ps_[:, lo:256],
                            op0=ALU.mult, op1=ALU.add)), [psk, "cst"], [tk])
                        E, Ek = tmp16()
                        pg.op("act", ("activation", dict(out=E[:, lo:256], in_=t_[:, lo:256],
                                                                               func=AF.Exp)), [tk], [Ek])
                        Es.append((E, Ek, lo))
                    pend[(ch, tb)] = Es

                def emit_pv(ch, tb):
                    r, n = blocks[tb]
                    tsl = tok_slice(dil, r, n)
                    tss = blk_ts(dil, r, n)
                    Es = pend.pop((ch, tb))
                    po, pok, _ = bank()
                    for hh in range(2):
                        E, Ek, lo = Es[hh]
                        prt = slice(hh * 64, (hh + 1) * 64)
                        vc = slice(ch * P + hh * 64, ch * P + hh * 64 + 64)
                        mm(po[prt, 0:128], vtm[:, tb, vc], E[:, 128:256], True, n == 0, [Ek, ("v", tb)], [pok])
                        if n > 0:
                            mm(po[prt, 0:128], vtm[:, tb - 1, vc], E[:, 0:128], False, True, [Ek, ("v", tb - 1)], [pok])
                        mm(po[prt, 128:256], ones_bf[:, 0:64], E[:, 128:256], True, n == 0, [Ek, "ones"], [pok])
                        if n > 0:
                            mm(po[prt, 128:256], ones_bf[:, 0:64], E[:, 0:128], False, True, [Ek, "ones"], [pok])
                    pov = po[:, 0:256].rearrange("p (o t) -> p o t", o=2)
                    akeys = [("acc", ch, t) for t in tss]
                    if gi == 0:
                        pg.op("dve", ("tensor_copy", dict(out=acc[:, ch, :, tsl], in_=pov)), [pok], akeys)
                    else:
                        pg.op("dve", ("tensor_tensor", dict(out=acc[:, ch, :, tsl], in0=acc[:, ch, :, tsl], in1=pov,
                                                               op=ALU.add)), [pok] + akeys, akeys)

                emit_qk(*work[0])
                for i in range(len(work)):
                    if i + 1 < len(work):
                        emit_qk(*work[i + 1])
                    emit_pv(*work[i])
            pg.transfer(Q_KEYS, OATT_KEYS)
            for ch in range(2):
                for ts in range(NTS):
                    sl = slice(ts * TS, (ts + 1) * TS)
                    r_, rk = tmp32()
                    pg.op("dve", ("reciprocal", dict(out=r_[:], in_=acc[:, ch, 1, sl])),
                          [("acc", ch, ts)], [rk])
                    pg.op("dve", ("tensor_tensor", dict(out=oatt[:, ch, sl], in0=acc[:, ch, 0, sl],
                                                                              in1=r_[:], op=ALU.mult)),
                          [("acc", ch, ts), rk], [("oatt", ts)])

        def gd_pb(ts):
            return 0 if ts == 3 else 32 * ts

        def gd_ap(ts):
            c0 = TS if ts == 3 else 0
            return gdT[gd_pb(ts):gd_pb(ts) + 16, c0:c0 + TS]

        def gla(b):
            wv, wk = load_w(kc_view(w_in, OFF_GD, 16), DC, 16)
            for ts in range(NTS):
                sl = slice(ts * TS, (ts + 1) * TS)
                pd, pdk, _ = bank()
                for kc in range(DC):
                    mm(pd[gd_pb(ts):gd_pb(ts) + 16, :], wv[:, kc, :], u[:, kc, sl], kc == 0, kc == DC - 1, [wk, ("u", ts)], [pdk])
                copy_op("act", gd_ap(ts), pd[gd_pb(ts):gd_pb(ts) + 16, :], [pdk], [("gd", ts)])

            def outnorm_units(hh):
                units = []
                st_ = {}

                def load():
                    st_["w"] = load_w(kc_view(w_in, OFF_GR + hh * 256, 256), DC, 256)
                units.append(load)
                for ts in range(NTS):
                    sl = slice(ts * TS, (ts + 1) * TS)

                    def u_ss_a(ts=ts, sl=sl):
                        pss, pssk, _ = bank()
                        for dv in range(2):
                            sq, sqk = tmp16()
                            pg.op("act", ("activation", dict(out=sq[:], in_=ogla[:, hh * 2 + dv, sl], func=AF.Square)),
                                  [("ogla", hh, ts)], [sqk])
                            mm(pss, ones_bf[:], sq[:], dv == 0, dv == 1, [sqk, "ones"], [pssk])
                        st_[("ss", ts)] = (pss, pssk)

                    def u_ss_b(ts=ts):
                        pss, pssk = st_[("ss", ts)]
                        st_[ts] = rstd_from(pss, pssk, 1.0 / 256)
                    units.append(u_ss_a)
                    units.append(u_ss_b)
                    for dv in range(2):
                        def u_dv_a(ts=ts, sl=sl, dv=dv):
                            wv_, wk_ = st_["w"]
                            pr, prk, _ = bank()
                            for kc in range(DC):
                                mm(pr, wv_[:, kc, dv * P:(dv + 1) * P], u[:, kc, sl], kc == 0, kc == DC - 1,
                                   [wk_, ("u", ts)], [prk])
                            sg, sgk = tmp32()
                            pg.op("act", ("activation", dict(out=sg[:], in_=pr, func=AF.Silu)), [prk], [sgk])
                            st_[("sg", ts, dv)] = (sg, sgk)

                        def u_dv_b(ts=ts, sl=sl, dv=dv):
                            r32, rk = st_[ts]
                            sg, sgk = st_[("sg", ts, dv)]
                            t1, t1k = tmp32()
                            pg.op("dve", ("scalar_tensor_tensor", dict(
                                out=t1[:], in0=ogla[:, hh * 2 + dv, sl], scalar=gno[:, dv:dv + 1], in1=r32[:],
                                op0=ALU.mult, op1=ALU.mult)), [("ogla", hh, ts), rk, "gno"], [t1k])
                            pg.op("dve", ("tensor_tensor", dict(
                                out=ogla[:, hh * 2 + dv, sl], in0=t1[:], in1=sg[:], op=ALU.mult)),
                                [t1k, sgk, ("ogla", hh, ts)], [("ogla", hh, ts)])
                        units.append(u_dv_a)
                        units.append(u_dv_b)
                return units

            pending = []
            for hh in range(4):
                wq, wqk = load_w(kc_view(w_in, OFF_GQ + hh * P, P), DC, P)
                wkk, wkkk = load_w(kc_view(w_in, OFF_GK + hh * P, P), DC, P)
                wvv, wvk = load_w(kc_view(w_in, OFF_GV + hh * 256, 256), DC, 256)
                for ts in range(NTS):
                    sl = slice(ts * TS, (ts + 1) * TS)
                    px, pxk, _ = bank()
                    mm(px, gup[gd_pb(ts):gd_pb(ts) + 16, hh * P:(hh + 1) * P], gd_ap(ts), True, True, ["gup", ("gd", ts)], [pxk])
                    e_, ek = tmp32()
                    pg.op("act", ("activation", dict(out=e_[:], in_=px, func=AF.Exp, bias=negb[:, hh:hh + 1], scale=-1.0)),
                          [pxk, "negb"], [ek])
                    sp_, spk = tmp32()
                    pg.op("act", ("activation", dict(out=sp_[:], in_=e_[:], func=AF.Ln, bias=kst[:, 1:2], scale=1.0)),
                          [ek, "kst1"], [spk])
                    B_, Bk = tmpL()
                    bks = [(Bk, c4) for c4 in range(4)]
                    for c4 in range(4):
                        pg.op("dve", ("tensor_tensor_scan", dict(
                            out=B_[:, c4 * P:(c4 + 1) * P], data0=scanM, data1=sp_[:, c4 * P:(c4 + 1) * P], initial=0.0,
                            op0=ALU.mult, op1=ALU.add)), [spk, "ones"], [bks[c4]])
                    pq, pqk, _ = bank()
                    for kc in range(DC):
                        mm(pq, wq[:, kc, :], u[:, kc, sl], kc == 0, kc == DC - 1, [wqk, ("u", ts)], [pqk])
                    pk_, pkk, _ = bank()
                    for kc in range(DC):
                        mm(pk_, wkk[:, kc, :], u[:, kc, sl], kc == 0, kc == DC - 1, [wkkk, ("u", ts)], [pkk])
                    pg.op("act", ("activation", dict(
                        out=ebl[:, ts * 4:ts * 4 + 4], in_=B_[:, 127:512:128], func=AF.Exp, scale=-1.0 / 16)),
                        bks, [("ebl", ts)])
                    eb, ebk = tmp32()
                    pg.op("act", ("activation", dict(out=eb[:], in_=B_[:], func=AF.Exp, bias=kst[:, 2:3], scale=-1.0 / 16)),
                          bks + ["kst2"], [ebk])
                    pg.op("dve", ("tensor_tensor", dict(out=qdec[:, sl], in0=pq, in1=eb[:], op=ALU.mult)),
                          [pqk, ebk], [("qd", ts)])
                    en, enk = tmp32()
                    pg.op("act", ("activation", dict(out=en[:], in_=B_[:], func=AF.Exp, scale=1.0 / 16)), bks, [enk])
                    pg.op("dve", ("tensor_tensor", dict(out=kinv[:, sl], in0=pk_, in1=en[:], op=ALU.mult)),
                          [pkk, enk], [("ki", ts)])
                    for tb in range(ts * 4, ts * 4 + 4):
                        pv, pvk, _ = bank()
                        for kc in range(DC):
                            mm(pv[:, 0:256], u[:, kc, tb * P:(tb + 1) * P], wvv[:, kc, :], kc == 0, kc == DC - 1,
                               [wvk, ("u", tb // 4)], [pvk])
                        copy_op("act", gvtm[:, tb, :], pv[:, 0:256], [pvk], [("gv", tb)])

                def emit_kd(cc):
                    csl = slice(cc * P, (cc + 1) * P)
                    j = cc % 2
                    pg.op("dve", ("tensor_scalar", dict(
                        out=kdT[:, j, :], in0=kinv[:, csl], scalar1=ebl[:, cc:cc + 1], scalar2=None, op0=ALU.mult)),
                        [("ki", cc // 4), ("ebl", cc // 4)], [("kdT", j)])
                    pt, ptk, bi = bank()
                    pt16 = psum16[:, bi * 2 * TS:bi * 2 * TS + P]
                    pg.op("pe", ("transpose", dict(out=pt16, in_=kdT[:, j, :], identity=ident_bf[:])),
                          [("kdT", j), "identbf"], [ptk])
                    copy_op("act", kdtm[:, j, :], pt16, [ptk], [("kd", j)])
                emit_kd(0)
                for cc in range(16):
                    csl = slice(cc * P, (cc + 1) * P)
                    ts = cc // 4
                    pa, pak, _ = bank()
                    mm(pa[:, 0:P], kinv[:, csl], qdec[:, csl], True, True, [("ki", ts), ("qd", ts)], [pak])
                    j = cc % 2
                    if cc + 1 < 15:
                        emit_kd(cc + 1)
                    pg.op("dve", ("tensor_tensor", dict(out=amk[:, j, :], in0=pa[:, 0:P], in1=causM, op=ALU.mult)),
                          [pak, "cst"], [("amk", j)])
                    if cc < 15:
                        pu, puk, _ = bank()
                        mm(pu[:, 0:256], kdtm[:, j, :], gvtm[:, cc, :], True, True, [("kd", j), ("gv", cc)], [puk])
                    po, pok, _ = bank()
                    for dv in range(2):
                        mm(po[:, dv * P:(dv + 1) * P], gvtm[:, cc, dv * P:(dv + 1) * P], amk[:, j, :], True, cc == 0,
                           [("gv", cc), ("amk", j)], [pok])
                        if cc > 0:
                            mm(po[:, dv * P:(dv + 1) * P], Sbf[:, (cc - 1) % 2, dv * P:(dv + 1) * P], qdec[:, csl], False, True,
                               [("Sbf", (cc - 1) % 2), ("qd", ts)], [pok])
                    if cc < 15:
                        if cc == 0:
                            pg.op("dve", ("tensor_copy", dict(out=Sst[:], in_=pu[:, 0:256])), [puk], ["Sst"])
                        else:
                            pg.op("dve", ("scalar_tensor_tensor", dict(
                                out=Sst[:], in0=Sst[:], scalar=ebl[:, cc:cc + 1], in1=pu[:, 0:256],
                                op0=ALU.mult, op1=ALU.add)), [puk, "Sst", ("ebl", ts)], ["Sst"])
                        copy_op("act", Sbf[:, cc % 2, :], Sst[:], ["Sst"], [("Sbf", cc % 2)])
                    copy_op("act", ogla[:, hh * 2:hh * 2 + 2, csl], po[:, 0:256].rearrange("p (a t) -> p a t", a=2),
                            [pok], [("ogla", hh, ts)])
                    for _ in range(2):
                        if pending:
                            pending.pop(0)()
                while pending:
                    pending.pop(0)()
                pending = outnorm_units(hh)
            while pending:
                pending.pop(0)()

        def merge_out(b, after_half=None):
            wba_all = w_branch_att.rearrange("(kc p) d -> p kc d", p=P)
            for th in range(2):
                for dp in range(4):
                    c0 = dp * 256
                    wga, wgak = load_w(kc_view(w_in, OFF_GA + c0, 256), DC, 256)
                    wba, wbak = load_w(wba_all[:, :, c0:c0 + 256], 2, 256)
                    for j in range(2):
                        dc = dp * 2 + j
                        for t2 in range(2):
                            ts = th * 2 + t2
                            sl = slice(ts * TS, (ts + 1) * TS)
                            p1, p1k, _ = bank()
                            for kc in range(DC):
                                mm(p1, wga[:, kc, j * P:(j + 1) * P], u[:, kc, sl], kc == 0, kc == DC - 1,
                                   [wgak, ("u", ts)], [p1k])
                            sa, sak = tmp32()
                            pg.op("act", ("activation", dict(out=sa[:], in_=p1, func=AF.Sigmoid)),
                                  [p1k], [sak])
                            p2, p2k, _ = bank()
                            for c2 in range(2):
                                mm(p2, wba[:, c2, j * P:(j + 1) * P], oatt[:, c2, sl], c2 == 0, c2 == 1,
                                   [wbak, ("oatt", ts)], [p2k])
                            pg.op("dve", ("tensor_tensor", dict(
                                out=merged[:, dc, t2 * TS:(t2 + 1) * TS], in0=p2, in1=sa[:], op=ALU.mult)),
                                [p2k, sak], [("mg", dc, t2)])
                    wgg, wggk = load_w(kc_view(w_in, OFF_GG + c0, 256), DC, 256)
                    wbg, wbgk = load_w(kc_view(w_branch_gla, c0, 256), DC, 256)
                    for j in range(2):
                        dc = dp * 2 + j
                        for t2 in range(2):
                            ts = th * 2 + t2
                            sl = slice(ts * TS, (ts + 1) * TS)
                            p1, p1k, _ = bank()
                            for kc in range(DC):
                                mm(p1, wgg[:, kc, j * P:(j + 1) * P], u[:, kc, sl], kc == 0, kc == DC - 1,
                                   [wggk, ("u", ts)], [p1k])
                            sa, sak = tmp32()
                            pg.op("act", ("activation", dict(out=sa[:], in_=p1, func=AF.Sigmoid)),
                                  [p1k], [sak])
                            p2, p2k, _ = bank()
                            for kc in range(DC):
                                mm(p2, wbg[:, kc, j * P:(j + 1) * P], ogla[:, kc, sl], kc == 0, kc == DC - 1,
                                   [wbgk, ("ogla", kc // 2, ts)], [p2k])
                            m2, m2k = tmp32()
                            pg.op("dve", ("tensor_tensor", dict(out=m2[:], in0=p2, in1=sa[:],
                                                                                         op=ALU.mult)), [p2k, sak], [m2k])
                            pg.op("dve", ("tensor_tensor", dict(
                                out=merged[:, dc, t2 * TS:(t2 + 1) * TS], in0=merged[:, dc, t2 * TS:(t2 + 1) * TS],
                                in1=m2[:], op=ALU.add)), [m2k, ("mg", dc, t2)], [("mg", dc, t2)])
                for dp in range(4):
                    wo, wok = load_w(kc_view(w_out, dp * 256, 256), DC, 256)
                    for j in range(2):
                        dc = dp * 2 + j
                        for t2 in range(2):
                            ts = th * 2 + t2
                            sl = slice(ts * TS, (ts + 1) * TS)
                            po, pok, _ = bank()
                            for kc in range(DC):
                                mm(po, wo[:, kc, j * P:(j + 1) * P], merged[:, kc, t2 * TS:(t2 + 1) * TS], kc == 0,
                                   kc == DC - 1, [wok, ("mg", kc, t2)], [pok])
                            pg.op("dve", ("scalar_tensor_tensor", dict(
                                out=h[:, dc, sl], in0=po, scalar=Gmod[:, 1, dc, b:b + 1], in1=h[:, dc, sl],
                                op0=ALU.mult, op1=ALU.add)), [pok, ("Gmod", 1, b), ("h", ts)], [("h", ts)])
                if after_half is not None:
                    after_half(th)

        def dump(slot_i, b):
            if dbg and b == 0:
                pg.dma(("dma_start", dict(out=dbg_out[slot_i].rearrange("p (c t) -> p c t", c=DC), in_=h[:])), "dbg",
                       reads=[("h", t) for t in range(NTS)])

        MIX_A = ACC_KEYS + Q_KEYS + K_KEYS + V_KEYS
        for b in range(nbr):
            if b == 0:
                load_x(b, between=prologue_between)
                mod_fin(0)
            norm_mod(0, b)
            if upto >= 1:
                ffn(0, 0, b)
            dump(0, b)
            run_bg(len(bg))
            if upto >= 2:
                norm_mod(1, b)
                pg.transfer(G_KEYS, MIX_A + KD_KEYS)
                attention(b)
                pg.transfer(ACC_KEYS, OGLA_KEYS)
                pg.transfer(K_KEYS, GV_KEYS)
                pg.transfer(V_KEYS, QD_KEYS + KI_KEYS)
                gla(b)
                pg.transfer(GV_KEYS + QD_KEYS + KI_KEYS, MG_KEYS)
                if upto >= 3:
                    merge_out(b, after_half=lambda th: norm_mod(2, b, (2 * th, 2 * th + 1)))
                else:
                    merge_out(b)
                dump(1, b)
            if upto >= 3:
                pg.transfer(OGLA_KEYS + OATT_KEYS + MG_KEYS + KD_KEYS + MIX_A + GV_KEYS + QD_KEYS + KI_KEYS, G_KEYS)
                ffn(1, 2, b)
            elif upto >= 2:
                pg.transfer(OGLA_KEYS + OATT_KEYS + MG_KEYS + KD_KEYS + MIX_A + GV_KEYS + QD_KEYS + KI_KEYS, G_KEYS)
            if b + 1 < nbr:
                for ts in range(NTS):
                    store_y(b, range(ts * 4, ts * 4 + 4))
                    load_x(b + 1, range(ts * 4, ts * 4 + 4))
            else:
                store_y(b)

        pg.emit(nc, block, sems, dsems, final_waits=["stg0", "stg1", "dbg"])
    return nc


def make_consts():
    cs = np.zeros((P, C_TOT), np.float32)
    cs[:, C_ID:C_ID + P] = np.eye(P, dtype=np.float32)
    kk = np.arange(P)[:, None]
    qq = np.arange(P)[None, :]
    BIG = 1.0e4
    prev = np.where(qq <= kk, (qq + P - kk).astype(np.float32), BIG)
    cur = np.where(qq >= kk, (qq - kk).astype(np.float32), BIG)
    cs[:, C_STEP:C_STEP + P] = prev
    cs[:, C_STEP + P:C_STEP + 2 * P] = cur
    cs[:, C_CAUS:C_CAUS + P] = (kk <= qq).astype(np.float32)
    bd = np.zeros((P, P), np.float32)
    bd[:64, :64] = 1.0
    bd[64:, 64:] = 1.0
    cs[:, C_BD:C_BD + P] = bd
    return cs


_NC_CACHE = {}


def _run(inputs, upto=99, dbg=False, ncores=8):
    key = (upto, dbg)
    if key not in _NC_CACHE:
        _NC_CACHE[key] = build_nc(upto, dbg)
    nc = _NC_CACHE[key]
    f = lambda a: np.ascontiguousarray(np.asarray(a, dtype=np.float32))
    sq = lambda a: f(a)[0]
    shared = {
        "w_mod": sq(inputs["w_mod"]), "b_mod": sq(inputs["b_mod"]),
        "g_ffn1": sq(inputs["g_ffn1"]), "g_mix": sq(inputs["g_mix"]), "g_ffn2": sq(inputs["g_ffn2"]),
        "ffn1_w1": sq(inputs["ffn1_w1"]), "ffn1_w3": sq(inputs["ffn1_w3"]), "ffn1_w2": sq(inputs["ffn1_w2"]),
        "ffn2_w1": sq(inputs["ffn2_w1"]), "ffn2_w3": sq(inputs["ffn2_w3"]), "ffn2_w2": sq(inputs["ffn2_w2"]),
        "w_in": sq(inputs["w_in"]), "q_norm_g": sq(inputs["q_norm_g"]), "k_norm_g": sq(inputs["k_norm_g"]),
        "gla_gate_up": sq(inputs["gla_gate_up"]), "gla_gate_bias": sq(inputs["gla_gate_bias"]),
        "gla_out_norm_g": sq(inputs["gla_out_norm_g"]), "w_branch_att": sq(inputs["w_branch_att"]),
        "w_branch_gla": sq(inputs["w_branch_gla"]), "w_out": sq(inputs["w_out"]),
        "consts": make_consts(),
    }
    xf = f(inputs["x"])
    cf = f(inputs["c"])
    in_maps = []
    for i in range(ncores):
        m = dict(shared)
        m["x"] = np.ascontiguousarray(xf[i * NB:(i + 1) * NB])
        m["c"] = np.ascontiguousarray(cf[i * NB:(i + 1) * NB])
        in_maps.append(m)
    res = run_bass_kernel_spmd(nc, in_maps, core_ids=list(range(ncores)))
    return res


def kernel(**inputs):
    res = _run(inputs)
    return np.concatenate([np.asarray(r["y"], dtype=np.float32) for r in res.results], axis=0)
```

```python
import math
from contextlib import ExitStack
import numpy as np
import concourse.bass as bass
import concourse.mybir as mybir
from concourse.bass_utils import run_bass_kernel_spmd

F32 = mybir.dt.float32
BF16 = mybir.dt.bfloat16
AF = mybir.ActivationFunctionType
ALU = mybir.AluOpType

P = 128
S = 2048
D = 1024
DC = 8
NTS = 4
TS = 512
FF = 2816
FC = 22
NB = 2
EPS = 1e-6
ATT_GROUPS = ((128, 1), (512, 4), (2048, 16))
OFF_AQ, OFF_AK, OFF_AV = 0, 768, 1536
OFF_GQ, OFF_GK, OFF_GV, OFF_GR, OFF_GD, OFF_GA, OFF_GG = 2304, 2816, 3328, 4352, 5376, 5392, 6416
IN_W = 7440
SLOPES = [2.0 ** (-8.0 * (i + 1) / 12.0) for i in range(12)]
C_ID, C_STEP, C_CAUS, C_BD = 0, 128, 384, 512
C_TOT = 640


class Prog:
    ENG = ("pe", "act", "dve", "pool", "sp")

    def __init__(self):
        self.ins = {e: [] for e in self.ENG}
        self.lastw = {}
        self.readers = {}
        self.waited = {e: {} for e in self.ENG}
        self.dcount = {}
        self.group_slots = set()

    def _deps(self, eng, reads, writes):
        toks = []
        for k in reads:
            t = self.lastw.get(k)
            if t is not None:
                toks.append(t)
        for k in writes:
            t = self.lastw.get(k)
            if t is not None:
                toks.append(t)
            toks.extend(self.readers.get(k, ()))
        need = {}
        for t in toks:
            src = (t[0], t[1])
            if t[0] == "e" and t[1] == eng and eng in ("pe", "sp"):
                continue
            if need.get(src, -1) < t[2]:
                need[src] = t[2]
        waits = []
        wd = self.waited[eng]
        for src, idx in need.items():
            if wd.get(src, -1) >= idx:
                continue
            wd[src] = idx
            if src[0] == "e":
                self.ins[src[1]][idx]["sig"] = True
            waits.append((src, idx))
        return waits

    def _commit(self, tok, reads, writes):
        for k in reads:
            self.readers.setdefault(k, []).append(tok)
        for k in writes:
            self.lastw[k] = tok
            self.readers[k] = []

    def op(self, eng, fn, reads=(), writes=()):
        waits = self._deps(eng, reads, writes)
        idx = len(self.ins[eng])
        self.ins[eng].append(dict(fn=fn, waits=waits, sig=False, dma=None))
        self._commit(("e", eng, idx), reads, writes)

    def dma(self, fn, slot, reads=(), writes=(), group=False, queue="sp"):
        waits = self._deps(queue, reads, writes)
        self.dcount[slot] = self.dcount.get(slot, 0) + 1
        if group:
            self.group_slots.add(slot)
            waits = [w for w in waits if not (w[0][0] == "d" and w[0][1] == slot)]
        self.ins[queue].append(dict(fn=fn, waits=waits, sig=False, dma=slot))
        self._commit(("d", slot, self.dcount[slot]), reads, writes)

    def transfer(self, old_keys, new_keys):
        toks = []
        for k in old_keys:
            t = self.lastw.get(k)
            if t is not None:
                toks.append(t)
            toks.extend(self.readers.get(k, ()))
        best = {}
        for t in toks:
            s = (t[0], t[1])
            if best.get(s, -1) < t[2]:
                best[s] = t[2]
        toks = [(s[0], s[1], i) for s, i in best.items()]
        for k in new_keys:
            self.lastw[k] = None
            self.readers[k] = list(toks)

    def emit(self, nc, block, sems, dsems, final_waits):
        signo = {}
        for e in self.ENG:
            c = 0
            arr = []
            for it in self.ins[e]:
                if it["sig"]:
                    c += 1
                arr.append(c)
            signo[e] = arr

        def run(e, eng):
            for it in self.ins[e]:
                for src, idx in it["waits"]:
                    if src[0] == "e":
                        eng.wait_ge(sems[src[1]], signo[src[1]][idx])
                    else:
                        cnt = self.dcount[src[1]] if src[1] in self.group_slots else idx
                        eng.wait_ge(dsems[src[1]], 16 * cnt)
                r = getattr(eng, it["fn"][0])(**it["fn"][1])
                if it["dma"] is not None:
                    r.then_inc(dsems[it["dma"]], 16)
                elif it["sig"]:
                    r.then_inc(sems[e], 1)
            if e == "sp":
                for slot in final_waits:
                    if self.dcount.get(slot, 0):
                        eng.wait_ge(dsems[slot], 16 * self.dcount[slot])

        @block.tensor
        def _(eng):
            run("pe", eng)

        @block.scalar
        def _(eng):
            run("act", eng)

        @block.vector
        def _(eng):
            run("dve", eng)

        @block.gpsimd
        def _(eng):
            run("pool", eng)

        @block.sync
        def _(eng):
            run("sp", eng)


def build_nc(upto=99, dbg=False, nbr=NB):
    nc = bass.Bass("TRN2", target_bir_lowering=False)

    def din(name, shape):
        return nc.dram_tensor(name, list(shape), F32, kind="ExternalInput").ap()

    x = din("x", [NB, S, D])
    c = din("c", [NB, D])
    w_mod = din("w_mod", [D, 9 * D])
    b_mod = din("b_mod", [9 * D])
    g_l = [din("g_ffn1", [D]), din("g_mix", [D]), din("g_ffn2", [D])]
    ffn_w = [(din("ffn1_w1", [D, FF]), din("ffn1_w3", [D, FF]), din("ffn1_w2", [FF, D])),
             (din("ffn2_w1", [D, FF]), din("ffn2_w3", [D, FF]), din("ffn2_w2", [FF, D]))]
    w_in = din("w_in", [D, IN_W])
    q_norm_g = din("q_norm_g", [64])
    k_norm_g = din("k_norm_g", [64])
    gla_gate_up = din("gla_gate_up", [16, 512])
    gla_gate_bias = din("gla_gate_bias", [512])
    gla_out_norm_g = din("gla_out_norm_g", [256])
    w_branch_att = din("w_branch_att", [256, D])
    w_branch_gla = din("w_branch_gla", [D, D])
    w_out = din("w_out", [D, D])
    consts = din("consts", [P, C_TOT])
    y = nc.dram_tensor("y", [NB, S, D], F32, kind="ExternalOutput").ap()
    dbg_out = None
    if dbg:
        dbg_out = nc.dram_tensor("dbg", [4, P, DC * S], F32, kind="ExternalOutput").ap()

    pg = Prog()
    es = ExitStack()

    def sb(name, shape, dt):
        return es.enter_context(nc.sbuf_tensor(name, list(shape), dt))

    with es:
        h = sb("h", [P, DC, S], F32)
        u = sb("u", [P, DC, S], BF16)
        arena = sb("arena", [P, 28 * 1024], BF16)
        NST = 2
        stg = [sb(f"stg{i}", [P, 2048], F32) for i in range(NST)]
        NWB = 4
        wbs = [sb(f"wb{i}", [P, 2048], BF16) for i in range(NWB)]
        cst = sb("cst", [P, C_TOT], F32)
        NT32 = 4
        t32 = [sb(f"t32_{i}", [P, TS], F32) for i in range(NT32)]
        NT16 = 4
        t16 = [sb(f"t16_{i}", [P, TS], BF16) for i in range(NT16)]
        ident_bf = sb("ident_bf", [P, P], BF16)
        ones_bf = sb("ones_bf", [P, P], BF16)
        bd_bf = sb("bd_bf", [P, P], BF16)
        kst = sb("kst", [P, 8], F32)
        cT = sb("cT", [P, DC, NB], F32)
        cact = sb("cact", [P, DC, NB], BF16)
        bmodT = sb("bmodT", [P, 72], F32)
        modT = sb("modT", [P, 72, NB], F32)
        gT = sb("gT", [P, 3, DC], F32)
        Amod = sb("Amod", [P, 3, DC, NB], F32)
        Gmod = sb("Gmod", [P, 3, DC, NB], F32)
        qg = sb("qg", [P, 1], F32)
        kg = sb("kg", [P, 1], F32)
        negb = sb("negb", [P, 4], F32)
        gno = sb("gno", [P, 2], F32)
        gup = sb("gup", [80, 512], BF16)
        gdT = sb("gdT", [80, 2 * TS], BF16)
        Sst = sb("Sst", [P, 256], F32)
        Sbf = sb("Sbf", [P, 2, 256], BF16)
        ebl = sb("ebl", [P, 16], F32)
        kdT = sb("kdT", [P, 2, P], BF16)
        kdtm = sb("kdtm", [P, 2, P], BF16)
        amk = sb("amk", [P, 2, P], BF16)
        psum = es.enter_context(nc.psum_tensor("ps", [P, 8 * TS], F32))
        psum16 = psum.bitcast(BF16) if hasattr(psum, "bitcast") else None

        sems = {e: es.enter_context(nc.semaphore(f"s_{e}")) for e in ("pe", "act", "dve", "pool")}
        dslots = ["pro", "stg0", "stg1", "dbg", "wbx0", "wbx1", "wbx2", "wbx3"]
        dsems = {s_: es.enter_context(nc.semaphore(f"d_{s_}")) for s_ in dslots}
        block = es.enter_context(nc.Block())

        cnt = dict(bank=0, t32=0, tL=0, t16=0, stg=0, wb=0, alt=0, xs=0)

        def bank():
            i = cnt["bank"] % 8
            cnt["bank"] += 1
            return psum[:, i * TS:(i + 1) * TS], ("ps", i), i

        def tmp32():
            i = cnt["t32"] % 2
            cnt["t32"] += 1
            return t32[i], ("t32", i)

        def tmpL():
            i = 2 + cnt["tL"] % 2
            cnt["tL"] += 1
            return t32[i], ("t32", i)

        def tmp16():
            i = cnt["t16"] % NT16
            cnt["t16"] += 1
            return t16[i], ("t16", i)

        def stage():
            i = cnt["stg"] % NST
            cnt["stg"] += 1
            return stg[i], ("stg", i), f"stg{i}"

        def xstage():
            i = cnt["xs"] % (NST + NWB)
            cnt["xs"] += 1
            if i < NST:
                return stg[i][:, 0:D], ("stg", i), f"stg{i}"
            j = i - NST
            return wbs[j][:].bitcast(F32), ("wb", j), f"wbx{j}"

        def alt():
            cnt["alt"] += 1
            return "act" if cnt["alt"] % 2 else "dve"

        def copy_op(eng, out, in_, reads, writes):
            if eng == "act":
                pg.op("act", ("copy", dict(out=out, in_=in_)), reads, writes)
            else:
                pg.op(eng, ("tensor_copy", dict(out=out, in_=in_)), reads, writes)

        def load_w(src, a, b_, ceng="pool"):
            st, skey, sslot = stage()
            n = a * b_
            stv = st[:, 0:n].rearrange("p (a b) -> p a b", a=a)
            pg.dma(("dma_start", dict(out=stv, in_=src)), sslot, writes=[skey])
            i = cnt["wb"] % NWB
            cnt["wb"] += 1
            wv = wbs[i][:, 0:n].rearrange("p (a b) -> p a b", a=a)
            copy_op(ceng, wv, stv, [skey], [("wb", i)])
            return wv, ("wb", i)

        def kc_view(w, c0, ncol):
            return w.rearrange("(kc p) f -> p kc f", p=P)[:, :, c0:c0 + ncol]

        def mm(out, lhsT, rhs, start, stop, reads, writes):
            pg.op("pe", ("matmul", dict(out=out, lhsT=lhsT, rhs=rhs, start=start, stop=stop)),
                  reads, writes)

        ukeys = [("u", t) for t in range(NTS)]

        def pro(out, in_, key, slow=False):
            if slow:
                pg.dma(("dma_start", dict(out=out, in_=in_, allow_slow_non_contiguous=True)), "pro",
                       writes=[key], group=True)
            else:
                pg.dma(("dma_start", dict(out=out, in_=in_)), "pro", writes=[key], group=True)

        pro(cst[:], consts, "cst")
        for b in range(NB):
            pro(cT[:, :, b], c[b].rearrange("(kc p) -> p kc", p=P), ("cT", b), slow=True)
        pro(bmodT[:], b_mod.rearrange("(n p) -> p n", p=P), "bmodT", slow=True)
        for l in range(3):
            pro(gT[:, l, :], g_l[l].rearrange("(n p) -> p n", p=P), ("gT", l), slow=True)
        for hh in range(2):
            pro(qg[hh * 64:(hh + 1) * 64, :], q_norm_g.rearrange("(p o) -> p o", o=1), ("qg", hh), slow=True)
            pro(kg[hh * 64:(hh + 1) * 64, :], k_norm_g.rearrange("(p o) -> p o", o=1), ("kg", hh), slow=True)
        pro(negb[:], gla_gate_bias.rearrange("(n p) -> p n", p=P), "negb", slow=True)
        pro(gno[:], gla_out_norm_g.rearrange("(n p) -> p n", p=P), "gno", slow=True)

        pg.op("dve", ("memset", dict(ap=ones_bf[:], constant=1.0)), [], ["ones"])
        pg.op("dve", ("memset", dict(ap=kst[:, 0:1], constant=EPS)), [], ["kst0"])
        pg.op("dve", ("memset", dict(ap=kst[:, 1:2], constant=1.0)), [], ["kst1"])
        pg.op("dve", ("memset", dict(ap=kst[:, 2:3], constant=math.log(128.0 ** -0.5))), [], ["kst2"])
        pg.op("dve", ("tensor_copy", dict(out=ident_bf[:], in_=cst[:, C_ID:C_ID + P])), ["cst"], ["identbf"])
        pg.op("dve", ("tensor_copy", dict(out=bd_bf[:], in_=cst[:, C_BD:C_BD + P])), ["cst"], ["bdbf"])
        for pb_ in (0, 32, 64):
            pro(t32[0][pb_:pb_ + 16, :], gla_gate_up, ("gupst", pb_))
        for pb_ in (0, 32, 64):
            pg.op("dve", ("tensor_copy", dict(out=gup[pb_:pb_ + 16, :], in_=t32[0][pb_:pb_ + 16, :])),
                  [("gupst", pb_)], ["gup", ("t32", 0)])
        pg.op("dve", ("tensor_scalar", dict(out=negb[:], in0=negb[:], scalar1=-1.0, scalar2=None, op0=ALU.mult)),
              ["negb"], ["negb"])
        pg.op("dve", ("tensor_scalar", dict(out=qg[:], in0=qg[:], scalar1=0.125, scalar2=None, op0=ALU.mult)),
              [("qg", 0), ("qg", 1)], [("qg", 0), ("qg", 1)])
        pg.op("act", ("activation", dict(out=cact[:], in_=cT[:], func=AF.Silu)), [("cT", 0), ("cT", 1)], ["cact"])
        ident = cst[:, C_ID:C_ID + P]
        stepsM = cst[:, C_STEP:C_STEP + 256]
        causM = cst[:, C_CAUS:C_CAUS + P]
        scanM = ones_bf[:]

        bg = []

        def run_bg(n=1):
            for _ in range(n):
                if bg:
                    bg.pop(0)()

        def mod_tile(t, ceng="pool"):
            l = t // 12
            wv, wk = load_w(kc_view(w_mod, t * 256, 256), DC, 256, ceng)
            pm, pmk, _ = bank()
            for j in range(2):
                for kc in range(DC):
                    mm(pm[:, j * 2:j * 2 + 2], wv[:, kc, j * P:(j + 1) * P], cact[:, kc, :],
                       kc == 0, kc == DC - 1, [wk, "cact"], [pmk])
            for b in range(NB):
                pg.op("dve", ("tensor_tensor", dict(out=modT[:, 2 * t:2 * t + 2, b], in0=pm[:, b:4:2],
                                                    in1=bmodT[:, 2 * t:2 * t + 2], op=ALU.add)),
                      [pmk, "bmodT"], [("modT", l, b)])

        def mod_fin(l):
            coef = 1.0 if l == 1 else 0.5
            for b in range(NB):
                pg.op("dve", ("scalar_tensor_tensor", dict(
                    out=Amod[:, l, :, b], in0=modT[:, l * 24 + 8:l * 24 + 16, b], scalar=1.0, in1=gT[:, l, :],
                    op0=ALU.add, op1=ALU.mult)), [("modT", l, b), ("gT", l)], [("Amod", l, b)])
                pg.op("dve", ("tensor_scalar", dict(
                    out=Gmod[:, l, :, b], in0=modT[:, l * 24 + 16:l * 24 + 24, b], scalar1=1.0, scalar2=coef,
                    op0=ALU.add, op1=ALU.mult)), [("modT", l, b)], [("Gmod", l, b)])

        HOIST = []

        PRO_ENG = ("dve", "act", "pool")

        def prologue_between(tt):
            if tt < 12:
                mod_tile(tt, PRO_ENG[tt % 3])
        for l in (1, 2):
            for t in range(12 * l, 12 * l + 12):
                bg.append(lambda t=t: mod_tile(t))
            bg.append(lambda l=l: mod_fin(l))

        def load_x(b, tts=range(16), between=None):
            for tt in tts:
                if between is not None:
                    between(tt)
                xs, skey, sslot = xstage()
                pg.dma(("dma_start", dict(out=xs, in_=x[b, tt * P:(tt + 1) * P, :])), sslot,
                       writes=[skey])
                for half in range(2):
                    pb, pk, _ = bank()
                    for j in range(4):
                        dc = half * 4 + j
                        pg.op("pe", ("transpose", dict(
                            out=pb[:, j * P:(j + 1) * P], in_=xs[:, dc * P:(dc + 1) * P], identity=ident)),
                            [skey, "cst"], [pk])
                    copy_op(alt(), h[:, half * 4:half * 4 + 4, tt * P:(tt + 1) * P],
                            pb.rearrange("p (a t) -> p a t", a=4), [pk], [("h", tt // 4)])

        def store_y(b, tts=range(16)):
            for tt in tts:
                ys, skey, sslot = xstage()
                for half in range(2):
                    pb, pk, _ = bank()
                    for j in range(4):
                        dc = half * 4 + j
                        pg.op("pe", ("transpose", dict(
                            out=pb[:, j * P:(j + 1) * P], in_=h[:, dc, tt * P:(tt + 1) * P], identity=ident)),
                            [("h", tt // 4), "cst"], [pk])
                    copy_op(alt(), ys[:, half * 512:(half + 1) * 512], pb, [pk], [skey])
                pg.dma(("dma_start", dict(out=y[b, tt * P:(tt + 1) * P, :], in_=ys)), sslot,
                       reads=[skey])

        def rstd_from(pss, pssk, scale):
            r32, rk = tmpL()
            pg.op("act", ("activation", dict(out=r32[:], in_=pss, func=AF.Ln, bias=kst[:, 0:1], scale=scale)),
                  [pssk, "kst0"], [rk])
            pg.op("act", ("activation", dict(out=r32[:], in_=r32[:], func=AF.Exp, scale=-0.5)), [rk], [rk])
            return r32, rk

        SQ_ENG = ("dve", "dve", "act", "dve", "dve", "act", "dve", "dve")

        def norm_mod(l, b, tsl=(0, 1, 2, 3)):
            def s1(ts):
                sl = slice(ts * TS, (ts + 1) * TS)
                pss, pssk, _ = bank()
                for dc in range(DC):
                    sq, sqk = tmp16()
                    e_ = SQ_ENG[dc]
                    if e_ == "act":
                        pg.op("act", ("activation", dict(out=sq[:], in_=h[:, dc, sl], func=AF.Square)), [("h", ts)], [sqk])
                    else:
                        pg.op(e_, ("tensor_tensor", dict(out=sq[:], in0=h[:, dc, sl], in1=h[:, dc, sl], op=ALU.mult)),
                              [("h", ts)], [sqk])
                    mm(pss, ones_bf[:], sq[:], dc == 0, dc == DC - 1, [sqk, "ones"], [pssk])
                return rstd_from(pss, pssk, 1.0 / D)

            def s2(ts, r32, rk):
                sl = slice(ts * TS, (ts + 1) * TS)
                for dc in range(DC):
                    tt_, tk = tmp32()
                    pg.op("dve", ("tensor_tensor", dict(out=tt_[:], in0=h[:, dc, sl], in1=r32[:], op=ALU.mult)),
                          [("h", ts), rk], [tk])
                    pg.op("act", ("activation", dict(
                        out=u[:, dc, sl], in_=tt_[:], func=AF.Identity, bias=modT[:, l * 24 + dc, b:b + 1],
                        scale=Amod[:, l, dc, b:b + 1])), [tk, ("modT", l, b), ("Amod", l, b)], [("u", ts)])

            rs = {tsl[0]: s1(tsl[0])}
            for i_, ts in enumerate(tsl):
                if i_ + 1 < len(tsl):
                    rs[tsl[i_ + 1]] = s1(tsl[i_ + 1])
                s2(ts, *rs.pop(ts))

        def gkey(f, ts):
            return ("g", f, ts)

        def ffn(fi, l, b):
            w1, w3, w2 = ffn_w[fi]
            gbuf = arena[:, 0:12 * S].rearrange("p (f t) -> p f t", f=12)
            for (f0, nf) in ((0, 12), (12, 10)):
                for tl in range(nf // 2):
                    c0 = (f0 + tl * 2) * P
                    w1v, w1k = load_w(kc_view(w1, c0, 256), DC, 256)
                    w3v, w3k = load_w(kc_view(w3, c0, 256), DC, 256)
                    for j in range(2):
                        fl = tl * 2 + j
                        for ts in range(NTS):
                            sl = slice(ts * TS, (ts + 1) * TS)
                            pa, pak, _ = bank()
                            pb, pbk, _ = bank()
                            for kc in range(DC):
                                mm(pa, w1v[:, kc, j * P:(j + 1) * P], u[:, kc, sl], kc == 0, kc == DC - 1,
                                   [w1k, ("u", ts)], [pak])
                            for kc in range(DC):
                                mm(pb, w3v[:, kc, j * P:(j + 1) * P], u[:, kc, sl], kc == 0, kc == DC - 1,
                                   [w3k, ("u", ts)], [pbk])
                            s1, s1k = tmp16()
                            pg.op("act", ("activation", dict(out=s1[:], in_=pa, func=AF.Silu)),
                                  [pak], [s1k])
                            pg.op("dve", ("tensor_tensor", dict(
                                out=gbuf[:, fl, sl], in0=pb, in1=s1[:], op=ALU.mult)), [pbk, s1k], [gkey(fl, ts)])
                    run_bg()
                w2v_all = w2.rearrange("(fc p) d -> p fc d", p=P)
                for dc in range(DC):
                    w2v, w2k = load_w(w2v_all[:, f0:f0 + nf, dc * P:(dc + 1) * P], nf, P)
                    for ts in range(NTS):
                        sl = slice(ts * TS, (ts + 1) * TS)
                        po, pok, _ = bank()
                        for fl in range(nf):
                            mm(po, w2v[:, fl, :], gbuf[:, fl, sl], fl == 0, fl == nf - 1, [w2k, gkey(fl, ts)], [pok])
                        pg.op("dve", ("scalar_tensor_tensor", dict(
                            out=h[:, dc, sl], in0=po, scalar=Gmod[:, l, dc, b:b + 1], in1=h[:, dc, sl],
                            op0=ALU.mult, op1=ALU.add)), [pok, ("Gmod", l, b), ("h", ts)], [("h", ts)])
                    run_bg()

        G_KEYS = [gkey(f, t) for f in range(12) for t in range(NTS)]

        KB = 512
        acc = arena[:, 0:32 * KB].bitcast(F32).rearrange("p (c o t) -> p c o t", c=2, o=2)
        ogla = arena[:, 0:32 * KB].rearrange("p (c t) -> p c t", c=8)
        qbuf = arena[:, 32 * KB:40 * KB].rearrange("p (c t) -> p c t", c=2)
        kbuf = arena[:, 40 * KB:48 * KB].rearrange("p (c t) -> p c t", c=2)
        vtm = arena[:, 48 * KB:56 * KB].rearrange("p (b f) -> p b f", b=16)
        oatt = arena[:, 32 * KB:40 * KB].rearrange("p (c t) -> p c t", c=2)
        gvtm = arena[:, 40 * KB:48 * KB].rearrange("p (b f) -> p b f", b=16)
        qdec = arena[:, 48 * KB:52 * KB]
        kinv = arena[:, 52 * KB:56 * KB]
        merged = arena[:, 40 * KB:56 * KB].rearrange("p (c t) -> p c t", c=8)

        ACC_KEYS = [("acc", ch, t) for ch in range(2) for t in range(NTS)]
        Q_KEYS = [("q", ch, t) for ch in range(2) for t in range(NTS)]
        K_KEYS = [("k", ch, t) for ch in range(2) for t in range(NTS)]
        V_KEYS = [("v", tb) for tb in range(16)]
        OATT_KEYS = [("oatt", t) for t in range(NTS)]
        OGLA_KEYS = [("ogla", hh, t) for hh in range(4) for t in range(NTS)]
        GV_KEYS = [("gv", tb) for tb in range(16)]
        QD_KEYS = [("qd", t) for t in range(NTS)]
        KI_KEYS = [("ki", t) for t in range(NTS)]
        KD_KEYS = []
        MG_KEYS = [("mg", dc, t) for dc in range(DC) for t in range(2)]

        def tok_slice(dil, r, n):
            st0 = r + dil * P * n
            return slice(st0, st0 + dil * (P - 1) + 1, dil)

        def blk_ts(dil, r, n):
            if dil == 1:
                return [n // 4]
            return list(range(NTS)) if dil == 16 else [n]

        def attention(b):
            for gi, (win, dil) in enumerate(ATT_GROUPS):
                nb = S // dil // P
                wq_, wqk_ = load_w(kc_view(w_in, OFF_AQ + gi * 256, 256), DC, 256)
                wk_, wkk_ = load_w(kc_view(w_in, OFF_AK + gi * 256, 256), DC, 256)
                items = [(wq_, wqk_, qbuf, qg, "q", ch, ts) for ch in range(2) for ts in range(NTS)] + \
                        [(wk_, wkk_, kbuf, kg, "k", ch, ts) for ch in range(2) for ts in range(NTS)]

                def qk_s1(it):
                    wv, wk, buf, gain, nm, ch, ts = it
                    sl = slice(ts * TS, (ts + 1) * TS)
                    pq, pqk, _ = bank()
                    for kc in range(DC):
                        mm(pq, wv[:, kc, ch * P:(ch + 1) * P], u[:, kc, sl], kc == 0, kc == DC - 1,
                           [wk, ("u", ts)], [pqk])
                    sq, sqk = tmp16()
                    pg.op("act", ("activation", dict(out=sq[:], in_=pq, func=AF.Square)), [pqk], [sqk])
                    return (pq, pqk, sq, sqk)

                def qk_s2(it, ctx):
                    wv, wk, buf, gain, nm, ch, ts = it
                    pq, pqk, sq, sqk = ctx
                    sl = slice(ts * TS, (ts + 1) * TS)
                    pss, pssk, _ = bank()
                    mm(pss, bd_bf[:], sq[:], True, True, [sqk, "bdbf"], [pssk])
                    r32, rk = rstd_from(pss, pssk, 1.0 / 64)
                    pg.op("dve", ("scalar_tensor_tensor", dict(out=buf[:, ch, sl], in0=pq, scalar=gain[:, 0:1], in1=r32[:],
                                                               op0=ALU.mult, op1=ALU.mult)),
                          [pqk, rk, (nm + "g", 0), (nm + "g", 1)], [(nm, ch, ts)])

                ctxs = {0: qk_s1(items[0])}
                for i in range(len(items)):
                    if i + 1 < len(items):
                        ctxs[i + 1] = qk_s1(items[i + 1])
                    qk_s2(items[i], ctxs.pop(i))
                wv, wk = load_w(kc_view(w_in, OFF_AV + gi * 256, 256), DC, 256)
                blocks = [(r, n) for r in range(dil) for n in range(nb)]
                for tb, (r, n) in enumerate(blocks):
                    tsl = tok_slice(dil, r, n)
                    pv, pvk, _ = bank()
                    for kc in range(DC):
                        mm(pv[:, 0:256], u[:, kc, tsl], wv[:, kc, :], kc == 0, kc == DC - 1,
                           [wk] + [("u", t) for t in blk_ts(dil, r, n)], [pvk])
                    copy_op(alt(), vtm[:, tb, :], pv[:, 0:256], [pvk], [("v", tb)])
                work = [(ch, tb) for ch in range(2) for tb in range(len(blocks))]
                pend = {}

                def emit_qk(ch, tb):
                    r, n = blocks[tb]
                    tsl = tok_slice(dil, r, n)
                    tss = blk_ts(dil, r, n)
                    Es = []
                    for hh in range(2):
                        head = gi * 4 + ch * 2 + hh
                        cs = -SLOPES[head] * dil
                        ps_, psk, _ = bank()
                        prt = slice(hh * 64, (hh + 1) * 64)
                        rd = [("q", ch, t) for t in tss] + [("k", ch, t) for t in tss]
                        mm(ps_[:, 128:256], kbuf[prt, ch, tsl], qbuf[prt, ch, tsl], True, True, rd, [psk])
                        lo = 128
                        if n > 0:
                            psl = tok_slice(dil, r, n - 1)
                            rd2 = rd + [("k", ch, t) for t in blk_ts(dil, r, n - 1)]
                            mm(ps_[:, 0:128], kbuf[prt, ch, psl], qbuf[prt, ch, tsl], True, True, rd2, [psk])
                            lo = 0
                        t_, tk = tmp32()
                        pg.op("dve", ("scalar_tensor_tensor", dict(
                            out=t_[:, lo:256], in0=stepsM[:, lo:256], scalar=cs, in1=ps_[:, lo:256],
                            op0=ALU.mult, op1=ALU.add)), [psk, "cst"], [tk])
                        E, Ek = tmp16()
                        pg.op("act", ("activation", dict(out=E[:, lo:256], in_=t_[:, lo:256],
                                                                               func=AF.Exp)), [tk], [Ek])
                        Es.append((E, Ek, lo))
                    pend[(ch, tb)] = Es

                def emit_pv(ch, tb):
                    r, n = blocks[tb]
                    tsl = tok_slice(dil, r, n)
                    tss = blk_ts(dil, r, n)
                    Es = pend.pop((ch, tb))
                    po, pok, _ = bank()
                    for hh in range(2):
                        E, Ek, lo = Es[hh]
                        prt = slice(hh * 64, (hh + 1) * 64)
                        vc = slice(ch * P + hh * 64, ch * P + hh * 64 + 64)
                        mm(po[prt, 0:128], vtm[:, tb, vc], E[:, 128:256], True, n == 0, [Ek, ("v", tb)], [pok])
                        if n > 0:
                            mm(po[prt, 0:128], vtm[:, tb - 1, vc], E[:, 0:128], False, True, [Ek, ("v", tb - 1)], [pok])
                        mm(po[prt, 128:256], ones_bf[:, 0:64], E[:, 128:256], True, n == 0, [Ek, "ones"], [pok])
                        if n > 0:
                            mm(po[prt, 128:256], ones_bf[:, 0:64], E[:, 0:128], False, True, [Ek, "ones"], [pok])
                    pov = po[:, 0:256].rearrange("p (o t) -> p o t", o=2)
                    akeys = [("acc", ch, t) for t in tss]
                    if gi == 0:
                        pg.op("dve", ("tensor_copy", dict(out=acc[:, ch, :, tsl], in_=pov)), [pok], akeys)
                    else:
                        pg.op("dve", ("tensor_tensor", dict(out=acc[:, ch, :, tsl], in0=acc[:, ch, :, tsl], in1=pov,
                                                               op=ALU.add)), [pok] + akeys, akeys)

                emit_qk(*work[0])
                for i in range(len(work)):
                    if i + 1 < len(work):
                        emit_qk(*work[i + 1])
                    emit_pv(*work[i])
            pg.transfer(Q_KEYS, OATT_KEYS)
            for ch in range(2):
                for ts in range(NTS):
                    sl = slice(ts * TS, (ts + 1) * TS)
                    r_, rk = tmp32()
                    pg.op("dve", ("reciprocal", dict(out=r_[:], in_=acc[:, ch, 1, sl])),
                          [("acc", ch, ts)], [rk])
                    pg.op("dve", ("tensor_tensor", dict(out=oatt[:, ch, sl], in0=acc[:, ch, 0, sl],
                                                                              in1=r_[:], op=ALU.mult)),
                          [("acc", ch, ts), rk], [("oatt", ts)])

        def gd_pb(ts):
            return 0 if ts == 3 else 32 * ts

        def gd_ap(ts):
            c0 = TS if ts == 3 else 0
            return gdT[gd_pb(ts):gd_pb(ts) + 16, c0:c0 + TS]

        def gla(b):
            wv, wk = load_w(kc_view(w_in, OFF_GD, 16), DC, 16)
            for ts in range(NTS):
                sl = slice(ts * TS, (ts + 1) * TS)
                pd, pdk, _ = bank()
                for kc in range(DC):
                    mm(pd[gd_pb(ts):gd_pb(ts) + 16, :], wv[:, kc, :], u[:, kc, sl], kc == 0, kc == DC - 1, [wk, ("u", ts)], [pdk])
                copy_op("act", gd_ap(ts), pd[gd_pb(ts):gd_pb(ts) + 16, :], [pdk], [("gd", ts)])

            def outnorm_units(hh):
                units = []
                st_ = {}

                def load():
                    st_["w"] = load_w(kc_view(w_in, OFF_GR + hh * 256, 256), DC, 256)
                units.append(load)
                for ts in range(NTS):
                    sl = slice(ts * TS, (ts + 1) * TS)

                    def u_ss_a(ts=ts, sl=sl):
                        pss, pssk, _ = bank()
                        for dv in range(2):
                            sq, sqk = tmp16()
                            pg.op("act", ("activation", dict(out=sq[:], in_=ogla[:, hh * 2 + dv, sl], func=AF.Square)),
                                  [("ogla", hh, ts)], [sqk])
                            mm(pss, ones_bf[:], sq[:], dv == 0, dv == 1, [sqk, "ones"], [pssk])
                        st_[("ss", ts)] = (pss, pssk)

                    def u_ss_b(ts=ts):
                        pss, pssk = st_[("ss", ts)]
                        st_[ts] = rstd_from(pss, pssk, 1.0 / 256)
                    units.append(u_ss_a)
                    units.append(u_ss_b)
                    for dv in range(2):
                        def u_dv_a(ts=ts, sl=sl, dv=dv):
                            wv_, wk_ = st_["w"]
                            pr, prk, _ = bank()
                            for kc in range(DC):
                                mm(pr, wv_[:, kc, dv * P:(dv + 1) * P], u[:, kc, sl], kc == 0, kc == DC - 1,
                                   [wk_, ("u", ts)], [prk])
                            sg, sgk = tmp32()
                            pg.op("act", ("activation", dict(out=sg[:], in_=pr, func=AF.Silu)), [prk], [sgk])
                            st_[("sg", ts, dv)] = (sg, sgk)

                        def u_dv_b(ts=ts, sl=sl, dv=dv):
                            r32, rk = st_[ts]
                            sg, sgk = st_[("sg", ts, dv)]
                            t1, t1k = tmp32()
                            pg.op("dve", ("scalar_tensor_tensor", dict(
                                out=t1[:], in0=ogla[:, hh * 2 + dv, sl], scalar=gno[:, dv:dv + 1], in1=r32[:],
                                op0=ALU.mult, op1=ALU.mult)), [("ogla", hh, ts), rk, "gno"], [t1k])
                            pg.op("dve", ("tensor_tensor", dict(
                                out=ogla[:, hh * 2 + dv, sl], in0=t1[:], in1=sg[:], op=ALU.mult)),
                                [t1k, sgk, ("ogla", hh, ts)], [("ogla", hh, ts)])
                        units.append(u_dv_a)
                        units.append(u_dv_b)
                return units

            pending = []
            for hh in range(4):
                wq, wqk = load_w(kc_view(w_in, OFF_GQ + hh * P, P), DC, P)
                wkk, wkkk = load_w(kc_view(w_in, OFF_GK + hh * P, P), DC, P)
                wvv, wvk = load_w(kc_view(w_in, OFF_GV + hh * 256, 256), DC, 256)
                for ts in range(NTS):
                    sl = slice(ts * TS, (ts + 1) * TS)
                    px, pxk, _ = bank()
                    mm(px, gup[gd_pb(ts):gd_pb(ts) + 16, hh * P:(hh + 1) * P], gd_ap(ts), True, True, ["gup", ("gd", ts)], [pxk])
                    e_, ek = tmp32()
                    pg.op("act", ("activation", dict(out=e_[:], in_=px, func=AF.Exp, bias=negb[:, hh:hh + 1], scale=-1.0)),
                          [pxk, "negb"], [ek])
                    sp_, spk = tmp32()
                    pg.op("act", ("activation", dict(out=sp_[:], in_=e_[:], func=AF.Ln, bias=kst[:, 1:2], scale=1.0)),
                          [ek, "kst1"], [spk])
                    B_, Bk = tmpL()
                    bks = [(Bk, c4) for c4 in range(4)]
                    for c4 in range(4):
                        pg.op("dve", ("tensor_tensor_scan", dict(
                            out=B_[:, c4 * P:(c4 + 1) * P], data0=scanM, data1=sp_[:, c4 * P:(c4 + 1) * P], initial=0.0,
                            op0=ALU.mult, op1=ALU.add)), [spk, "ones"], [bks[c4]])
                    pq, pqk, _ = bank()
                    for kc in range(DC):
                        mm(pq, wq[:, kc, :], u[:, kc, sl], kc == 0, kc == DC - 1, [wqk, ("u", ts)], [pqk])
                    pk_, pkk, _ = bank()
                    for kc in range(DC):
                        mm(pk_, wkk[:, kc, :], u[:, kc, sl], kc == 0, kc == DC - 1, [wkkk, ("u", ts)], [pkk])
                    pg.op("act", ("activation", dict(
                        out=ebl[:, ts * 4:ts * 4 + 4], in_=B_[:, 127:512:128], func=AF.Exp, scale=-1.0 / 16)),
                        bks, [("ebl", ts)])
                    eb, ebk = tmp32()
                    pg.op("act", ("activation", dict(out=eb[:], in_=B_[:], func=AF.Exp, bias=kst[:, 2:3], scale=-1.0 / 16)),
                          bks + ["kst2"], [ebk])
                    pg.op("dve", ("tensor_tensor", dict(out=qdec[:, sl], in0=pq, in1=eb[:], op=ALU.mult)),
                          [pqk, ebk], [("qd", ts)])
                    en, enk = tmp32()
                    pg.op("act", ("activation", dict(out=en[:], in_=B_[:], func=AF.Exp, scale=1.0 / 16)), bks, [enk])
                    pg.op("dve", ("tensor_tensor", dict(out=kinv[:, sl], in0=pk_, in1=en[:], op=ALU.mult)),
                          [pkk, enk], [("ki", ts)])
                    for tb in range(ts * 4, ts * 4 + 4):
                        pv, pvk, _ = bank()
                        for kc in range(DC):
                            mm(pv[:, 0:256], u[:, kc, tb * P:(tb + 1) * P], wvv[:, kc, :], kc == 0, kc == DC - 1,
                               [wvk, ("u", tb // 4)], [pvk])
                        copy_op("act", gvtm[:, tb, :], pv[:, 0:256], [pvk], [("gv", tb)])

                def emit_kd(cc):
                    csl = slice(cc * P, (cc + 1) * P)
                    j = cc % 2
                    pg.op("dve", ("tensor_scalar", dict(
                        out=kdT[:, j, :], in0=kinv[:, csl], scalar1=ebl[:, cc:cc + 1], scalar2=None, op0=ALU.mult)),
                        [("ki", cc // 4), ("ebl", cc // 4)], [("kdT", j)])
                    pt, ptk, bi = bank()
                    pt16 = psum16[:, bi * 2 * TS:bi * 2 * TS + P]
                    pg.op("pe", ("transpose", dict(out=pt16, in_=kdT[:, j, :], identity=ident_bf[:])),
                          [("kdT", j), "identbf"], [ptk])
                    copy_op("act", kdtm[:, j, :], pt16, [ptk], [("kd", j)])
                emit_kd(0)
                for cc in range(16):
                    csl = slice(cc * P, (cc + 1) * P)
                    ts = cc // 4
                    pa, pak, _ = bank()
                    mm(pa[:, 0:P], kinv[:, csl], qdec[:, csl], True, True, [("ki", ts), ("qd", ts)], [pak])
                    j = cc % 2
                    if cc + 1 < 15:
                        emit_kd(cc + 1)
                    pg.op("dve", ("tensor_tensor", dict(out=amk[:, j, :], in0=pa[:, 0:P], in1=causM, op=ALU.mult)),
                          [pak, "cst"], [("amk", j)])
                    if cc < 15:
                        pu, puk, _ = bank()
                        mm(pu[:, 0:256], kdtm[:, j, :], gvtm[:, cc, :], True, True, [("kd", j), ("gv", cc)], [puk])
                    po, pok, _ = bank()
                    for dv in range(2):
                        mm(po[:, dv * P:(dv + 1) * P], gvtm[:, cc, dv * P:(dv + 1) * P], amk[:, j, :], True, cc == 0,
                           [("gv", cc), ("amk", j)], [pok])
                        if cc > 0:
                            mm(po[:, dv * P:(dv + 1) * P], Sbf[:, (cc - 1) % 2, dv * P:(dv + 1) * P], qdec[:, csl], False, True,
                               [("Sbf", (cc - 1) % 2), ("qd", ts)], [pok])
                    if cc < 15:
                        if cc == 0:
                            pg.op("dve", ("tensor_copy", dict(out=Sst[:], in_=pu[:, 0:256])), [puk], ["Sst"])
                        else:
                            pg.op("dve", ("scalar_tensor_tensor", dict(
                                out=Sst[:], in0=Sst[:], scalar=ebl[:, cc:cc + 1], in1=pu[:, 0:256],
                                op0=ALU.mult, op1=ALU.add)), [puk, "Sst", ("ebl", ts)], ["Sst"])
                        copy_op("act", Sbf[:, cc % 2, :], Sst[:], ["Sst"], [("Sbf", cc % 2)])
                    copy_op("act", ogla[:, hh * 2:hh * 2 + 2, csl], po[:, 0:256].rearrange("p (a t) -> p a t", a=2),
                            [pok], [("ogla", hh, ts)])
                    for _ in range(2):
                        if pending:
                            pending.pop(0)()
                while pending:
                    pending.pop(0)()
                pending = outnorm_units(hh)
            while pending:
                pending.pop(0)()

        def run_tasks(tasks):
            loaded = {}

            def ld(i):
                loaded[i] = [load_w(src, a, b_, ce) for (src, a, b_, ce) in tasks[i][0]]
            ld(0)
            for i in range(len(tasks)):
                if i + 1 < len(tasks):
                    ld(i + 1)
                tasks[i][1](loaded.pop(i))

        def merge_out(b, after_half=None):
            wba_all = w_branch_att.rearrange("(kc p) d -> p kc d", p=P)
            for th in range(2):
                def comp_a(ws, dp):
                    (wga, wgak), (wba, wbak) = ws
                    for j in range(2):
                        dc = dp * 2 + j
                        for t2 in range(2):
                            ts = th * 2 + t2
                            sl = slice(ts * TS, (ts + 1) * TS)
                            p1, p1k, _ = bank()
                            for kc in range(DC):
                                mm(p1, wga[:, kc, j * P:(j + 1) * P], u[:, kc, sl], kc == 0, kc == DC - 1,
                                   [wgak, ("u", ts)], [p1k])
                            sa, sak = tmp32()
                            pg.op("act", ("activation", dict(out=sa[:], in_=p1, func=AF.Sigmoid)), [p1k], [sak])
                            p2, p2k, _ = bank()
                            for c2 in range(2):
                                mm(p2, wba[:, c2, j * P:(j + 1) * P], oatt[:, c2, sl], c2 == 0, c2 == 1,
                                   [wbak, ("oatt", ts)], [p2k])
                            pg.op("dve", ("tensor_tensor", dict(
                                out=merged[:, dc, t2 * TS:(t2 + 1) * TS], in0=p2, in1=sa[:], op=ALU.mult)),
                                [p2k, sak], [("mg", dc, t2)])

                def comp_b(ws, dp):
                    (wgg, wggk), (wbg, wbgk) = ws
                    for j in range(2):
                        dc = dp * 2 + j
                        for t2 in range(2):
                            ts = th * 2 + t2
                            sl = slice(ts * TS, (ts + 1) * TS)
                            p1, p1k, _ = bank()
                            for kc in range(DC):
                                mm(p1, wgg[:, kc, j * P:(j + 1) * P], u[:, kc, sl], kc == 0, kc == DC - 1,
                                   [wggk, ("u", ts)], [p1k])
                            sa, sak = tmp32()
                            pg.op("act", ("activation", dict(out=sa[:], in_=p1, func=AF.Sigmoid)), [p1k], [sak])
                            p2, p2k, _ = bank()
                            for kc in range(DC):
                                mm(p2, wbg[:, kc, j * P:(j + 1) * P], ogla[:, kc, sl], kc == 0, kc == DC - 1,
                                   [wbgk, ("ogla", kc // 2, ts)], [p2k])
                            m2, m2k = tmp32()
                            pg.op("dve", ("tensor_tensor", dict(out=m2[:], in0=p2, in1=sa[:], op=ALU.mult)),
                                  [p2k, sak], [m2k])
                            pg.op("dve", ("tensor_tensor", dict(
                                out=merged[:, dc, t2 * TS:(t2 + 1) * TS], in0=merged[:, dc, t2 * TS:(t2 + 1) * TS],
                                in1=m2[:], op=ALU.add)), [m2k, ("mg", dc, t2)], [("mg", dc, t2)])

                def comp_o(ws, dp):
                    (wo, wok), = ws
                    for j in range(2):
                        dc = dp * 2 + j
                        for t2 in range(2):
                            ts = th * 2 + t2
                            sl = slice(ts * TS, (ts + 1) * TS)
                            po, pok, _ = bank()
                            for kc in range(DC):
                                mm(po, wo[:, kc, j * P:(j + 1) * P], merged[:, kc, t2 * TS:(t2 + 1) * TS], kc == 0,
                                   kc == DC - 1, [wok, ("mg", kc, t2)], [pok])
                            pg.op("dve", ("scalar_tensor_tensor", dict(
                                out=h[:, dc, sl], in0=po, scalar=Gmod[:, 1, dc, b:b + 1], in1=h[:, dc, sl],
                                op0=ALU.mult, op1=ALU.add)), [pok, ("Gmod", 1, b), ("h", ts)], [("h", ts)])

                tasks = []
                for dp in range(4):
                    c0 = dp * 256
                    tasks.append(([(kc_view(w_in, OFF_GA + c0, 256), DC, 256, "act"),
                                   (wba_all[:, :, c0:c0 + 256], 2, 256, "dve")],
                                  lambda ws, dp=dp: comp_a(ws, dp)))
                    tasks.append(([(kc_view(w_in, OFF_GG + c0, 256), DC, 256, "act"),
                                   (kc_view(w_branch_gla, c0, 256), DC, 256, "dve")],
                                  lambda ws, dp=dp: comp_b(ws, dp)))
                for dp in range(4):
                    tasks.append(([(kc_view(w_out, dp * 256, 256), DC, 256, "act")],
                                  lambda ws, dp=dp: comp_o(ws, dp)))
                run_tasks(tasks)
                if after_half is not None:
                    after_half(th)

        def dump(slot_i, b):
            if dbg and b == 0:
                pg.dma(("dma_start", dict(out=dbg_out[slot_i].rearrange("p (c t) -> p c t", c=DC), in_=h[:])), "dbg",
                       reads=[("h", t) for t in range(NTS)])

        MIX_A = ACC_KEYS + Q_KEYS + K_KEYS + V_KEYS
        for b in range(nbr):
            if b == 0:
                load_x(b, between=prologue_between)
                mod_fin(0)
                norm_mod(0, b)
            if upto >= 1:
                ffn(0, 0, b)
            dump(0, b)
            run_bg(len(bg))
            if upto >= 2:
                norm_mod(1, b)
                pg.transfer(G_KEYS, MIX_A + KD_KEYS)
                attention(b)
                pg.transfer(ACC_KEYS, OGLA_KEYS)
                pg.transfer(K_KEYS, GV_KEYS)
                pg.transfer(V_KEYS, QD_KEYS + KI_KEYS)
                gla(b)
                pg.transfer(GV_KEYS + QD_KEYS + KI_KEYS, MG_KEYS)
                if upto >= 3:
                    merge_out(b, after_half=lambda th: norm_mod(2, b, (2 * th, 2 * th + 1)))
                else:
                    merge_out(b)
                dump(1, b)
            if upto >= 3:
                pg.transfer(OGLA_KEYS + OATT_KEYS + MG_KEYS + KD_KEYS + MIX_A + GV_KEYS + QD_KEYS + KI_KEYS, G_KEYS)
                ffn(1, 2, b)
            elif upto >= 2:
                pg.transfer(OGLA_KEYS + OATT_KEYS + MG_KEYS + KD_KEYS + MIX_A + GV_KEYS + QD_KEYS + KI_KEYS, G_KEYS)
            if b + 1 < nbr:
                for ts in range(NTS):
                    store_y(b, range(ts * 4, ts * 4 + 4))
                    load_x(b + 1, range(ts * 4, ts * 4 + 4))
                    norm_mod(0, b + 1, (ts,))
            else:
                store_y(b)

        pg.emit(nc, block, sems, dsems, final_waits=["stg0", "stg1", "dbg", "wbx0", "wbx1", "wbx2", "wbx3"])
    return nc


def make_consts():
    cs = np.zeros((P, C_TOT), np.float32)
    cs[:, C_ID:C_ID + P] = np.eye(P, dtype=np.float32)
    kk = np.arange(P)[:, None]
    qq = np.arange(P)[None, :]
    BIG = 1.0e4
    prev = np.where(qq <= kk, (qq + P - kk).astype(np.float32), BIG)
    cur = np.where(qq >= kk, (qq - kk).astype(np.float32), BIG)
    cs[:, C_STEP:C_STEP + P] = prev
    cs[:, C_STEP + P:C_STEP + 2 * P] = cur
    cs[:, C_CAUS:C_CAUS + P] = (kk <= qq).astype(np.float32)
    bd = np.zeros((P, P), np.float32)
    bd[:64, :64] = 1.0
    bd[64:, 64:] = 1.0
    cs[:, C_BD:C_BD + P] = bd
    return cs


_NC_CACHE = {}


def _run(inputs, upto=99, dbg=False, ncores=8):
    key = (upto, dbg)
    if key not in _NC_CACHE:
        _NC_CACHE[key] = build_nc(upto, dbg)
    nc = _NC_CACHE[key]
    f = lambda a: np.ascontiguousarray(np.asarray(a, dtype=np.float32))
    sq = lambda a: f(a)[0]
    shared = {
        "w_mod": sq(inputs["w_mod"]), "b_mod": sq(inputs["b_mod"]),
        "g_ffn1": sq(inputs["g_ffn1"]), "g_mix": sq(inputs["g_mix"]), "g_ffn2": sq(inputs["g_ffn2"]),
        "ffn1_w1": sq(inputs["ffn1_w1"]), "ffn1_w3": sq(inputs["ffn1_w3"]), "ffn1_w2": sq(inputs["ffn1_w2"]),
        "ffn2_w1": sq(inputs["ffn2_w1"]), "ffn2_w3": sq(inputs["ffn2_w3"]), "ffn2_w2": sq(inputs["ffn2_w2"]),
        "w_in": sq(inputs["w_in"]), "q_norm_g": sq(inputs["q_norm_g"]), "k_norm_g": sq(inputs["k_norm_g"]),
        "gla_gate_up": sq(inputs["gla_gate_up"]), "gla_gate_bias": sq(inputs["gla_gate_bias"]),
        "gla_out_norm_g": sq(inputs["gla_out_norm_g"]), "w_branch_att": sq(inputs["w_branch_att"]),
        "w_branch_gla": sq(inputs["w_branch_gla"]), "w_out": sq(inputs["w_out"]),
        "consts": make_consts(),
    }
    xf = f(inputs["x"])
    cf = f(inputs["c"])
    in_maps = []
    for i in range(ncores):
        m = dict(shared)
        m["x"] = np.ascontiguousarray(xf[i * NB:(i + 1) * NB])
        m["c"] = np.ascontiguousarray(cf[i * NB:(i + 1) * NB])
        in_maps.append(m)
    res = run_bass_kernel_spmd(nc, in_maps, core_ids=list(range(ncores)))
    return res


def kernel(**inputs):
    res = _run(inputs)
    return np.concatenate([np.asarray(r["y"], dtype=np.float32) for r in res.results], axis=0)
```

```python
import math
from contextlib import ExitStack
import numpy as np
import concourse.bass as bass
import concourse.mybir as mybir
from concourse.bass_utils import run_bass_kernel_spmd

F32 = mybir.dt.float32
BF16 = mybir.dt.bfloat16
AF = mybir.ActivationFunctionType
ALU = mybir.AluOpType

P = 128
S = 2048
D = 1024
DC = 8
NTS = 4
TS = 512
FF = 2816
FC = 22
NB = 2
EPS = 1e-6
ATT_GROUPS = ((128, 1), (512, 4), (2048, 16))
OFF_AQ, OFF_AK, OFF_AV = 0, 768, 1536
OFF_GQ, OFF_GK, OFF_GV, OFF_GR, OFF_GD, OFF_GA, OFF_GG = 2304, 2816, 3328, 4352, 5376, 5392, 6416
IN_W = 7440
SLOPES = [2.0 ** (-8.0 * (i + 1) / 12.0) for i in range(12)]
C_ID, C_STEP, C_CAUS, C_BD = 0, 128, 384, 512
C_TOT = 640


class Prog:
    ENG = ("pe", "act", "dve", "pool", "sp")

    def __init__(self):
        self.ins = {e: [] for e in self.ENG}
        self.lastw = {}
        self.readers = {}
        self.waited = {e: {} for e in self.ENG}
        self.dcount = {}
        self.group_slots = set()

    def _deps(self, eng, reads, writes):
        toks = []
        for k in reads:
            t = self.lastw.get(k)
            if t is not None:
                toks.append(t)
        for k in writes:
            t = self.lastw.get(k)
            if t is not None:
                toks.append(t)
            toks.extend(self.readers.get(k, ()))
        need = {}
        for t in toks:
            src = (t[0], t[1])
            if t[0] == "e" and t[1] == eng and eng in ("pe", "sp"):
                continue
            if need.get(src, -1) < t[2]:
                need[src] = t[2]
        waits = []
        wd = self.waited[eng]
        for src, idx in need.items():
            if wd.get(src, -1) >= idx:
                continue
            wd[src] = idx
            if src[0] == "e":
                self.ins[src[1]][idx]["sig"] = True
            waits.append((src, idx))
        return waits

    def _commit(self, tok, reads, writes):
        for k in reads:
            self.readers.setdefault(k, []).append(tok)
        for k in writes:
            self.lastw[k] = tok
            self.readers[k] = []

    def op(self, eng, fn, reads=(), writes=()):
        waits = self._deps(eng, reads, writes)
        idx = len(self.ins[eng])
        self.ins[eng].append(dict(fn=fn, waits=waits, sig=False, dma=None))
        self._commit(("e", eng, idx), reads, writes)

    def dma(self, fn, slot, reads=(), writes=(), group=False, queue="sp"):
        waits = self._deps(queue, reads, writes)
        self.dcount[slot] = self.dcount.get(slot, 0) + 1
        if group:
            self.group_slots.add(slot)
            waits = [w for w in waits if not (w[0][0] == "d" and w[0][1] == slot)]
        self.ins[queue].append(dict(fn=fn, waits=waits, sig=False, dma=slot))
        self._commit(("d", slot, self.dcount[slot]), reads, writes)

    def transfer(self, old_keys, new_keys):
        toks = []
        for k in old_keys:
            t = self.lastw.get(k)
            if t is not None:
                toks.append(t)
            toks.extend(self.readers.get(k, ()))
        best = {}
        for t in toks:
            s = (t[0], t[1])
            if best.get(s, -1) < t[2]:
                best[s] = t[2]
        toks = [(s[0], s[1], i) for s, i in best.items()]
        for k in new_keys:
            self.lastw[k] = None
            self.readers[k] = list(toks)

    def emit(self, nc, block, sems, dsems, final_waits):
        signo = {}
        for e in self.ENG:
            c = 0
            arr = []
            for it in self.ins[e]:
                if it["sig"]:
                    c += 1
                arr.append(c)
            signo[e] = arr

        def run(e, eng):
            for it in self.ins[e]:
                for src, idx in it["waits"]:
                    if src[0] == "e":
                        eng.wait_ge(sems[src[1]], signo[src[1]][idx])
                    else:
                        cnt = self.dcount[src[1]] if src[1] in self.group_slots else idx
                        eng.wait_ge(dsems[src[1]], 16 * cnt)
                r = getattr(eng, it["fn"][0])(**it["fn"][1])
                if it["dma"] is not None:
                    r.then_inc(dsems[it["dma"]], 16)
                elif it["sig"]:
                    r.then_inc(sems[e], 1)
            if e == "sp":
                for slot in final_waits:
                    if self.dcount.get(slot, 0):
                        eng.wait_ge(dsems[slot], 16 * self.dcount[slot])

        @block.tensor
        def _(eng):
            run("pe", eng)

        @block.scalar
        def _(eng):
            run("act", eng)

        @block.vector
        def _(eng):
            run("dve", eng)

        @block.gpsimd
        def _(eng):
            run("pool", eng)

        @block.sync
        def _(eng):
            run("sp", eng)


def build_nc(upto=99, dbg=False, nbr=NB):
    nc = bass.Bass("TRN2", target_bir_lowering=False)

    def din(name, shape):
        return nc.dram_tensor(name, list(shape), F32, kind="ExternalInput").ap()

    x = din("x", [NB, S, D])
    c = din("c", [NB, D])
    w_mod = din("w_mod", [D, 9 * D])
    b_mod = din("b_mod", [9 * D])
    g_l = [din("g_ffn1", [D]), din("g_mix", [D]), din("g_ffn2", [D])]
    ffn_w = [(din("ffn1_w1", [D, FF]), din("ffn1_w3", [D, FF]), din("ffn1_w2", [FF, D])),
             (din("ffn2_w1", [D, FF]), din("ffn2_w3", [D, FF]), din("ffn2_w2", [FF, D]))]
    w_in = din("w_in", [D, IN_W])
    q_norm_g = din("q_norm_g", [64])
    k_norm_g = din("k_norm_g", [64])
    gla_gate_up = din("gla_gate_up", [16, 512])
    gla_gate_bias = din("gla_gate_bias", [512])
    gla_out_norm_g = din("gla_out_norm_g", [256])
    w_branch_att = din("w_branch_att", [256, D])
    w_branch_gla = din("w_branch_gla", [D, D])
    w_out = din("w_out", [D, D])
    consts = din("consts", [P, C_TOT])
    y = nc.dram_tensor("y", [NB, S, D], F32, kind="ExternalOutput").ap()
    dbg_out = None
    if dbg:
        dbg_out = nc.dram_tensor("dbg", [4, P, DC * S], F32, kind="ExternalOutput").ap()

    pg = Prog()
    es = ExitStack()

    def sb(name, shape, dt):
        return es.enter_context(nc.sbuf_tensor(name, list(shape), dt))

    with es:
        h = sb("h", [P, DC, S], F32)
        u = sb("u", [P, DC, S], BF16)
        arena = sb("arena", [P, 28 * 1024], BF16)
        NST = 2
        stg = [sb(f"stg{i}", [P, 2048], F32) for i in range(NST)]
        NWB = 4
        wbs = [sb(f"wb{i}", [P, 2048], BF16) for i in range(NWB)]
        cst = sb("cst", [P, C_TOT], F32)
        NT32 = 4
        t32 = [sb(f"t32_{i}", [P, TS], F32) for i in range(NT32)]
        NT16 = 4
        t16 = [sb(f"t16_{i}", [P, TS], BF16) for i in range(NT16)]
        ident_bf = sb("ident_bf", [P, P], BF16)
        ones_bf = sb("ones_bf", [P, P], BF16)
        bd_bf = sb("bd_bf", [P, P], BF16)
        kst = sb("kst", [P, 8], F32)
        cT = sb("cT", [P, DC, NB], F32)
        cact = sb("cact", [P, DC, NB], BF16)
        bmodT = sb("bmodT", [P, 72], F32)
        modT = sb("modT", [P, 72, NB], F32)
        gT = sb("gT", [P, 3, DC], F32)
        Amod = sb("Amod", [P, 3, DC, NB], F32)
        Gmod = sb("Gmod", [P, 3, DC, NB], F32)
        qg = sb("qg", [P, 1], F32)
        kg = sb("kg", [P, 1], F32)
        negb = sb("negb", [P, 4], F32)
        gno = sb("gno", [P, 2], F32)
        gup = sb("gup", [80, 512], BF16)
        gdT = sb("gdT", [80, 2 * TS], BF16)
        Sst = sb("Sst", [P, 256], F32)
        Sbf = sb("Sbf", [P, 2, 256], BF16)
        ebl = sb("ebl", [P, 16], F32)
        kdT = sb("kdT", [P, 2, P], BF16)
        kdtm = sb("kdtm", [P, 2, P], BF16)
        amk = sb("amk", [P, 2, P], BF16)
        psum = es.enter_context(nc.psum_tensor("ps", [P, 8 * TS], F32))
        psum16 = psum.bitcast(BF16) if hasattr(psum, "bitcast") else None

        sems = {e: es.enter_context(nc.semaphore(f"s_{e}")) for e in ("pe", "act", "dve", "pool")}
        dslots = ["pro", "stg0", "stg1", "dbg", "wbx0", "wbx1", "wbx2", "wbx3"]
        dsems = {s_: es.enter_context(nc.semaphore(f"d_{s_}")) for s_ in dslots}
        block = es.enter_context(nc.Block())

        cnt = dict(bank=0, t32=0, tL=0, t16=0, stg=0, wb=0, alt=0, xs=0)

        def bank():
            i = cnt["bank"] % 8
            cnt["bank"] += 1
            return psum[:, i * TS:(i + 1) * TS], ("ps", i), i

        def tmp32():
            i = cnt["t32"] % 2
            cnt["t32"] += 1
            return t32[i], ("t32", i)

        def tmpL():
            i = 2 + cnt["tL"] % 2
            cnt["tL"] += 1
            return t32[i], ("t32", i)

        def tmp16():
            i = cnt["t16"] % NT16
            cnt["t16"] += 1
            return t16[i], ("t16", i)

        def stage():
            i = cnt["stg"] % NST
            cnt["stg"] += 1
            return stg[i], ("stg", i), f"stg{i}"

        def xstage():
            i = cnt["xs"] % (NST + NWB)
            cnt["xs"] += 1
            if i < NST:
                return stg[i][:, 0:D], ("stg", i), f"stg{i}"
            j = i - NST
            return wbs[j][:].bitcast(F32), ("wb", j), f"wbx{j}"

        def alt():
            cnt["alt"] += 1
            return "act" if cnt["alt"] % 2 else "dve"

        def copy_op(eng, out, in_, reads, writes):
            if eng == "act":
                pg.op("act", ("copy", dict(out=out, in_=in_)), reads, writes)
            else:
                pg.op(eng, ("tensor_copy", dict(out=out, in_=in_)), reads, writes)

        def load_w(src, a, b_, ceng="pool"):
            st, skey, sslot = stage()
            n = a * b_
            stv = st[:, 0:n].rearrange("p (a b) -> p a b", a=a)
            pg.dma(("dma_start", dict(out=stv, in_=src)), sslot, writes=[skey])
            i = cnt["wb"] % NWB
            cnt["wb"] += 1
            wv = wbs[i][:, 0:n].rearrange("p (a b) -> p a b", a=a)
            copy_op(ceng, wv, stv, [skey], [("wb", i)])
            return wv, ("wb", i)

        def kc_view(w, c0, ncol):
            return w.rearrange("(kc p) f -> p kc f", p=P)[:, :, c0:c0 + ncol]

        def mm(out, lhsT, rhs, start, stop, reads, writes):
            pg.op("pe", ("matmul", dict(out=out, lhsT=lhsT, rhs=rhs, start=start, stop=stop)),
                  reads, writes)

        ukeys = [("u", t) for t in range(NTS)]

        def pro(out, in_, key, slow=False):
            if slow:
                pg.dma(("dma_start", dict(out=out, in_=in_, allow_slow_non_contiguous=True)), "pro",
                       writes=[key], group=True)
            else:
                pg.dma(("dma_start", dict(out=out, in_=in_)), "pro", writes=[key], group=True)

        pro(cst[:], consts, "cst")
        for b in range(NB):
            pro(cT[:, :, b], c[b].rearrange("(kc p) -> p kc", p=P), ("cT", b), slow=True)
        pro(bmodT[:], b_mod.rearrange("(n p) -> p n", p=P), "bmodT", slow=True)
        for l in range(3):
            pro(gT[:, l, :], g_l[l].rearrange("(n p) -> p n", p=P), ("gT", l), slow=True)
        for hh in range(2):
            pro(qg[hh * 64:(hh + 1) * 64, :], q_norm_g.rearrange("(p o) -> p o", o=1), ("qg", hh), slow=True)
            pro(kg[hh * 64:(hh + 1) * 64, :], k_norm_g.rearrange("(p o) -> p o", o=1), ("kg", hh), slow=True)
        pro(negb[:], gla_gate_bias.rearrange("(n p) -> p n", p=P), "negb", slow=True)
        pro(gno[:], gla_out_norm_g.rearrange("(n p) -> p n", p=P), "gno", slow=True)

        pg.op("dve", ("memset", dict(ap=ones_bf[:], constant=1.0)), [], ["ones"])
        pg.op("dve", ("memset", dict(ap=kst[:, 0:1], constant=EPS)), [], ["kst0"])
        pg.op("dve", ("memset", dict(ap=kst[:, 1:2], constant=1.0)), [], ["kst1"])
        pg.op("dve", ("memset", dict(ap=kst[:, 2:3], constant=math.log(128.0 ** -0.5))), [], ["kst2"])
        pg.op("dve", ("tensor_copy", dict(out=ident_bf[:], in_=cst[:, C_ID:C_ID + P])), ["cst"], ["identbf"])
        pg.op("dve", ("tensor_copy", dict(out=bd_bf[:], in_=cst[:, C_BD:C_BD + P])), ["cst"], ["bdbf"])
        for pb_ in (0, 32, 64):
            pro(t32[0][pb_:pb_ + 16, :], gla_gate_up, ("gupst", pb_))
        for pb_ in (0, 32, 64):
            pg.op("dve", ("tensor_copy", dict(out=gup[pb_:pb_ + 16, :], in_=t32[0][pb_:pb_ + 16, :])),
                  [("gupst", pb_)], ["gup", ("t32", 0)])
        pg.op("dve", ("tensor_scalar", dict(out=negb[:], in0=negb[:], scalar1=-1.0, scalar2=None, op0=ALU.mult)),
              ["negb"], ["negb"])
        pg.op("dve", ("tensor_scalar", dict(out=qg[:], in0=qg[:], scalar1=0.125, scalar2=None, op0=ALU.mult)),
              [("qg", 0), ("qg", 1)], [("qg", 0), ("qg", 1)])
        pg.op("act", ("activation", dict(out=cact[:], in_=cT[:], func=AF.Silu)), [("cT", 0), ("cT", 1)], ["cact"])
        ident = cst[:, C_ID:C_ID + P]
        stepsM = cst[:, C_STEP:C_STEP + 256]
        causM = cst[:, C_CAUS:C_CAUS + P]
        scanM = ones_bf[:]

        bg = []

        def run_bg(n=1):
            for _ in range(n):
                if bg:
                    bg.pop(0)()

        def mod_tile(t, ceng="pool"):
            l = t // 12
            wv, wk = load_w(kc_view(w_mod, t * 256, 256), DC, 256, ceng)
            pm, pmk, _ = bank()
            for j in range(2):
                for kc in range(DC):
                    mm(pm[:, j * 2:j * 2 + 2], wv[:, kc, j * P:(j + 1) * P], cact[:, kc, :],
                       kc == 0, kc == DC - 1, [wk, "cact"], [pmk])
            for b in range(NB):
                pg.op("dve", ("tensor_tensor", dict(out=modT[:, 2 * t:2 * t + 2, b], in0=pm[:, b:4:2],
                                                    in1=bmodT[:, 2 * t:2 * t + 2], op=ALU.add)),
                      [pmk, "bmodT"], [("modT", l, b)])

        def mod_fin(l):
            coef = 1.0 if l == 1 else 0.5
            for b in range(NB):
                pg.op("dve", ("scalar_tensor_tensor", dict(
                    out=Amod[:, l, :, b], in0=modT[:, l * 24 + 8:l * 24 + 16, b], scalar=1.0, in1=gT[:, l, :],
                    op0=ALU.add, op1=ALU.mult)), [("modT", l, b), ("gT", l)], [("Amod", l, b)])
                pg.op("dve", ("tensor_scalar", dict(
                    out=Gmod[:, l, :, b], in0=modT[:, l * 24 + 16:l * 24 + 24, b], scalar1=1.0, scalar2=coef,
                    op0=ALU.add, op1=ALU.mult)), [("modT", l, b)], [("Gmod", l, b)])

        HOIST = []

        PRO_ENG = ("dve", "act")

        def prologue_between(tt):
            if tt < 12:
                mod_tile(tt, PRO_ENG[tt % 2])
        for l in (1, 2):
            for t in range(12 * l, 12 * l + 12):
                bg.append(lambda t=t: mod_tile(t))
            bg.append(lambda l=l: mod_fin(l))

        def load_x(b, tts=range(16), between=None):
            for tt in tts:
                if between is not None:
                    between(tt)
                xs, skey, sslot = xstage()
                pg.dma(("dma_start", dict(out=xs, in_=x[b, tt * P:(tt + 1) * P, :])), sslot,
                       writes=[skey])
                for half in range(2):
                    pb, pk, _ = bank()
                    for j in range(4):
                        dc = half * 4 + j
                        pg.op("pe", ("transpose", dict(
                            out=pb[:, j * P:(j + 1) * P], in_=xs[:, dc * P:(dc + 1) * P], identity=ident)),
                            [skey, "cst"], [pk])
                    copy_op(alt(), h[:, half * 4:half * 4 + 4, tt * P:(tt + 1) * P],
                            pb.rearrange("p (a t) -> p a t", a=4), [pk], [("h", tt // 4)])

        def store_y(b, tts=range(16)):
            for tt in tts:
                ys, skey, sslot = xstage()
                for half in range(2):
                    pb, pk, _ = bank()
                    for j in range(4):
                        dc = half * 4 + j
                        pg.op("pe", ("transpose", dict(
                            out=pb[:, j * P:(j + 1) * P], in_=h[:, dc, tt * P:(tt + 1) * P], identity=ident)),
                            [("h", tt // 4), "cst"], [pk])
                    copy_op(alt(), ys[:, half * 512:(half + 1) * 512], pb, [pk], [skey])
                pg.dma(("dma_start", dict(out=y[b, tt * P:(tt + 1) * P, :], in_=ys)), sslot,
                       reads=[skey])

        def rstd_from(pss, pssk, scale):
            r32, rk = tmpL()
            pg.op("act", ("activation", dict(out=r32[:], in_=pss, func=AF.Ln, bias=kst[:, 0:1], scale=scale)),
                  [pssk, "kst0"], [rk])
            pg.op("act", ("activation", dict(out=r32[:], in_=r32[:], func=AF.Exp, scale=-0.5)), [rk], [rk])
            return r32, rk

        SQ_ENG = ("dve", "dve", "act", "dve", "dve", "act", "dve", "dve")

        def norm_mod(l, b, tsl=(0, 1, 2, 3)):
            def s1(ts):
                sl = slice(ts * TS, (ts + 1) * TS)
                pss, pssk, _ = bank()
                for dc in range(DC):
                    sq, sqk = tmp16()
                    e_ = SQ_ENG[dc]
                    if e_ == "act":
                        pg.op("act", ("activation", dict(out=sq[:], in_=h[:, dc, sl], func=AF.Square)), [("h", ts)], [sqk])
                    else:
                        pg.op(e_, ("tensor_tensor", dict(out=sq[:], in0=h[:, dc, sl], in1=h[:, dc, sl], op=ALU.mult)),
                              [("h", ts)], [sqk])
                    mm(pss, ones_bf[:], sq[:], dc == 0, dc == DC - 1, [sqk, "ones"], [pssk])
                return rstd_from(pss, pssk, 1.0 / D)

            def s2(ts, r32, rk):
                sl = slice(ts * TS, (ts + 1) * TS)
                for dc in range(DC):
                    tt_, tk = tmp32()
                    pg.op("dve", ("tensor_tensor", dict(out=tt_[:], in0=h[:, dc, sl], in1=r32[:], op=ALU.mult)),
                          [("h", ts), rk], [tk])
                    pg.op("act", ("activation", dict(
                        out=u[:, dc, sl], in_=tt_[:], func=AF.Identity, bias=modT[:, l * 24 + dc, b:b + 1],
                        scale=Amod[:, l, dc, b:b + 1])), [tk, ("modT", l, b), ("Amod", l, b)], [("u", ts)])

            rs = {tsl[0]: s1(tsl[0])}
            for i_, ts in enumerate(tsl):
                if i_ + 1 < len(tsl):
                    rs[tsl[i_ + 1]] = s1(tsl[i_ + 1])
                s2(ts, *rs.pop(ts))

        def gkey(f, ts):
            return ("g", f, ts)

        def ffn(fi, l, b):
            w1, w3, w2 = ffn_w[fi]
            gbuf = arena[:, 0:12 * S].rearrange("p (f t) -> p f t", f=12)
            for (f0, nf) in ((0, 12), (12, 10)):
                for tl in range(nf // 2):
                    c0 = (f0 + tl * 2) * P
                    w1v, w1k = load_w(kc_view(w1, c0, 256), DC, 256)
                    w3v, w3k = load_w(kc_view(w3, c0, 256), DC, 256)
                    for j in range(2):
                        fl = tl * 2 + j
                        for ts in range(NTS):
                            sl = slice(ts * TS, (ts + 1) * TS)
                            pa, pak, _ = bank()
                            pb, pbk, _ = bank()
                            for kc in range(DC):
                                mm(pa, w1v[:, kc, j * P:(j + 1) * P], u[:, kc, sl], kc == 0, kc == DC - 1,
                                   [w1k, ("u", ts)], [pak])
                            for kc in range(DC):
                                mm(pb, w3v[:, kc, j * P:(j + 1) * P], u[:, kc, sl], kc == 0, kc == DC - 1,
                                   [w3k, ("u", ts)], [pbk])
                            s1, s1k = tmp16()
                            pg.op("act", ("activation", dict(out=s1[:], in_=pa, func=AF.Silu)),
                                  [pak], [s1k])
                            pg.op("dve", ("tensor_tensor", dict(
                                out=gbuf[:, fl, sl], in0=pb, in1=s1[:], op=ALU.mult)), [pbk, s1k], [gkey(fl, ts)])
                    run_bg()
                w2v_all = w2.rearrange("(fc p) d -> p fc d", p=P)
                for dc in range(DC):
                    w2v, w2k = load_w(w2v_all[:, f0:f0 + nf, dc * P:(dc + 1) * P], nf, P)
                    for ts in range(NTS):
                        sl = slice(ts * TS, (ts + 1) * TS)
                        po, pok, _ = bank()
                        for fl in range(nf):
                            mm(po, w2v[:, fl, :], gbuf[:, fl, sl], fl == 0, fl == nf - 1, [w2k, gkey(fl, ts)], [pok])
                        pg.op("dve", ("scalar_tensor_tensor", dict(
                            out=h[:, dc, sl], in0=po, scalar=Gmod[:, l, dc, b:b + 1], in1=h[:, dc, sl],
                            op0=ALU.mult, op1=ALU.add)), [pok, ("Gmod", l, b), ("h", ts)], [("h", ts)])
                    run_bg()

        G_KEYS = [gkey(f, t) for f in range(12) for t in range(NTS)]

        KB = 512
        acc = arena[:, 0:32 * KB].bitcast(F32).rearrange("p (c o t) -> p c o t", c=2, o=2)
        ogla = arena[:, 0:32 * KB].rearrange("p (c t) -> p c t", c=8)
        qbuf = arena[:, 32 * KB:40 * KB].rearrange("p (c t) -> p c t", c=2)
        kbuf = arena[:, 40 * KB:48 * KB].rearrange("p (c t) -> p c t", c=2)
        vtm = arena[:, 48 * KB:56 * KB].rearrange("p (b f) -> p b f", b=16)
        oatt = arena[:, 32 * KB:40 * KB].rearrange("p (c t) -> p c t", c=2)
        gvtm = arena[:, 40 * KB:48 * KB].rearrange("p (b f) -> p b f", b=16)
        qdec = arena[:, 48 * KB:52 * KB]
        kinv = arena[:, 52 * KB:56 * KB]
        merged = arena[:, 40 * KB:56 * KB].rearrange("p (c t) -> p c t", c=8)

        ACC_KEYS = [("acc", ch, t) for ch in range(2) for t in range(NTS)]
        Q_KEYS = [("q", ch, t) for ch in range(2) for t in range(NTS)]
        K_KEYS = [("k", ch, t) for ch in range(2) for t in range(NTS)]
        V_KEYS = [("v", tb) for tb in range(16)]
        OATT_KEYS = [("oatt", t) for t in range(NTS)]
        OGLA_KEYS = [("ogla", hh, t) for hh in range(4) for t in range(NTS)]
        GV_KEYS = [("gv", tb) for tb in range(16)]
        QD_KEYS = [("qd", t) for t in range(NTS)]
        KI_KEYS = [("ki", t) for t in range(NTS)]
        KD_KEYS = []
        MG_KEYS = [("mg", dc, t) for dc in range(DC) for t in range(2)]

        def tok_slice(dil, r, n):
            st0 = r + dil * P * n
            return slice(st0, st0 + dil * (P - 1) + 1, dil)

        def blk_ts(dil, r, n):
            if dil == 1:
                return [n // 4]
            return list(range(NTS)) if dil == 16 else [n]

        def attention(b):
            for gi, (win, dil) in enumerate(ATT_GROUPS):
                nb = S // dil // P
                wq_, wqk_ = load_w(kc_view(w_in, OFF_AQ + gi * 256, 256), DC, 256)
                wk_, wkk_ = load_w(kc_view(w_in, OFF_AK + gi * 256, 256), DC, 256)
                items = [(wq_, wqk_, qbuf, qg, "q", ch, ts) for ch in range(2) for ts in range(NTS)] + \
                        [(wk_, wkk_, kbuf, kg, "k", ch, ts) for ch in range(2) for ts in range(NTS)]

                def qk_s1(it):
                    wv, wk, buf, gain, nm, ch, ts = it
                    sl = slice(ts * TS, (ts + 1) * TS)
                    pq, pqk, _ = bank()
                    for kc in range(DC):
                        mm(pq, wv[:, kc, ch * P:(ch + 1) * P], u[:, kc, sl], kc == 0, kc == DC - 1,
                           [wk, ("u", ts)], [pqk])
                    sq, sqk = tmp16()
                    pg.op("act", ("activation", dict(out=sq[:], in_=pq, func=AF.Square)), [pqk], [sqk])
                    return (pq, pqk, sq, sqk)

                def qk_s2(it, ctx):
                    wv, wk, buf, gain, nm, ch, ts = it
                    pq, pqk, sq, sqk = ctx
                    sl = slice(ts * TS, (ts + 1) * TS)
                    pss, pssk, _ = bank()
                    mm(pss, bd_bf[:], sq[:], True, True, [sqk, "bdbf"], [pssk])
                    r32, rk = rstd_from(pss, pssk, 1.0 / 64)
                    pg.op("dve", ("scalar_tensor_tensor", dict(out=buf[:, ch, sl], in0=pq, scalar=gain[:, 0:1], in1=r32[:],
                                                               op0=ALU.mult, op1=ALU.mult)),
                          [pqk, rk, (nm + "g", 0), (nm + "g", 1)], [(nm, ch, ts)])

                ctxs = {0: qk_s1(items[0])}
                for i in range(len(items)):
                    if i + 1 < len(items):
                        ctxs[i + 1] = qk_s1(items[i + 1])
                    qk_s2(items[i], ctxs.pop(i))
                wv, wk = load_w(kc_view(w_in, OFF_AV + gi * 256, 256), DC, 256)
                blocks = [(r, n) for r in range(dil) for n in range(nb)]
                for tb, (r, n) in enumerate(blocks):
                    tsl = tok_slice(dil, r, n)
                    pv, pvk, _ = bank()
                    for kc in range(DC):
                        mm(pv[:, 0:256], u[:, kc, tsl], wv[:, kc, :], kc == 0, kc == DC - 1,
                           [wk] + [("u", t) for t in blk_ts(dil, r, n)], [pvk])
                    copy_op(alt(), vtm[:, tb, :], pv[:, 0:256], [pvk], [("v", tb)])
                work = [(ch, tb) for ch in range(2) for tb in range(len(blocks))]
                pend = {}

                def emit_qk(ch, tb):
                    r, n = blocks[tb]
                    tsl = tok_slice(dil, r, n)
                    tss = blk_ts(dil, r, n)
                    Es = []
                    for hh in range(2):
                        head = gi * 4 + ch * 2 + hh
                        cs = -SLOPES[head] * dil
                        ps_, psk, _ = bank()
                        prt = slice(hh * 64, (hh + 1) * 64)
                        rd = [("q", ch, t) for t in tss] + [("k", ch, t) for t in tss]
                        mm(ps_[:, 128:256], kbuf[prt, ch, tsl], qbuf[prt, ch, tsl], True, True, rd, [psk])
                        lo = 128
                        if n > 0:
                            psl = tok_slice(dil, r, n - 1)
                            rd2 = rd + [("k", ch, t) for t in blk_ts(dil, r, n - 1)]
                            mm(ps_[:, 0:128], kbuf[prt, ch, psl], qbuf[prt, ch, tsl], True, True, rd2, [psk])
                            lo = 0
                        t_, tk = tmp32()
                        pg.op("dve", ("scalar_tensor_tensor", dict(
                            out=t_[:, lo:256], in0=stepsM[:, lo:256], scalar=cs, in1=ps_[:, lo:256],
                            op0=ALU.mult, op1=ALU.add)), [psk, "cst"], [tk])
                        E, Ek = tmp16()
                        pg.op("act", ("activation", dict(out=E[:, lo:256], in_=t_[:, lo:256],
                                                                               func=AF.Exp)), [tk], [Ek])
                        Es.append((E, Ek, lo))
                    pend[(ch, tb)] = Es

                def emit_pv(ch, tb):
                    r, n = blocks[tb]
                    tsl = tok_slice(dil, r, n)
                    tss = blk_ts(dil, r, n)
                    Es = pend.pop((ch, tb))
                    po, pok, _ = bank()
                    for hh in range(2):
                        E, Ek, lo = Es[hh]
                        prt = slice(hh * 64, (hh + 1) * 64)
                        vc = slice(ch * P + hh * 64, ch * P + hh * 64 + 64)
                        mm(po[prt, 0:128], vtm[:, tb, vc], E[:, 128:256], True, n == 0, [Ek, ("v", tb)], [pok])
                        if n > 0:
                            mm(po[prt, 0:128], vtm[:, tb - 1, vc], E[:, 0:128], False, True, [Ek, ("v", tb - 1)], [pok])
                        mm(po[prt, 128:256], ones_bf[:, 0:64], E[:, 128:256], True, n == 0, [Ek, "ones"], [pok])
                        if n > 0:
                            mm(po[prt, 128:256], ones_bf[:, 0:64], E[:, 0:128], False, True, [Ek, "ones"], [pok])
                    pov = po[:, 0:256].rearrange("p (o t) -> p o t", o=2)
                    akeys = [("acc", ch, t) for t in tss]
                    if gi == 0:
                        pg.op("dve", ("tensor_copy", dict(out=acc[:, ch, :, tsl], in_=pov)), [pok], akeys)
                    else:
                        pg.op("dve", ("tensor_tensor", dict(out=acc[:, ch, :, tsl], in0=acc[:, ch, :, tsl], in1=pov,
                                                               op=ALU.add)), [pok] + akeys, akeys)

                emit_qk(*work[0])
                for i in range(len(work)):
                    if i + 1 < len(work):
                        emit_qk(*work[i + 1])
                    emit_pv(*work[i])
            pg.transfer(Q_KEYS, OATT_KEYS)
            for ch in range(2):
                for ts in range(NTS):
                    sl = slice(ts * TS, (ts + 1) * TS)
                    r_, rk = tmp32()
                    pg.op("dve", ("reciprocal", dict(out=r_[:], in_=acc[:, ch, 1, sl])),
                          [("acc", ch, ts)], [rk])
                    pg.op("dve", ("tensor_tensor", dict(out=oatt[:, ch, sl], in0=acc[:, ch, 0, sl],
                                                                              in1=r_[:], op=ALU.mult)),
                          [("acc", ch, ts), rk], [("oatt", ts)])

        def gd_pb(ts):
            return 0 if ts == 3 else 32 * ts

        def gd_ap(ts):
            c0 = TS if ts == 3 else 0
            return gdT[gd_pb(ts):gd_pb(ts) + 16, c0:c0 + TS]

        def gla(b):
            wv, wk = load_w(kc_view(w_in, OFF_GD, 16), DC, 16)
            for ts in range(NTS):
                sl = slice(ts * TS, (ts + 1) * TS)
                pd, pdk, _ = bank()
                for kc in range(DC):
                    mm(pd[gd_pb(ts):gd_pb(ts) + 16, :], wv[:, kc, :], u[:, kc, sl], kc == 0, kc == DC - 1, [wk, ("u", ts)], [pdk])
                copy_op("act", gd_ap(ts), pd[gd_pb(ts):gd_pb(ts) + 16, :], [pdk], [("gd", ts)])

            def outnorm_units(hh):
                units = []
                st_ = {}

                def load():
                    st_["w"] = load_w(kc_view(w_in, OFF_GR + hh * 256, 256), DC, 256)
                units.append(load)
                for ts in range(NTS):
                    sl = slice(ts * TS, (ts + 1) * TS)

                    def u_ss_a(ts=ts, sl=sl):
                        pss, pssk, _ = bank()
                        for dv in range(2):
                            sq, sqk = tmp16()
                            pg.op("act", ("activation", dict(out=sq[:], in_=ogla[:, hh * 2 + dv, sl], func=AF.Square)),
                                  [("ogla", hh, ts)], [sqk])
                            mm(pss, ones_bf[:], sq[:], dv == 0, dv == 1, [sqk, "ones"], [pssk])
                        st_[("ss", ts)] = (pss, pssk)

                    def u_ss_b(ts=ts):
                        pss, pssk = st_[("ss", ts)]
                        st_[ts] = rstd_from(pss, pssk, 1.0 / 256)
                    units.append(u_ss_a)
                    units.append(u_ss_b)
                    for dv in range(2):
                        def u_dv_a(ts=ts, sl=sl, dv=dv):
                            wv_, wk_ = st_["w"]
                            pr, prk, _ = bank()
                            for kc in range(DC):
                                mm(pr, wv_[:, kc, dv * P:(dv + 1) * P], u[:, kc, sl], kc == 0, kc == DC - 1,
                                   [wk_, ("u", ts)], [prk])
                            sg, sgk = tmp32()
                            pg.op("act", ("activation", dict(out=sg[:], in_=pr, func=AF.Silu)), [prk], [sgk])
                            st_[("sg", ts, dv)] = (sg, sgk)

                        def u_dv_b(ts=ts, sl=sl, dv=dv):
                            r32, rk = st_[ts]
                            sg, sgk = st_[("sg", ts, dv)]
                            t1, t1k = tmp32()
                            pg.op("dve", ("scalar_tensor_tensor", dict(
                                out=t1[:], in0=ogla[:, hh * 2 + dv, sl], scalar=gno[:, dv:dv + 1], in1=r32[:],
                                op0=ALU.mult, op1=ALU.mult)), [("ogla", hh, ts), rk, "gno"], [t1k])
                            pg.op("dve", ("tensor_tensor", dict(
                                out=ogla[:, hh * 2 + dv, sl], in0=t1[:], in1=sg[:], op=ALU.mult)),
                                [t1k, sgk, ("ogla", hh, ts)], [("ogla", hh, ts)])
                        units.append(u_dv_a)
                        units.append(u_dv_b)
                return units

            pending = []
            for hh in range(4):
                wq, wqk = load_w(kc_view(w_in, OFF_GQ + hh * P, P), DC, P)
                wkk, wkkk = load_w(kc_view(w_in, OFF_GK + hh * P, P), DC, P)
                wvv, wvk = load_w(kc_view(w_in, OFF_GV + hh * 256, 256), DC, 256)
                vprev = None

                def v_transposes(ts_, vts_):
                    pt, ptk, bi = bank()
                    for bl in range(4):
                        for dv in range(2):
                            vt, vtk = vts_[dv]
                            o0 = bi * 2 * TS + bl * 256 + dv * P
                            pg.op("pe", ("transpose", dict(out=psum16[:, o0:o0 + P], in_=vt[:, bl * P:(bl + 1) * P],
                                                           identity=ident_bf[:])), [vtk, "identbf"], [ptk])
                    copy_op("act", gvtm[:, ts_ * 4:ts_ * 4 + 4, :],
                            psum16[:, bi * 2 * TS:(bi + 1) * 2 * TS].rearrange("p (b f) -> p b f", b=4),
                            [ptk], [("gv", tb) for tb in range(ts_ * 4, ts_ * 4 + 4)])

                for ts in range(NTS):
                    sl = slice(ts * TS, (ts + 1) * TS)
                    px, pxk, _ = bank()
                    mm(px, gup[gd_pb(ts):gd_pb(ts) + 16, hh * P:(hh + 1) * P], gd_ap(ts), True, True, ["gup", ("gd", ts)], [pxk])
                    e_, ek = tmp32()
                    pg.op("act", ("activation", dict(out=e_[:], in_=px, func=AF.Exp, bias=negb[:, hh:hh + 1], scale=-1.0)),
                          [pxk, "negb"], [ek])
                    sp_, spk = tmp32()
                    pg.op("act", ("activation", dict(out=sp_[:], in_=e_[:], func=AF.Ln, bias=kst[:, 1:2], scale=1.0)),
                          [ek, "kst1"], [spk])
                    B_, Bk = tmpL()
                    bks = [(Bk, c4) for c4 in range(4)]
                    for c4 in range(4):
                        pg.op("dve", ("tensor_tensor_scan", dict(
                            out=B_[:, c4 * P:(c4 + 1) * P], data0=scanM, data1=sp_[:, c4 * P:(c4 + 1) * P], initial=0.0,
                            op0=ALU.mult, op1=ALU.add)), [spk, "ones"], [bks[c4]])
                    pq, pqk, _ = bank()
                    for kc in range(DC):
                        mm(pq, wq[:, kc, :], u[:, kc, sl], kc == 0, kc == DC - 1, [wqk, ("u", ts)], [pqk])
                    pk_, pkk, _ = bank()
                    for kc in range(DC):
                        mm(pk_, wkk[:, kc, :], u[:, kc, sl], kc == 0, kc == DC - 1, [wkkk, ("u", ts)], [pkk])
                    pg.op("act", ("activation", dict(
                        out=ebl[:, ts * 4:ts * 4 + 4], in_=B_[:, 127:512:128], func=AF.Exp, scale=-1.0 / 16)),
                        bks, [("ebl", ts)])
                    eb, ebk = tmp32()
                    pg.op("act", ("activation", dict(out=eb[:], in_=B_[:], func=AF.Exp, bias=kst[:, 2:3], scale=-1.0 / 16)),
                          bks + ["kst2"], [ebk])
                    pg.op("dve", ("tensor_tensor", dict(out=qdec[:, sl], in0=pq, in1=eb[:], op=ALU.mult)),
                          [pqk, ebk], [("qd", ts)])
                    en, enk = tmp32()
                    pg.op("act", ("activation", dict(out=en[:], in_=B_[:], func=AF.Exp, scale=1.0 / 16)), bks, [enk])
                    pg.op("dve", ("tensor_tensor", dict(out=kinv[:, sl], in0=pk_, in1=en[:], op=ALU.mult)),
                          [pkk, enk], [("ki", ts)])
                    vts = []
                    for dv in range(2):
                        pv, pvk, _ = bank()
                        for kc in range(DC):
                            mm(pv, wvv[:, kc, dv * P:(dv + 1) * P], u[:, kc, sl], kc == 0, kc == DC - 1,
                               [wvk, ("u", ts)], [pvk])
                        vt, vtk = tmp16()
                        copy_op("act", vt[:], pv, [pvk], [vtk])
                        vts.append((vt, vtk))
                    if vprev is not None:
                        v_transposes(*vprev)
                    vprev = (ts, vts)
                v_transposes(*vprev)
                vprev = None

                def emit_kd(cc):
                    csl = slice(cc * P, (cc + 1) * P)
                    j = cc % 2
                    pg.op("dve", ("tensor_scalar", dict(
                        out=kdT[:, j, :], in0=kinv[:, csl], scalar1=ebl[:, cc:cc + 1], scalar2=None, op0=ALU.mult)),
                        [("ki", cc // 4), ("ebl", cc // 4)], [("kdT", j)])
                    pt, ptk, bi = bank()
                    pt16 = psum16[:, bi * 2 * TS:bi * 2 * TS + P]
                    pg.op("pe", ("transpose", dict(out=pt16, in_=kdT[:, j, :], identity=ident_bf[:])),
                          [("kdT", j), "identbf"], [ptk])
                    copy_op("act", kdtm[:, j, :], pt16, [ptk], [("kd", j)])
                emit_kd(0)
                for cc in range(16):
                    csl = slice(cc * P, (cc + 1) * P)
                    ts = cc // 4
                    pa, pak, _ = bank()
                    mm(pa[:, 0:P], kinv[:, csl], qdec[:, csl], True, True, [("ki", ts), ("qd", ts)], [pak])
                    j = cc % 2
                    if cc + 1 < 15:
                        emit_kd(cc + 1)
                    pg.op("dve", ("tensor_tensor", dict(out=amk[:, j, :], in0=pa[:, 0:P], in1=causM, op=ALU.mult)),
                          [pak, "cst"], [("amk", j)])
                    if cc < 15:
                        pu, puk, _ = bank()
                        mm(pu[:, 0:256], kdtm[:, j, :], gvtm[:, cc, :], True, True, [("kd", j), ("gv", cc)], [puk])
                    po, pok, _ = bank()
                    for dv in range(2):
                        mm(po[:, dv * P:(dv + 1) * P], gvtm[:, cc, dv * P:(dv + 1) * P], amk[:, j, :], True, cc == 0,
                           [("gv", cc), ("amk", j)], [pok])
                        if cc > 0:
                            mm(po[:, dv * P:(dv + 1) * P], Sbf[:, (cc - 1) % 2, dv * P:(dv + 1) * P], qdec[:, csl], False, True,
                               [("Sbf", (cc - 1) % 2), ("qd", ts)], [pok])
                    if cc < 15:
                        if cc == 0:
                            pg.op("dve", ("tensor_copy", dict(out=Sst[:], in_=pu[:, 0:256])), [puk], ["Sst"])
                        else:
                            pg.op("dve", ("scalar_tensor_tensor", dict(
                                out=Sst[:], in0=Sst[:], scalar=ebl[:, cc:cc + 1], in1=pu[:, 0:256],
                                op0=ALU.mult, op1=ALU.add)), [puk, "Sst", ("ebl", ts)], ["Sst"])
                        copy_op("act", Sbf[:, cc % 2, :], Sst[:], ["Sst"], [("Sbf", cc % 2)])
                    copy_op("act", ogla[:, hh * 2:hh * 2 + 2, csl], po[:, 0:256].rearrange("p (a t) -> p a t", a=2),
                            [pok], [("ogla", hh, ts)])
                    for _ in range(2):
                        if pending:
                            pending.pop(0)()
                while pending:
                    pending.pop(0)()
                pending = outnorm_units(hh)
            while pending:
                pending.pop(0)()

        def run_tasks(tasks):
            loaded = {}

            def ld(i):
                loaded[i] = [load_w(src, a, b_, ce) for (src, a, b_, ce) in tasks[i][0]]
            ld(0)
            for i in range(len(tasks)):
                if i + 1 < len(tasks):
                    ld(i + 1)
                tasks[i][1](loaded.pop(i))

        def merge_out(b, after_half=None):
            wba_all = w_branch_att.rearrange("(kc p) d -> p kc d", p=P)
            for th in range(2):
                def comp_a(ws, dp):
                    (wga, wgak), (wba, wbak) = ws
                    for j in range(2):
                        dc = dp * 2 + j
                        for t2 in range(2):
                            ts = th * 2 + t2
                            sl = slice(ts * TS, (ts + 1) * TS)
                            p1, p1k, _ = bank()
                            for kc in range(DC):
                                mm(p1, wga[:, kc, j * P:(j + 1) * P], u[:, kc, sl], kc == 0, kc == DC - 1,
                                   [wgak, ("u", ts)], [p1k])
                            sa, sak = tmp32()
                            pg.op("act", ("activation", dict(out=sa[:], in_=p1, func=AF.Sigmoid)), [p1k], [sak])
                            p2, p2k, _ = bank()
                            for c2 in range(2):
                                mm(p2, wba[:, c2, j * P:(j + 1) * P], oatt[:, c2, sl], c2 == 0, c2 == 1,
                                   [wbak, ("oatt", ts)], [p2k])
                            pg.op("dve", ("tensor_tensor", dict(
                                out=merged[:, dc, t2 * TS:(t2 + 1) * TS], in0=p2, in1=sa[:], op=ALU.mult)),
                                [p2k, sak], [("mg", dc, t2)])

                def comp_b(ws, dp):
                    (wgg, wggk), (wbg, wbgk) = ws
                    for j in range(2):
                        dc = dp * 2 + j
                        for t2 in range(2):
                            ts = th * 2 + t2
                            sl = slice(ts * TS, (ts + 1) * TS)
                            p1, p1k, _ = bank()
                            for kc in range(DC):
                                mm(p1, wgg[:, kc, j * P:(j + 1) * P], u[:, kc, sl], kc == 0, kc == DC - 1,
                                   [wggk, ("u", ts)], [p1k])
                            sa, sak = tmp32()
                            pg.op("act", ("activation", dict(out=sa[:], in_=p1, func=AF.Sigmoid)), [p1k], [sak])
                            p2, p2k, _ = bank()
                            for kc in range(DC):
                                mm(p2, wbg[:, kc, j * P:(j + 1) * P], ogla[:, kc, sl], kc == 0, kc == DC - 1,
                                   [wbgk, ("ogla", kc // 2, ts)], [p2k])
                            m2, m2k = tmp32()
                            pg.op("dve", ("tensor_tensor", dict(out=m2[:], in0=p2, in1=sa[:], op=ALU.mult)),
                                  [p2k, sak], [m2k])
                            pg.op("dve", ("tensor_tensor", dict(
                                out=merged[:, dc, t2 * TS:(t2 + 1) * TS], in0=merged[:, dc, t2 * TS:(t2 + 1) * TS],
                                in1=m2[:], op=ALU.add)), [m2k, ("mg", dc, t2)], [("mg", dc, t2)])

                def comp_o(ws, dp):
                    (wo, wok), = ws
                    for j in range(2):
                        dc = dp * 2 + j
                        for t2 in range(2):
                            ts = th * 2 + t2
                            sl = slice(ts * TS, (ts + 1) * TS)
                            po, pok, _ = bank()
                            for kc in range(DC):
                                mm(po, wo[:, kc, j * P:(j + 1) * P], merged[:, kc, t2 * TS:(t2 + 1) * TS], kc == 0,
                                   kc == DC - 1, [wok, ("mg", kc, t2)], [pok])
                            pg.op("dve", ("scalar_tensor_tensor", dict(
                                out=h[:, dc, sl], in0=po, scalar=Gmod[:, 1, dc, b:b + 1], in1=h[:, dc, sl],
                                op0=ALU.mult, op1=ALU.add)), [pok, ("Gmod", 1, b), ("h", ts)], [("h", ts)])

                tasks = []
                for dp in range(4):
                    c0 = dp * 256
                    tasks.append(([(kc_view(w_in, OFF_GA + c0, 256), DC, 256, "act"),
                                   (wba_all[:, :, c0:c0 + 256], 2, 256, "dve")],
                                  lambda ws, dp=dp: comp_a(ws, dp)))
                    tasks.append(([(kc_view(w_in, OFF_GG + c0, 256), DC, 256, "act"),
                                   (kc_view(w_branch_gla, c0, 256), DC, 256, "dve")],
                                  lambda ws, dp=dp: comp_b(ws, dp)))
                for dp in range(4):
                    tasks.append(([(kc_view(w_out, dp * 256, 256), DC, 256, "act")],
                                  lambda ws, dp=dp: comp_o(ws, dp)))
                run_tasks(tasks)
                if after_half is not None:
                    after_half(th)

        def dump(slot_i, b):
            if dbg and b == 0:
                pg.dma(("dma_start", dict(out=dbg_out[slot_i].rearrange("p (c t) -> p c t", c=DC), in_=h[:])), "dbg",
                       reads=[("h", t) for t in range(NTS)])

        MIX_A = ACC_KEYS + Q_KEYS + K_KEYS + V_KEYS
        for b in range(nbr):
            if b == 0:
                load_x(b, between=prologue_between)
                mod_fin(0)
                norm_mod(0, b)
            if upto >= 1:
                ffn(0, 0, b)
            dump(0, b)
            run_bg(len(bg))
            if upto >= 2:
                norm_mod(1, b)
                pg.transfer(G_KEYS, MIX_A + KD_KEYS)
                attention(b)
                pg.transfer(ACC_KEYS, OGLA_KEYS)
                pg.transfer(K_KEYS, GV_KEYS)
                pg.transfer(V_KEYS, QD_KEYS + KI_KEYS)
                gla(b)
                pg.transfer(GV_KEYS + QD_KEYS + KI_KEYS, MG_KEYS)
                if upto >= 3:
                    merge_out(b, after_half=lambda th: norm_mod(2, b, (2 * th, 2 * th + 1)))
                else:
                    merge_out(b)
                dump(1, b)
            if upto >= 3:
                pg.transfer(OGLA_KEYS + OATT_KEYS + MG_KEYS + KD_KEYS + MIX_A + GV_KEYS + QD_KEYS + KI_KEYS, G_KEYS)
                ffn(1, 2, b)
            elif upto >= 2:
                pg.transfer(OGLA_KEYS + OATT_KEYS + MG_KEYS + KD_KEYS + MIX_A + GV_KEYS + QD_KEYS + KI_KEYS, G_KEYS)
            if b + 1 < nbr:
                for ts in range(NTS):
                    store_y(b, range(ts * 4, ts * 4 + 4))
                    load_x(b + 1, range(ts * 4, ts * 4 + 4))
                    norm_mod(0, b + 1, (ts,))
            else:
                store_y(b)

        pg.emit(nc, block, sems, dsems, final_waits=["stg0", "stg1", "dbg", "wbx0", "wbx1", "wbx2", "wbx3"])
    return nc


def make_consts():
    cs = np.zeros((P, C_TOT), np.float32)
    cs[:, C_ID:C_ID + P] = np.eye(P, dtype=np.float32)
    kk = np.arange(P)[:, None]
    qq = np.arange(P)[None, :]
    BIG = 1.0e4
    prev = np.where(qq <= kk, (qq + P - kk).astype(np.float32), BIG)
    cur = np.where(qq >= kk, (qq - kk).astype(np.float32), BIG)
    cs[:, C_STEP:C_STEP + P] = prev
    cs[:, C_STEP + P:C_STEP + 2 * P] = cur
    cs[:, C_CAUS:C_CAUS + P] = (kk <= qq).astype(np.float32)
    bd = np.zeros((P, P), np.float32)
    bd[:64, :64] = 1.0
    bd[64:, 64:] = 1.0
    cs[:, C_BD:C_BD + P] = bd
    return cs


_NC_CACHE = {}


def _run(inputs, upto=99, dbg=False, ncores=8):
    key = (upto, dbg)
    if key not in _NC_CACHE:
        _NC_CACHE[key] = build_nc(upto, dbg)
    nc = _NC_CACHE[key]
    f = lambda a: np.ascontiguousarray(np.asarray(a, dtype=np.float32))
    sq = lambda a: f(a)[0]
    shared = {
        "w_mod": sq(inputs["w_mod"]), "b_mod": sq(inputs["b_mod"]),
        "g_ffn1": sq(inputs["g_ffn1"]), "g_mix": sq(inputs["g_mix"]), "g_ffn2": sq(inputs["g_ffn2"]),
        "ffn1_w1": sq(inputs["ffn1_w1"]), "ffn1_w3": sq(inputs["ffn1_w3"]), "ffn1_w2": sq(inputs["ffn1_w2"]),
        "ffn2_w1": sq(inputs["ffn2_w1"]), "ffn2_w3": sq(inputs["ffn2_w3"]), "ffn2_w2": sq(inputs["ffn2_w2"]),
        "w_in": sq(inputs["w_in"]), "q_norm_g": sq(inputs["q_norm_g"]), "k_norm_g": sq(inputs["k_norm_g"]),
        "gla_gate_up": sq(inputs["gla_gate_up"]), "gla_gate_bias": sq(inputs["gla_gate_bias"]),
        "gla_out_norm_g": sq(inputs["gla_out_norm_g"]), "w_branch_att": sq(inputs["w_branch_att"]),
        "w_branch_gla": sq(inputs["w_branch_gla"]), "w_out": sq(inputs["w_out"]),
        "consts": make_consts(),
    }
    xf = f(inputs["x"])
    cf = f(inputs["c"])
    in_maps = []
    for i in range(ncores):
        m = dict(shared)
        m["x"] = np.ascontiguousarray(xf[i * NB:(i + 1) * NB])
        m["c"] = np.ascontiguousarray(cf[i * NB:(i + 1) * NB])
        in_maps.append(m)
    res = run_bass_kernel_spmd(nc, in_maps, core_ids=list(range(ncores)))
    return res


def kernel(**inputs):
    res = _run(inputs)
    return np.concatenate([np.asarray(r["y"], dtype=np.float32) for r in res.results], axis=0)
```
